# Optimizing a Trainium2 kernel written in Bass

```python
import jax, jax.numpy as jnp
from jax import lax
import numpy as np

D_MODEL = 4096
BATCH = 1
SEQ = 16384
DEPTH = 1
DEC_BATCH = 4
DEC_SEQ = 2048
PAST_LEN = 128

GRID_W = 64
MLSTM_WIDTH = D_MODEL // 2
MLSTM_HEADS = 8
MLSTM_HEAD_DIM = MLSTM_WIDTH // MLSTM_HEADS
MLSTM_CHUNK = 64
NA_WIDTH = D_MODEL - MLSTM_WIDTH
NA_HEADS = 16
NA_HEAD_DIM = NA_WIDTH // NA_HEADS
NA_WIN_ROWS = 8
NA_WIN_COLS = 16
N_GATES = 4 * MLSTM_HEADS
PROJ_WIDTH = 4 * MLSTM_WIDTH + N_GATES + 3 * NA_WIDTH
PEER_HEADS = 8
PEER_QUERY_DIM = 256
PEER_HALF = PEER_QUERY_DIM // 2
PEER_N_KEYS = 128
PEER_N_EXPERTS = PEER_N_KEYS * PEER_N_KEYS
PEER_TOPK = 16
PEER_TOKEN_BLOCK = 64
EPS = 1e-6

kernel_name = 'hymba_mlstm_natten_peer_encoder'


def _rmsnorm(x, g):
    xf = x.astype(jnp.float32)
    y = xf * lax.rsqrt(jnp.mean(xf * xf, axis=-1, keepdims=True) + EPS)
    return (y * g.astype(jnp.float32)).astype(x.dtype)


def _mlstm_direction(q, k, v, logi, logf):
    B, H, L, dh = q.shape
    nc = L // MLSTM_CHUNK

    def chunks(a):
        return jnp.moveaxis(a.reshape((B, H, nc, MLSTM_CHUNK) + a.shape[3:]), 2, 0)

    tril = jnp.tril(jnp.ones((MLSTM_CHUNK, MLSTM_CHUNK), dtype=bool))

    def step(carry, inp):
        C, n, m = carry
        qc, kc, vc, ic, fc = inp
        b = jnp.cumsum(fc, axis=-1)
        d = b[..., :, None] - b[..., None, :] + ic[..., None, :]
        d = jnp.where(tril, d, -jnp.inf)
        inter = b + m[..., None]
        m_t = jnp.maximum(inter, jnp.max(d, axis=-1))
        w_inter = jnp.exp(inter - m_t)
        p = jnp.exp(d - m_t[..., None]) * jnp.einsum('bhtd,bhsd->bhts', qc, kc)
        num = w_inter[..., None] * jnp.einsum('bhtd,bhde->bhte', qc, C) + jnp.einsum('bhts,bhse->bhte', p, vc)
        den = w_inter * jnp.einsum('bhtd,bhd->bht', qc, n) + jnp.sum(p, axis=-1)
        h = num / jnp.maximum(jnp.abs(den), jnp.exp(-m_t))[..., None]
        b_last = b[..., -1]
        dec = b_last[..., None] - b + ic
        m_new = jnp.maximum(b_last + m, jnp.max(dec, axis=-1))
        w_old = jnp.exp(b_last + m - m_new)
        w_s = jnp.exp(dec - m_new[..., None])
        C = w_old[..., None, None] * C + jnp.einsum('bhs,bhsd,bhse->bhde', w_s, kc, vc)
        n = w_old[..., None] * n + jnp.einsum('bhs,bhsd->bhd', w_s, kc)
        return (C, n, m_new), h

    init = (jnp.zeros((B, H, dh, dh), jnp.float32),
            jnp.zeros((B, H, dh), jnp.float32),
            jnp.zeros((B, H), jnp.float32))
    _, hs = lax.scan(step, init, (chunks(q), chunks(k), chunks(v), chunks(logi), chunks(logf)))
    return jnp.moveaxis(hs, 0, 2).reshape(B, H, L, dh)


def _neighbourhood_attention(q, k, v, rpb):
    B, L, H, dh = q.shape
    rows = L // GRID_W
    wr = min(NA_WIN_ROWS, rows)
    qg = q.reshape(B, rows, GRID_W, H, dh)
    kg = k.reshape(B, rows, GRID_W, H, dh)
    vg = v.reshape(B, rows, GRID_W, H, dh)
    r = jnp.arange(rows)
    rs = jnp.clip(r - wr // 2, 0, rows - wr)
    key_rows = rs[:, None] + jnp.arange(wr)[None, :]
    kb = kg[:, key_rows]
    vb = vg[:, key_rows]
    s = jnp.einsum('brqhd,brjkhd->bhrqjk', qg, kb).astype(jnp.float32) * (dh ** -0.5)
    c = jnp.arange(GRID_W)
    cs = jnp.clip(c - NA_WIN_COLS // 2, 0, GRID_W - NA_WIN_COLS)
    col_ok = (c[None, :] >= cs[:, None]) & (c[None, :] < cs[:, None] + NA_WIN_COLS)
    dr = key_rows - r[:, None] + (NA_WIN_ROWS - 1)
    dc = jnp.clip(c[None, :] - c[:, None], -(NA_WIN_COLS - 1), NA_WIN_COLS - 1) + (NA_WIN_COLS - 1)
    bias = rpb.astype(jnp.float32)[:, dr[:, None, :, None], dc[None, :, None, :]]
    s = jnp.where(col_ok[None, None, None, :, None, :], s + bias[None], -jnp.inf)
    p = jax.nn.softmax(s.reshape(B, H, rows, GRID_W, wr * GRID_W), axis=-1)
    p = p.reshape(B, H, rows, GRID_W, wr, GRID_W).astype(v.dtype)
    o = jnp.einsum('bhrqjk,brjkhd->brqhd', p, vb)
    return o.reshape(B, L, H * dh)


def _mixer(h, w_in, gate_b, mlstm_norm_g, na_rpb, w_out):
    B, L, _ = h.shape
    proj = h @ w_in
    mw, aw = MLSTM_WIDTH, NA_WIDTH
    splits = [mw, 2 * mw, 3 * mw, 4 * mw, 4 * mw + N_GATES,
              4 * mw + N_GATES + aw, 4 * mw + N_GATES + 2 * aw]
    mq, mk, mv, mo, gates, nq, nk, nv = jnp.split(proj, splits, axis=-1)

    def heads(a):
        return a.reshape(B, L, MLSTM_HEADS, MLSTM_HEAD_DIM).transpose(0, 2, 1, 3).astype(jnp.float32)

    q = heads(mq)
    k = heads(mk) * (MLSTM_HEAD_DIM ** -0.5)
    v = heads(mv)
    g = (gates.astype(jnp.float32) + gate_b.astype(jnp.float32)).reshape(B, L, 4, MLSTM_HEADS)
    g = g.transpose(2, 0, 3, 1)
    h_fwd = _mlstm_direction(q, k, v, g[0], jax.nn.log_sigmoid(g[1]))
    fl = lambda a: jnp.flip(a, axis=2)
    h_bwd = fl(_mlstm_direction(fl(q), fl(k), fl(v), fl(g[2]), fl(jax.nn.log_sigmoid(g[3]))))
    hm = h_fwd + h_bwd
    hm = hm * lax.rsqrt(jnp.mean(hm * hm, axis=-1, keepdims=True) + EPS)
    hm = hm.transpose(0, 2, 1, 3).reshape(B, L, MLSTM_WIDTH) * mlstm_norm_g.astype(jnp.float32)
    hm = (hm * jax.nn.sigmoid(mo.astype(jnp.float32))).astype(h.dtype)

    hn = _neighbourhood_attention(nq.reshape(B, L, NA_HEADS, NA_HEAD_DIM),
                                  nk.reshape(B, L, NA_HEADS, NA_HEAD_DIM),
                                  nv.reshape(B, L, NA_HEADS, NA_HEAD_DIM), na_rpb)
    return jnp.concatenate([hm, hn], axis=-1) @ w_out


def _peer(h, w_q, sub_k1, sub_k2, u, v):
    B, L, D = h.shape
    q = (h @ w_q).astype(jnp.float32).reshape(B, L, PEER_HEADS, 2, PEER_HALF)
    s1 = jnp.einsum('blhd,hnd->blhn', q[..., 0, :], sub_k1.astype(jnp.float32))
    s2 = jnp.einsum('blhd,hnd->blhn', q[..., 1, :], sub_k2.astype(jnp.float32))
    v1, i1 = lax.top_k(s1, PEER_TOPK)
    v2, i2 = lax.top_k(s2, PEER_TOPK)
    cand = (v1[..., :, None] + v2[..., None, :]).reshape(B, L, PEER_HEADS, PEER_TOPK * PEER_TOPK)
    sc, ci = lax.top_k(cand, PEER_TOPK)
    e1 = jnp.take_along_axis(i1, ci // PEER_TOPK, axis=-1)
    e2 = jnp.take_along_axis(i2, ci % PEER_TOPK, axis=-1)
    expert = e1 * PEER_N_KEYS + e2
    gw = jax.nn.softmax(sc, axis=-1)
    nblk = (B * L) // PEER_TOKEN_BLOCK
    hb = h.reshape(nblk, PEER_TOKEN_BLOCK, D)
    eb = expert.reshape(nblk, PEER_TOKEN_BLOCK, PEER_HEADS * PEER_TOPK)
    gb = gw.astype(h.dtype).reshape(nblk, PEER_TOKEN_BLOCK, PEER_HEADS * PEER_TOPK)

    def block(args):
        xt, et, gt = args
        act = jax.nn.gelu(jnp.einsum('td,tkd->tk', xt, u[et]))
        return jnp.einsum('tk,tkd->td', gt * act, v[et])

    out = lax.map(block, (hb, eb, gb))
    return out.reshape(B, L, D)


def _layer(x, c, ada_w, ada_b, norm1_g, w_in, gate_b, mlstm_norm_g, na_rpb, w_out,
           norm2_g, peer_wq, peer_k1, peer_k2, peer_u, peer_v):
    mod = jax.nn.silu(c) @ ada_w + ada_b
    sh1, sc1, g1, sh2, sc2, g2 = jnp.split(mod[:, None, :], 6, axis=-1)
    h = _rmsnorm(x, norm1_g) * (1 + sc1) + sh1
    x = x + g1 * _mixer(h, w_in, gate_b, mlstm_norm_g, na_rpb, w_out)
    h = _rmsnorm(x, norm2_g) * (1 + sc2) + sh2
    x = x + g2 * _peer(h, peer_wq, peer_k1, peer_k2, peer_u, peer_v)
    return x


def _trunk(x, c, ada_w, ada_b, norm1_g, w_in, gate_b, mlstm_norm_g, na_rpb, w_out,
           norm2_g, peer_wq, peer_k1, peer_k2, peer_u, peer_v, final_g):
    for layer in range(DEPTH):
        x = _layer(x, c, ada_w[layer], ada_b[layer], norm1_g[layer], w_in[layer], gate_b[layer],
                   mlstm_norm_g[layer], na_rpb[layer], w_out[layer], norm2_g[layer],
                   peer_wq[layer], peer_k1[layer], peer_k2[layer], peer_u[layer], peer_v[layer])
    return _rmsnorm(x, final_g)


def setup_inputs(seed: int = 0) -> dict:
    key = jax.random.key(seed)
    ks = jax.random.split(key, 24)
    D = D_MODEL
    nrm = jax.random.normal
    x_prompt = nrm(ks[0], (BATCH, SEQ, D), jnp.float32)
    x_sample = nrm(ks[1], (DEC_BATCH, DEC_SEQ, D), jnp.float32)
    c_prompt = nrm(ks[2], (BATCH, D), jnp.float32)
    c_sample = nrm(ks[3], (DEC_BATCH, D), jnp.float32)
    ada_w = nrm(ks[4], (DEPTH, D, 6 * D), jnp.float32) * (0.5 * D ** -0.5)
    ada_b = nrm(ks[5], (DEPTH, 6 * D), jnp.float32) * 0.02
    norm1_g = 1.0 + 0.01 * nrm(ks[6], (DEPTH, D), jnp.float32)
    w_in = nrm(ks[7], (DEPTH, D, PROJ_WIDTH), jnp.float32) * (D ** -0.5)
    i_bias = 0.1 * nrm(ks[8], (DEPTH, 2, 1, MLSTM_HEADS), jnp.float32)
    f_bias = jnp.linspace(3.0, 6.0, MLSTM_HEADS, dtype=jnp.float32) + 0.1 * nrm(ks[9], (DEPTH, 2, 1, MLSTM_HEADS), jnp.float32)
    gate_b = jnp.concatenate([i_bias, f_bias], axis=2).reshape(DEPTH, N_GATES)
    mlstm_norm_g = 1.0 + 0.01 * nrm(ks[10], (DEPTH, MLSTM_WIDTH), jnp.float32)
    na_rpb = 0.2 * nrm(ks[11], (DEPTH, NA_HEADS, 2 * NA_WIN_ROWS - 1, 2 * NA_WIN_COLS - 1), jnp.float32)
    w_out = nrm(ks[12], (DEPTH, D, D), jnp.float32) * (D ** -0.5)
    norm2_g = 1.0 + 0.01 * nrm(ks[13], (DEPTH, D), jnp.float32)
    peer_wq = nrm(ks[14], (DEPTH, D, PEER_HEADS * PEER_QUERY_DIM), jnp.float32) * (D ** -0.5)
    peer_k1 = nrm(ks[15], (DEPTH, PEER_HEADS, PEER_N_KEYS, PEER_HALF), jnp.float32) * (PEER_HALF ** -0.5)
    peer_k2 = nrm(ks[16], (DEPTH, PEER_HEADS, PEER_N_KEYS, PEER_HALF), jnp.float32) * (PEER_HALF ** -0.5)
    peer_u = nrm(ks[17], (DEPTH, PEER_N_EXPERTS, D), jnp.float32) * (D ** -0.5)
    peer_v = nrm(ks[18], (DEPTH, PEER_N_EXPERTS, D), jnp.float32) * 0.5
    final_g = 1.0 + 0.01 * nrm(ks[19], (D,), jnp.float32)
    return {'x_prompt': x_prompt, 'x_sample': x_sample, 'c_prompt': c_prompt, 'c_sample': c_sample,
            'ada_w': ada_w, 'ada_b': ada_b, 'norm1_g': norm1_g, 'w_in': w_in, 'gate_b': gate_b,
            'mlstm_norm_g': mlstm_norm_g, 'na_rpb': na_rpb, 'w_out': w_out, 'norm2_g': norm2_g,
            'peer_wq': peer_wq, 'peer_k1': peer_k1, 'peer_k2': peer_k2, 'peer_u': peer_u,
            'peer_v': peer_v, 'final_g': final_g}


def reference(x_prompt, x_sample, c_prompt, c_sample, ada_w, ada_b, norm1_g, w_in, gate_b,
              mlstm_norm_g, na_rpb, w_out, norm2_g, peer_wq, peer_k1, peer_k2, peer_u, peer_v,
              final_g):
    y_prompt = _trunk(x_prompt, c_prompt, ada_w, ada_b, norm1_g, w_in, gate_b, mlstm_norm_g,
                      na_rpb, w_out, norm2_g, peer_wq, peer_k1, peer_k2, peer_u, peer_v, final_g)
    y_sample = _trunk(x_sample, c_sample, ada_w, ada_b, norm1_g, w_in, gate_b, mlstm_norm_g,
                      na_rpb, w_out, norm2_g, peer_wq, peer_k1, peer_k2, peer_u, peer_v, final_g)
    return (y_prompt, y_sample)
```

```python
import numpy as np
from contextlib import ExitStack
import ml_dtypes
import concourse.bass as bass
import concourse.mybir as mybir
from concourse.bass_utils import run_bass_kernel_spmd

F32 = mybir.dt.float32
BF16 = mybir.dt.bfloat16
I32 = mybir.dt.int32
U32 = mybir.dt.uint32
AF = mybir.ActivationFunctionType
ALU = mybir.AluOpType
AX = mybir.AxisListType
EPS = 1e-6
NEG = -30000.0

CFG = dict(D=4096, SEQ=16384, DB=4, DS=2048, NKEYS=128, NC=8)


class Res:
    __slots__ = ("name", "lw", "rd", "sem", "cnt")

    def __init__(self, name):
        self.name = name
        self.lw = None
        self.rd = {}
        self.sem = None
        self.cnt = 0


class Prog:
    ENG = ("pe", "act", "dve", "pool", "sp")

    def __init__(self, nc, stack):
        self.nc = nc
        self.stack = stack
        self.sems = {}
        self.lists = {e: [] for e in self.ENG}
        self.seq = {e: 0 for e in self.ENG}
        self.seen = {e: {} for e in self.ENG}
        self.final = {}
        for e in self.ENG:
            self._sem("E_" + e)
        self.ninst = 0
        self.dsems = []
        self.dsem_i = 0

    def _sem(self, key):
        if key not in self.sems:
            self.sems[key] = self.stack.enter_context(self.nc.semaphore(key))
            self.final[key] = 0
        return self.sems[key]

    def _deps(self, e, reads, writes):
        evs = {}

        def add(ev):
            if ev is None:
                return
            k, v = ev
            if evs.get(k, 0) < v:
                evs[k] = v
        for r in reads:
            add(r.lw)
        for w in writes:
            add(w.lw)
            for k, v in w.rd.items():
                add((k, v))
        out = []
        for k, v in evs.items():
            if e == "pe" and k == "E_pe":
                continue
            if self.seen[e].get(k, 0) >= v:
                continue
            self.seen[e][k] = v
            out.append((k, v))
        return out

    def _mark(self, ev, reads, writes):
        k, v = ev
        for r in reads:
            if r.rd.get(k, 0) < v:
                r.rd[k] = v
        for w in writes:
            w.lw = ev
            w.rd = {}

    def op(self, e, fn, reads=(), writes=()):
        for k, v in self._deps(e, reads, writes):
            self.lists[e].append(("w", k, v))
        self.seq[e] += 1
        ev = ("E_" + e, self.seq[e])
        self.final[ev[0]] = ev[1]
        self.lists[e].append(("i", fn, ev[0], 1))
        self._mark(ev, reads, writes)
        self.ninst += 1

    def dma(self, e, fn, reads=(), writes=(), dst=None):
        if dst is None:
            dst = writes[0]
        if dst.sem is None:
            dst.sem = "D_" + dst.name
            self._sem(dst.sem)
        for k, v in self._deps(e, reads, writes):
            self.lists[e].append(("w", k, v))
        dst.cnt += 16
        ev = (dst.sem, dst.cnt)
        self.final[dst.sem] = dst.cnt
        self.lists[e].append(("i", fn, dst.sem, 16))
        self._mark(ev, reads, writes)
        self.ninst += 1

    def barrier(self):
        for e in self.ENG:
            for k, v in self.final.items():
                if v > 0 and self.seen[e].get(k, 0) < v and not (k == "E_" + e):
                    self.seen[e][k] = v
                    self.lists[e].append(("w", k, v))

    def flush(self):
        nc = self.nc
        lists = self.lists
        sems = self.sems

        def replay(eng, lst):
            for it in lst:
                if it[0] == "w":
                    eng.wait_ge(sems[it[1]], it[2])
                else:
                    it[1](eng).then_inc(sems[it[2]], it[3])

        with nc.Block() as block:
            @block.tensor
            def _(eng):
                replay(eng, lists["pe"])

            @block.scalar
            def _(eng):
                replay(eng, lists["act"])

            @block.vector
            def _(eng):
                replay(eng, lists["dve"])

            @block.gpsimd
            def _(eng):
                replay(eng, lists["pool"])

            @block.sync
            def _(eng):
                replay(eng, lists["sp"])
        self.lists = {e: [] for e in self.ENG}


class Ctx:
    _n = [0]

    def __init__(self, nc, st, P):
        self.nc, self.st, self.P = nc, st, P
        Ctx._n[0] += 1
        self.pre = "c%d_" % Ctx._n[0]

    def sb(self, name, shape, dt):
        t = self.st.enter_context(self.nc.sbuf_tensor(self.pre + name, shape, dt))
        return t, Res(self.pre + name)

    def ps(self, name, shape, dt):
        t = self.st.enter_context(self.nc.psum_tensor(self.pre + name, shape, dt))
        return t, Res(self.pre + name)


def make_ident(P, C, n=128):
    idf, r_idf = C.sb("identf", [128, 128], F32)
    idb, r_idb = C.sb("identb", [128, 128], BF16)
    P.op("pool", lambda e: e.memset(idf[:], 0.0), writes=[r_idf])
    P.op("pool", lambda e: e.affine_select(out=idf[:], in_=idf[:], pattern=[[-1, 128]], compare_op=ALU.not_equal,
                                           fill=1.0, base=0, channel_multiplier=1), reads=[r_idf], writes=[r_idf])
    P.op("dve", lambda e: e.tensor_copy(out=idb[:], in_=idf[:]), reads=[r_idf], writes=[r_idb])
    return idf, r_idf, idb, r_idb


def build_l0(cfg):
    D = cfg["D"]
    KD = D // 128
    NS = 1 + cfg["DB"]
    NCOL = 6 * D // cfg["NC"]
    nc = bass.Bass("TRN2", target_bir_lowering=False)
    cT = nc.dram_tensor("cT", [128, KD, NS], F32, kind="ExternalInput")
    w = nc.dram_tensor("w", [D, NCOL], F32, kind="ExternalInput")
    b = nc.dram_tensor("b", [1, NCOL], F32, kind="ExternalInput")
    y = nc.dram_tensor("y", [NS, NCOL], F32, kind="ExternalOutput")
    r_y = Res("y")
    wv = w[:, :].rearrange("(k p) n -> p k n", p=128)
    with ExitStack() as st:
        P = Prog(nc, st)
        C = Ctx(nc, st, P)
        ct, r_ct = C.sb("ct", [128, KD, NS], F32)
        sg, r_sg = C.sb("sg", [128, KD, NS], F32)
        bt, r_bt = C.sb("bt", [NS, NCOL], F32)
        ot, r_ot = C.sb("ot", [NS, NCOL], F32)
        wt = [C.sb("wt%d" % i, [128, KD, 512], F32) for i in range(2)]
        pm = [C.ps("pm%d" % i, [128, 512], F32) for i in range(2)]
        P.dma("sp", lambda e: e.dma_start(out=ct[:], in_=cT[:, :, :]), writes=[r_ct])
        P.dma("sp", lambda e: e.dma_start(out=bt[:], in_=b[:, :].partition_broadcast(NS)), writes=[r_bt])
        P.op("act", lambda e: e.activation(out=sg[:], in_=ct[:], func=AF.Silu), reads=[r_ct], writes=[r_sg])
        nb = NCOL // 512
        for j in range(nb):
            wtj, r_w = wt[j % 2]
            pmj, r_p = pm[j % 2]
            for h in range(4):
                P.dma("sp" if h % 2 == 0 else "act", lambda e, wtj=wtj, j=j, h=h: e.dma_start(
                    out=wtj[:, h * (KD // 4):(h + 1) * (KD // 4), :], in_=wv[:, h * (KD // 4):(h + 1) * (KD // 4), j * 512:(j + 1) * 512]), writes=[r_w])
            for k in range(KD):
                P.op("pe", lambda e, wtj=wtj, pmj=pmj, k=k: e.matmul(pmj[0:NS, :], lhsT=sg[:, k, :], rhs=wtj[:, k, :],
                                                                     start=(k == 0), stop=(k == KD - 1)), reads=[r_sg, r_w], writes=[r_p])
            P.op("dve", lambda e, pmj=pmj, j=j: e.tensor_tensor(out=ot[:, j * 512:(j + 1) * 512], in0=pmj[0:NS, :], in1=bt[:, j * 512:(j + 1) * 512], op=ALU.add),
                 reads=[r_p, r_bt], writes=[r_ot])
        P.dma("sp", lambda e: e.dma_start(out=y[:, :], in_=ot[:]), reads=[r_ot], writes=[r_y])
        P.barrier()
        P.flush()
    return nc


WC = 2052


def seq_list(cfg):
    s = [(0, cfg["SEQ"])]
    for b in range(cfg["DB"]):
        s.append((cfg["SEQ"] + b * cfg["DS"], cfg["DS"]))
    return s


def build_l1(cfg, phases="ABCD"):
    D = cfg["D"]
    KD = D // 128
    NS = 1 + cfg["DB"]
    NT = cfg["SEQ"] + cfg["DB"] * cfg["DS"]
    seqs = seq_list(cfg)
    nc = bass.Bass("TRN2", target_bir_lowering=False)
    x = nc.dram_tensor("x", [NT, D], F32, kind="ExternalInput")
    sc1T = nc.dram_tensor("sc1T", [128, KD, NS], F32, kind="ExternalInput")
    sh1T = nc.dram_tensor("sh1T", [128, KD, NS], F32, kind="ExternalInput")
    n1g = nc.dram_tensor("n1g", [128, KD], F32, kind="ExternalInput")
    w = nc.dram_tensor("w", [D, WC], F32, kind="ExternalInput")
    gb = nc.dram_tensor("gb", [1, 4], F32, kind="ExternalInput")
    mng = nc.dram_tensor("mng", [1, 256], F32, kind="ExternalInput")
    zall = nc.dram_tensor("zall", [30, 127], F32, kind="ExternalInput")
    nmask = nc.dram_tensor("nmask", [64, 64], F32, kind="ExternalInput")
    mix = nc.dram_tensor("mix", [NT, 512], BF16, kind="ExternalOutput")
    dbg = "E" in phases
    okind = "ExternalOutput" if dbg else "Internal"
    wb = nc.dram_tensor("wb", [D, WC], BF16, kind="Internal")
    fm = nc.dram_tensor("fm", [8, 128, NT], BF16, kind=okind)
    kv = nc.dram_tensor("kv", [NT, 512], BF16, kind=okind)
    mo = nc.dram_tensor("mo", [NT, 256], F32, kind=okind)
    nv = nc.dram_tensor("nv", [NT, 256], BF16, kind=okind)
    gt = nc.dram_tensor("gt", [NT, 4], F32, kind=okind)
    hf = nc.dram_tensor("hf", [NT, 256], F32, kind="Internal")
    r_mix, r_wb, r_fm, r_kv, r_mo, r_nv, r_gt, r_hf = [Res(n) for n in ("mix", "wb", "fm", "kv", "mo", "nv", "gt", "hf")]
    wv = w[:, :].rearrange("(k p) n -> p k n", p=128)
    wbv = wb[:, :].rearrange("(k p) n -> p k n", p=128)

    with ExitStack() as st0:
        P = Prog(nc, st0)
        if "A" in phases:
            with ExitStack() as st:
                C = Ctx(nc, st, P)
                wf = [C.sb("wf%d" % i, [128, WC], F32) for i in range(2)]
                wo = [C.sb("wo%d" % i, [128, WC], BF16) for i in range(2)]
                for k in range(KD):
                    a, r_a = wf[k % 2]
                    o, r_o = wo[k % 2]
                    P.dma("sp", lambda e, a=a, k=k: e.dma_start(out=a[:], in_=w[k * 128:(k + 1) * 128, :]), writes=[r_a])
                    P.op("act" if k % 2 else "dve", (lambda e, a=a, o=o: e.activation(out=o[:], in_=a[:], func=AF.Copy)) if k % 2 else
                         (lambda e, a=a, o=o: e.tensor_copy(out=o[:], in_=a[:])), reads=[r_a], writes=[r_o])
                    P.dma("sp", lambda e, o=o, k=k: e.dma_start(out=wb[k * 128:(k + 1) * 128, :], in_=o[:]), reads=[r_o], writes=[r_wb])
                P.barrier()
                P.flush()
        if "B" in phases:
            with ExitStack() as st:
                C = Ctx(nc, st, P)
                idf, r_idf, idb, r_idb = make_ident(P, C)
                sct, r_sct = C.sb("sct", [128, KD, NS], F32)
                sht, r_sht = C.sb("sht", [128, KD, NS], F32)
                ngt, r_ngt = C.sb("ngt", [128, KD], F32)
                gbt, r_gbt = C.sb("gbt", [128, 4], F32)
                P.dma("sp", lambda e: e.dma_start(out=sct[:], in_=sc1T[:, :, :]), writes=[r_sct])
                P.dma("sp", lambda e: e.dma_start(out=sht[:], in_=sh1T[:, :, :]), writes=[r_sht])
                P.dma("sp", lambda e: e.dma_start(out=ngt[:], in_=n1g[:, :]), writes=[r_ngt])
                P.dma("sp", lambda e: e.dma_start(out=gbt[:], in_=gb[:, :].partition_broadcast(128)), writes=[r_gbt])
                P.op("dve", lambda e: e.tensor_scalar(out=sct[:], in0=sct[:], scalar1=1.0, scalar2=None, op0=ALU.add), reads=[r_sct], writes=[r_sct])
                for s in range(NS):
                    P.op("dve", lambda e, s=s: e.tensor_tensor(out=sct[:, :, s], in0=sct[:, :, s], in1=ngt[:], op=ALU.mult), reads=[r_sct, r_ngt], writes=[r_sct])
                xt = [C.sb("xt%d" % i, [128, D], F32) for i in range(2)]
                xn, r_xn = C.sb("xn", [128, D], BF16)
                jk, r_jk = C.sb("jk", [128, D], BF16)
                ssq, r_ssq = C.sb("ssq", [128, 1], F32)
                rstd, r_rstd = C.sb("rstd", [128, 1], F32)
                hT = [C.sb("hT%d" % i, [128, KD, 512], BF16) for i in range(2)]
                wfm = [C.sb("wfm%d" % i, [128, KD, 128], BF16) for i in range(2)]
                wtm = [C.sb("wtm%d" % i, [128, KD, 512], BF16) for i in range(2)]
                wg, r_wg = C.sb("wg", [128, KD, 4], BF16)
                ofm = [C.sb("ofm%d" % i, [128, 512], BF16) for i in range(2)]
                okv = [C.sb("okv%d" % i, [128, 512], BF16) for i in range(2)]
                omo = [C.sb("omo%d" % i, [128, 512], F32) for i in range(2)]
                og = [C.sb("og%d" % i, [128, 4], F32) for i in range(2)]
                pT = [C.ps("pT%d" % i, [128, 1024], BF16) for i in range(2)]
                pM = [C.ps("pM%d" % i, [128, 512], F32) for i in range(4)]
                P.dma("sp", lambda e: e.dma_start(out=wg[:], in_=wbv[:, :, 2048:2052]), reads=[r_wb], writes=[r_wg])
                ngrp = NT // 512
                cnt = dict(t=0, p=0, fm=0, tm=0, o=0)
                for g in range(ngrp):
                    hTg, r_hT = hT[g % 2]
                    for ti in range(4):
                        tok0 = g * 512 + ti * 128
                        s = [i for i, (a, l) in enumerate(seqs) if a <= tok0 < a + l][0]
                        xtt, r_xt = xt[cnt["t"] % 2]
                        cnt["t"] += 1
                        for h in range(4):
                            P.dma("sp", lambda e, xtt=xtt, tok0=tok0, h=h: e.dma_start(out=xtt[:, h * (D // 4):(h + 1) * (D // 4)], in_=x[tok0:tok0 + 128, h * (D // 4):(h + 1) * (D // 4)]), writes=[r_xt])
                        P.op("act", lambda e, xtt=xtt: e.activation(out=jk[:], in_=xtt[:], func=AF.Square, accum_out=ssq[:]), reads=[r_xt], writes=[r_jk, r_ssq])
                        P.op("dve", lambda e: e.tensor_scalar(out=rstd[:], in0=ssq[:], scalar1=1.0 / D, scalar2=EPS, op0=ALU.mult, op1=ALU.add), reads=[r_ssq], writes=[r_rstd])
                        P.op("act", lambda e: e.activation(out=rstd[:], in_=rstd[:], func=AF.Sqrt), reads=[r_rstd], writes=[r_rstd])
                        P.op("dve", lambda e: e.reciprocal(out=rstd[:], in_=rstd[:]), reads=[r_rstd], writes=[r_rstd])
                        P.op("act", lambda e, xtt=xtt: e.activation(out=xn[:], in_=xtt[:], func=AF.Copy, scale=rstd[:]), reads=[r_xt, r_rstd], writes=[r_xn])
                        for q in range(KD // 8):
                            pTt, r_pT = pT[cnt["p"] % 2]
                            cnt["p"] += 1
                            for j in range(8):
                                k = q * 8 + j
                                P.op("pe", lambda e, pTt=pTt, j=j, k=k: e.transpose(out=pTt[:, j * 128:(j + 1) * 128], in_=xn[:, k * 128:(k + 1) * 128], identity=idb[:]),
                                     reads=[r_xn, r_idb], writes=[r_pT])
                            for j in range(8):
                                k = q * 8 + j
                                if j % 2 == 0:
                                    P.op("act", lambda e, pTt=pTt, j=j, k=k, s=s, hTg=hTg, ti=ti: e.activation(out=hTg[:, k, ti * 128:(ti + 1) * 128], in_=pTt[:, j * 128:(j + 1) * 128],
                                                                                                          func=AF.Identity, scale=sct[:, k, s:s + 1], bias=sht[:, k, s:s + 1]),
                                         reads=[r_pT, r_sct, r_sht], writes=[r_hT])
                                else:
                                    P.op("dve", lambda e, pTt=pTt, j=j, k=k, s=s, hTg=hTg, ti=ti: e.tensor_scalar(out=hTg[:, k, ti * 128:(ti + 1) * 128], in0=pTt[:, j * 128:(j + 1) * 128],
                                                                                                             scalar1=sct[:, k, s:s + 1], scalar2=sht[:, k, s:s + 1], op0=ALU.mult, op1=ALU.add),
                                         reads=[r_pT, r_sct, r_sht], writes=[r_hT])
                    if cfg.get('lim', 9) < 2:
                        continue
                    for cb in range(8):
                        wt_, r_w = wfm[cnt["fm"] % 2]
                        cnt["fm"] += 1
                        P.dma("sp", lambda e, wt_=wt_, cb=cb: e.dma_start(out=wt_[:], in_=wbv[:, :, cb * 128:(cb + 1) * 128]), reads=[r_wb], writes=[r_w])
                        pm_, r_p = pM[cnt["o"] % 4]
                        o_, r_o = ofm[cnt["o"] % 2]
                        cnt["o"] += 1
                        for k in range(KD):
                            P.op("pe", lambda e, pm_=pm_, wt_=wt_, hTg=hTg, k=k: e.matmul(pm_[:, :], lhsT=wt_[:, k, :], rhs=hTg[:, k, :], start=(k == 0), stop=(k == KD - 1)),
                                 reads=[r_w, r_hT], writes=[r_p])
                        P.op("act", lambda e, pm_=pm_, o_=o_: e.activation(out=o_[:], in_=pm_[:, :], func=AF.Copy), reads=[r_p], writes=[r_o])
                        P.dma("sp", lambda e, o_=o_, cb=cb, g=g: e.dma_start(out=fm[cb, :, g * 512:(g + 1) * 512], in_=o_[:]), reads=[r_o], writes=[r_fm])
                    if cfg.get('lim', 9) < 2.5:
                        continue
                    for blk in range(2 if cfg.get('lim', 9) != 2.5 else 1):
                        wt_, r_w = wtm[cnt["tm"] % 2]
                        cnt["tm"] += 1
                        for h in range(2):
                            P.dma("sp", lambda e, wt_=wt_, blk=blk, h=h: e.dma_start(out=wt_[:, h * (KD // 2):(h + 1) * (KD // 2), :], in_=wbv[:, h * (KD // 2):(h + 1) * (KD // 2), 1024 + blk * 512:1536 + blk * 512]), reads=[r_wb], writes=[r_w])
                        for ti in range(4):
                            tok0 = g * 512 + ti * 128
                            pm_, r_p = pM[cnt["o"] % 4]
                            o_, r_o = okv[cnt["o"] % 2]
                            o2_, r_o2 = omo[cnt["o"] % 2]
                            cnt["o"] += 1
                            for k in range(KD):
                                P.op("pe", lambda e, pm_=pm_, wt_=wt_, hTg=hTg, k=k, ti=ti: e.matmul(pm_[:, :], lhsT=hTg[:, k, ti * 128:(ti + 1) * 128], rhs=wt_[:, k, :], start=(k == 0), stop=(k == KD - 1)),
                                     reads=[r_w, r_hT], writes=[r_p])
                            if blk == 0:
                                P.op("dve", lambda e, pm_=pm_, o_=o_: e.tensor_copy(out=o_[:], in_=pm_[:, :]), reads=[r_p], writes=[r_o])
                                P.dma("sp", lambda e, o_=o_, tok0=tok0: e.dma_start(out=kv[tok0:tok0 + 128, :], in_=o_[:]), reads=[r_o], writes=[r_kv])
                            else:
                                P.op("act", lambda e, pm_=pm_, o2_=o2_: e.activation(out=o2_[:], in_=pm_[:, :], func=AF.Copy), reads=[r_p], writes=[r_o2])
                                P.op("dve", lambda e, o2_=o2_, o_=o_: e.tensor_copy(out=o_[:, 0:256], in_=o2_[:, 256:512]), reads=[r_o2], writes=[r_o])
                                P.dma("sp", lambda e, o2_=o2_, tok0=tok0: e.dma_start(out=mo[tok0:tok0 + 128, :], in_=o2_[:, 0:256]), reads=[r_o2], writes=[r_mo])
                                P.dma("sp", lambda e, o_=o_, tok0=tok0: e.dma_start(out=nv[tok0:tok0 + 128, :], in_=o_[:, 0:256]), reads=[r_o], writes=[r_nv])
                    if cfg.get('lim', 9) < 4:
                        continue
                    for ti in range(4):
                        tok0 = g * 512 + ti * 128
                        pm_, r_p = pM[cnt["o"] % 4]
                        o_, r_o = og[cnt["o"] % 2]
                        cnt["o"] += 1
                        for k in range(KD):
                            P.op("pe", lambda e, pm_=pm_, hTg=hTg, k=k, ti=ti: e.matmul(pm_[:, 0:4], lhsT=hTg[:, k, ti * 128:(ti + 1) * 128], rhs=wg[:, k, :], start=(k == 0), stop=(k == KD - 1)),
                                 reads=[r_wg, r_hT], writes=[r_p])
                        P.op("dve", lambda e, pm_=pm_, o_=o_: e.tensor_tensor(out=o_[:], in0=pm_[:, 0:4], in1=gbt[:], op=ALU.add), reads=[r_p, r_gbt], writes=[r_o])
                        P.dma("sp", lambda e, o_=o_, tok0=tok0: e.dma_start(out=gt[tok0:tok0 + 128, :], in_=o_[:]), reads=[r_o], writes=[r_gt])
                P.barrier()
                P.flush()
        if "C" in phases:
            gt2 = nc.dram_tensor("gt2", [NT, 4], F32, kind="Internal")
            r_gt2 = Res("gt2")
            with ExitStack() as st:
                C = Ctx(nc, st, P)
                idf, r_idf, idb, r_idb = make_ident(P, C)
                NB = NT // 128
                g_in, r_gin = C.sb("g_in", [128, NB, 4], F32)
                g_a, r_ga = C.sb("g_a", [128, NB, 4], F32)
                g_b, r_gb = C.sb("g_b", [128, NB, 4], F32)
                P.dma("sp", lambda e: e.dma_start(out=g_in[:], in_=gt[:, :].rearrange("(n p) c -> p n c", p=128)), reads=[r_gt], writes=[r_gin])
                P.op("act", lambda e: e.activation(out=g_a[:], in_=g_in[:], func=AF.Abs), reads=[r_gin], writes=[r_ga])
                P.op("act", lambda e: e.activation(out=g_a[:], in_=g_a[:], func=AF.Exp, scale=-1.0), reads=[r_ga], writes=[r_ga])
                P.op("act", lambda e: e.activation(out=g_a[:], in_=g_a[:], func=AF.Ln, bias=1.0), reads=[r_ga], writes=[r_ga])
                P.op("dve", lambda e: e.tensor_scalar(out=g_b[:], in0=g_in[:], scalar1=0.0, scalar2=None, op0=ALU.min), reads=[r_gin], writes=[r_gb])
                P.op("dve", lambda e: e.tensor_tensor(out=g_b[:], in0=g_b[:], in1=g_a[:], op=ALU.subtract), reads=[r_gb, r_ga], writes=[r_gb])
                for col in (0, 2):
                    P.op("dve", lambda e, col=col: e.tensor_copy(out=g_b[:, :, col:col + 1], in_=g_in[:, :, col:col + 1]), reads=[r_gin, r_gb], writes=[r_gb])
                P.dma("sp", lambda e: e.dma_start(out=gt2[:, :].rearrange("(n p) c -> p n c", p=128), in_=g_b[:]), reads=[r_gb], writes=[r_gt2])
                tri = {}
                mneg = {}
                sel = {}
                for dn in ("f", "b"):
                    t_, r_t = C.sb("tri" + dn, [64, 64], F32)
                    m_, r_m = C.sb("mneg" + dn, [64, 64], F32)
                    s_, r_s = C.sb("sel" + dn, [64, 128], F32)
                    sgn = 1 if dn == "f" else -1
                    P.op("pool", lambda e, t_=t_: e.memset(t_[:], 1.0), writes=[r_t])
                    P.op("pool", lambda e, t_=t_, sgn=sgn: e.affine_select(out=t_[:], in_=t_[:], pattern=[[sgn, 64]], compare_op=ALU.is_ge, fill=0.0, base=0, channel_multiplier=-sgn), reads=[r_t], writes=[r_t])
                    P.op("pool", lambda e, m_=m_: e.memset(m_[:], 0.0), writes=[r_m])
                    P.op("pool", lambda e, m_=m_, sgn=sgn: e.affine_select(out=m_[:], in_=m_[:], pattern=[[-sgn, 64]], compare_op=ALU.is_ge, fill=NEG, base=0, channel_multiplier=sgn), reads=[r_m], writes=[r_m])
                    last = 63 if dn == "f" else 0
                    P.op("pool", lambda e, s_=s_: e.memset(s_[:], 0.0), writes=[r_s])
                    P.op("pool", lambda e, s_=s_, last=last: e.affine_select(out=s_[:], in_=s_[:], pattern=[[0, 128]], compare_op=ALU.not_equal, fill=1.0, base=-last, channel_multiplier=1), reads=[r_s], writes=[r_s])
                    tri[dn], mneg[dn], sel[dn] = (t_, r_t), (m_, r_m), (s_, r_s)
                ones, r_ones = C.sb("ones64", [64, 64], F32)
                P.op("pool", lambda e: e.memset(ones[:], 1.0), writes=[r_ones])
                mngb, r_mngb = C.sb("mngb", [64, 256], F32)
                P.dma("sp", lambda e: e.dma_start(out=mngb[:], in_=mng[:, :].partition_broadcast(64)), writes=[r_mngb])
                Cs, r_Cs = C.sb("Cs", [128, 2, 257], F32)
                Cb, r_Cb = C.sb("Cb", [128, 2, 257], BF16)
                mcur, r_mcur = C.sb("mcur", [128, 1], F32)
                gch = [C.sb("gch%d" % i, [64, 4], F32) for i in range(2)]
                qTc = [C.sb("qTc%d" % i, [128, 2, 64], BF16) for i in range(2)]
                kTc = [C.sb("kTc%d" % i, [128, 2, 64], BF16) for i in range(2)]
                kvt = [C.sb("kvt%d" % i, [64, 512], BF16) for i in range(2)]
                hfc = [C.sb("hfc%d" % i, [64, 256], F32) for i in range(2)]
                moc = [C.sb("moc%d" % i, [64, 256], F32) for i in range(2)]
                sm = {n: C.sb("sm_" + n, [64, 1], F32) for n in ("a", "cmax", "ws", "ecl", "wint", "em", "ad", "rc", "ssq", "rstd", "wsk")}
                sc2, r_sc2 = C.sb("sc2", [64, 2], F32)
                ml2, r_ml2 = C.sb("ml2", [128, 2], F32)
                nml2, r_nml2 = C.sb("nml2", [128, 2], F32)
                wold, r_wold = C.sb("wold", [128, 1], F32)
                diagA, r_diagA = C.sb("diagA", [64, 64], F32)
                am, r_am = C.sb("am", [64, 64], F32)
                PT, r_PT = C.sb("PT", [64, 64], BF16)
                vs, r_vs = C.sb("vs", [64, 257], BF16)
                H1, r_H1 = C.sb("H1", [64, 257], F32)
                Hh, r_Hh = C.sb("Hh", [64, 257], F32)
                hout = [C.sb("hout%d" % i, [64, 256], F32) for i in range(2)]
                hs, r_hs = C.sb("hs", [64, 256], F32)
                jk2, r_jk2 = C.sb("jk2", [64, 256], F32)
                sgm, r_sgm = C.sb("sgm", [64, 256], F32)
                omx = [C.sb("omx%d" % i, [64, 256], BF16) for i in range(2)]
                psA, r_psA = C.ps("psA", [128, 512], F32)
                psS, r_psS = C.ps("psS", [128, 512], F32)
                psI, r_psI = C.ps("psI", [128, 512], F32)
                psQ, r_psQ = C.ps("psQ", [128, 512], F32)
                psU = [C.ps("psU%d" % i, [128, 512], F32) for i in range(2)]
                KS = 256 ** -0.5
                fmv = fm[:, :, :].rearrange("h p t -> p h t")
                it = 0
                for dn in ("f", "b"):
                    icol, fcol = (0, 1) if dn == "f" else (2, 3)
                    tri_, r_tri = tri[dn]
                    mneg_, r_mneg = mneg[dn]
                    sel_, r_sel = sel[dn]
                    for (s0, sl) in seqs:
                        P.op("pool", lambda e: e.memset(Cs[:], 0.0), writes=[r_Cs])
                        P.op("pool", lambda e: e.memset(Cb[:], 0.0), writes=[r_Cb])
                        P.op("pool", lambda e: e.memset(mcur[:], 0.0), writes=[r_mcur])
                        nch = sl // 64
                        order = range(nch) if dn == "f" else range(nch - 1, -1, -1)
                        for ch in order:
                            tok0 = s0 + ch * 64
                            g_, r_g = gch[it % 2]
                            q_, r_q = qTc[it % 2]
                            k_, r_k = kTc[it % 2]
                            kv_, r_kvt = kvt[it % 2]
                            ho_, r_ho = hout[it % 2]
                            hf_, r_hfc = hfc[it % 2]
                            mo_, r_moc = moc[it % 2]
                            ox_, r_ox = omx[it % 2]
                            it += 1
                            P.dma("sp", lambda e, g_=g_, tok0=tok0: e.dma_start(out=g_[:], in_=gt2[tok0:tok0 + 64, :]), reads=[r_gt2], writes=[r_g])
                            P.dma("sp", lambda e, q_=q_, tok0=tok0: e.dma_start(out=q_[:], in_=fmv[:, 0:2, tok0:tok0 + 64]), reads=[r_fm], writes=[r_q])
                            P.dma("sp", lambda e, k_=k_, tok0=tok0: e.dma_start(out=k_[:], in_=fmv[:, 2:4, tok0:tok0 + 64]), reads=[r_fm], writes=[r_k])
                            P.dma("sp", lambda e, kv_=kv_, tok0=tok0: e.dma_start(out=kv_[:], in_=kv[tok0:tok0 + 64, :]), reads=[r_kv], writes=[r_kvt])
                            if dn == "b":
                                P.dma("sp", lambda e, hf_=hf_, tok0=tok0: e.dma_start(out=hf_[:], in_=hf[tok0:tok0 + 64, :]), reads=[r_hf], writes=[r_hfc])
                                P.dma("sp", lambda e, mo_=mo_, tok0=tok0: e.dma_start(out=mo_[:], in_=mo[tok0:tok0 + 64, :]), reads=[r_mo], writes=[r_moc])
                            a_, r_a = sm["a"]
                            cm_, r_cm = sm["cmax"]
                            ws_, r_ws = sm["ws"]
                            ecl_, r_ecl = sm["ecl"]
                            wi_, r_wi = sm["wint"]
                            em_, r_em = sm["em"]
                            ad_, r_ad = sm["ad"]
                            rc_, r_rc = sm["rc"]
                            ssq_, r_ssq = sm["ssq"]
                            rs_, r_rs = sm["rstd"]
                            wsk_, r_wsk = sm["wsk"]
                            P.op("pe", lambda e, g_=g_, fcol=fcol, tri_=tri_: e.matmul(psA[0:64, 0:1], lhsT=tri_[:], rhs=g_[:, fcol:fcol + 1], start=True, stop=True), reads=[r_tri, r_g], writes=[r_psA])
                            P.op("dve", lambda e, g_=g_, icol=icol: e.tensor_tensor(out=a_[:], in0=g_[:, icol:icol + 1], in1=psA[0:64, 0:1], op=ALU.subtract), reads=[r_g, r_psA], writes=[r_a])
                            P.op("dve", lambda e: e.tensor_scalar(out=diagA[:], in0=idf[0:64, 0:64], scalar1=a_[:, 0:1], scalar2=None, op0=ALU.mult), reads=[r_idf, r_a], writes=[r_diagA])
                            P.op("pe", lambda e: e.matmul(psA[0:64, 64:128], lhsT=ones[:], rhs=diagA[:], start=True, stop=True), reads=[r_ones, r_diagA], writes=[r_psA])
                            P.op("dve", lambda e, mneg_=mneg_: e.tensor_tensor(out=am[:], in0=psA[0:64, 64:128], in1=mneg_[:], op=ALU.add), reads=[r_psA, r_mneg], writes=[r_am])
                            P.op("dve", lambda e: e.reduce_max(out=cm_[:], in_=am[:], axis=AX.X), reads=[r_am], writes=[r_cm])
                            P.op("dve", lambda e: e.tensor_tensor(out=sc2[:, 0:1], in0=cm_[:], in1=mcur[0:64, :], op=ALU.max), reads=[r_cm, r_mcur], writes=[r_sc2])
                            P.op("dve", lambda e: e.tensor_tensor(out=sc2[:, 1:2], in0=sc2[:, 0:1], in1=psA[0:64, 0:1], op=ALU.add), reads=[r_sc2, r_psA], writes=[r_sc2])
                            P.op("pe", lambda e, sel_=sel_: e.matmul(psA[:, 128:130], lhsT=sel_[:], rhs=sc2[:], start=True, stop=True), reads=[r_sel, r_sc2], writes=[r_psA])
                            P.op("dve", lambda e: e.tensor_copy(out=ml2[:], in_=psA[:, 128:130]), reads=[r_psA], writes=[r_ml2])
                            P.op("dve", lambda e: e.tensor_scalar(out=nml2[:], in0=ml2[:], scalar1=-1.0, scalar2=None, op0=ALU.mult), reads=[r_ml2], writes=[r_nml2])
                            P.op("act", lambda e: e.activation(out=ws_[:], in_=a_[:], func=AF.Exp, bias=nml2[0:64, 0:1]), reads=[r_a, r_nml2], writes=[r_ws])
                            P.op("act", lambda e: e.activation(out=ecl_[:], in_=sc2[:, 0:1], func=AF.Exp, scale=-1.0, bias=ml2[0:64, 0:1]), reads=[r_sc2, r_ml2], writes=[r_ecl])
                            P.op("act", lambda e: e.activation(out=wi_[:], in_=sc2[:, 0:1], func=AF.Exp, scale=-1.0, bias=mcur[0:64, :]), reads=[r_sc2, r_mcur], writes=[r_wi])
                            P.op("act", lambda e: e.activation(out=em_[:], in_=sc2[:, 1:2], func=AF.Exp, scale=-1.0), reads=[r_sc2], writes=[r_em])
                            P.op("act", lambda e: e.activation(out=wold[:], in_=mcur[:], func=AF.Exp, bias=nml2[:, 0:1]), reads=[r_mcur, r_nml2], writes=[r_wold])
                            P.op("dve", lambda e: e.tensor_scalar(out=wsk_[:], in0=ws_[:], scalar1=KS, scalar2=None, op0=ALU.mult), reads=[r_ws], writes=[r_wsk])
                            P.op("dve", lambda e, kv_=kv_: e.tensor_scalar(out=vs[:, 0:256], in0=kv_[:, 256:512], scalar1=wsk_[:, 0:1], scalar2=None, op0=ALU.mult), reads=[r_kvt, r_wsk], writes=[r_vs])
                            P.op("dve", lambda e: e.tensor_copy(out=vs[:, 256:257], in_=wsk_[:]), reads=[r_wsk, r_vs], writes=[r_vs])
                            for hh in range(2):
                                P.op("pe", lambda e, k_=k_, q_=q_, hh=hh: e.matmul(psS[0:64, 0:64], lhsT=k_[:, hh, :], rhs=q_[:, hh, :], start=(hh == 0), stop=(hh == 1)), reads=[r_k, r_q], writes=[r_psS])
                            P.op("dve", lambda e, tri_=tri_: e.tensor_tensor(out=PT[:], in0=psS[0:64, 0:64], in1=tri_[:], op=ALU.mult), reads=[r_psS, r_tri], writes=[r_PT])
                            P.op("pe", lambda e: e.matmul(psI[0:64, 0:257], lhsT=PT[:], rhs=vs[:], start=True, stop=True), reads=[r_PT, r_vs], writes=[r_psI])
                            for hh in range(2):
                                P.op("pe", lambda e, q_=q_, hh=hh: e.matmul(psQ[0:64, 0:257], lhsT=q_[:, hh, :], rhs=Cb[:, hh, :], start=(hh == 0), stop=(hh == 1)), reads=[r_q, r_Cb], writes=[r_psQ])
                            P.op("dve", lambda e: e.tensor_scalar(out=H1[:], in0=psQ[0:64, 0:257], scalar1=wi_[:, 0:1], scalar2=None, op0=ALU.mult), reads=[r_psQ, r_wi], writes=[r_H1])
                            P.op("dve", lambda e: e.scalar_tensor_tensor(out=Hh[:], in0=psI[0:64, 0:257], scalar=ecl_[:, 0:1], in1=H1[:], op0=ALU.mult, op1=ALU.add), reads=[r_psI, r_ecl, r_H1], writes=[r_Hh])
                            P.op("act", lambda e: e.activation(out=ad_[:], in_=Hh[:, 256:257], func=AF.Abs), reads=[r_Hh], writes=[r_ad])
                            P.op("dve", lambda e: e.tensor_tensor(out=ad_[:], in0=ad_[:], in1=em_[:], op=ALU.max), reads=[r_ad, r_em], writes=[r_ad])
                            P.op("dve", lambda e: e.reciprocal(out=rc_[:], in_=ad_[:]), reads=[r_ad], writes=[r_rc])
                            P.op("dve", lambda e, ho_=ho_: e.tensor_scalar(out=ho_[:], in0=Hh[:, 0:256], scalar1=rc_[:, 0:1], scalar2=None, op0=ALU.mult), reads=[r_Hh, r_rc], writes=[r_ho])
                            if dn == "f":
                                P.dma("sp", lambda e, ho_=ho_, tok0=tok0: e.dma_start(out=hf[tok0:tok0 + 64, :], in_=ho_[:]), reads=[r_ho], writes=[r_hf])
                            else:
                                P.op("dve", lambda e, ho_=ho_, hf_=hf_: e.tensor_tensor(out=hs[:], in0=ho_[:], in1=hf_[:], op=ALU.add), reads=[r_ho, r_hfc], writes=[r_hs])
                                P.op("act", lambda e: e.activation(out=jk2[:], in_=hs[:], func=AF.Square, accum_out=ssq_[:]), reads=[r_hs], writes=[r_jk2, r_ssq])
                                P.op("dve", lambda e: e.tensor_scalar(out=rs_[:], in0=ssq_[:], scalar1=1.0 / 256, scalar2=EPS, op0=ALU.mult, op1=ALU.add), reads=[r_ssq], writes=[r_rs])
                                P.op("act", lambda e: e.activation(out=rs_[:], in_=rs_[:], func=AF.Sqrt), reads=[r_rs], writes=[r_rs])
                                P.op("dve", lambda e: e.reciprocal(out=rs_[:], in_=rs_[:]), reads=[r_rs], writes=[r_rs])
                                P.op("act", lambda e, mo_=mo_: e.activation(out=sgm[:], in_=mo_[:], func=AF.Sigmoid), reads=[r_moc], writes=[r_sgm])
                                P.op("dve", lambda e: e.scalar_tensor_tensor(out=hs[:], in0=hs[:], scalar=rs_[:, 0:1], in1=mngb[:], op0=ALU.mult, op1=ALU.mult), reads=[r_hs, r_rs, r_mngb], writes=[r_hs])
                                P.op("dve", lambda e, ox_=ox_: e.tensor_tensor(out=ox_[:], in0=hs[:], in1=sgm[:], op=ALU.mult), reads=[r_hs, r_sgm], writes=[r_ox])
                                P.dma("sp", lambda e, ox_=ox_, tok0=tok0: e.dma_start(out=mix[tok0:tok0 + 64, 0:256], in_=ox_[:]), reads=[r_ox], writes=[r_mix])
                            for hh in range(2):
                                pu_, r_pu = psU[hh]
                                P.op("pe", lambda e, kv_=kv_, hh=hh, pu_=pu_: e.matmul(pu_[:, 0:257], lhsT=kv_[:, hh * 128:(hh + 1) * 128], rhs=vs[:], start=True, stop=True), reads=[r_kvt, r_vs], writes=[r_pu])
                                P.op("dve", lambda e, hh=hh, pu_=pu_: e.scalar_tensor_tensor(out=Cs[:, hh, :], in0=Cs[:, hh, :], scalar=wold[:, 0:1], in1=pu_[:, 0:257], op0=ALU.mult, op1=ALU.add), reads=[r_Cs, r_wold, r_pu], writes=[r_Cs])
                            P.op("act", lambda e: e.activation(out=Cb[:], in_=Cs[:], func=AF.Copy), reads=[r_Cs], writes=[r_Cb])
                            P.op("dve", lambda e: e.tensor_copy(out=mcur[:], in_=ml2[:, 1:2]), reads=[r_ml2, r_mcur], writes=[r_mcur])
                P.barrier()
                P.flush()
        if "D" in phases:
            with ExitStack() as st:
                C = Ctx(nc, st, P)
                idf, r_idf, idb, r_idb = make_ident(P, C)
                Zt, r_Zt = C.sb("Zt", [64, 30, 64], F32)
                mk_, r_mk = C.sb("nmaskt", [64, 64], F32)
                P.dma("sp", lambda e: e.dma_start(out=mk_[:], in_=nmask[:, :]), writes=[r_mk])
                for qc in range(64):
                    P.dma("sp" if qc % 2 else "act", lambda e, qc=qc: e.dma_start(out=Zt[qc:qc + 1, :, :], in_=zall[:, 63 - qc:127 - qc].unsqueeze(0)), writes=[r_Zt])
                for i in range(30):
                    P.op("dve", lambda e, i=i: e.tensor_tensor(out=Zt[:, i, :], in0=Zt[:, i, :], in1=mk_[:], op=ALU.add), reads=[r_Zt, r_mk], writes=[r_Zt])
                Ztf = Zt[:].rearrange("p a b -> p (a b)")
                q2 = [C.sb("nq%d" % i, [128, 2, 64], BF16) for i in range(2)]
                k2 = [C.sb("nk%d" % i, [128, 2, 512], BF16) for i in range(2)]
                v2 = [C.sb("nvv%d" % i, [128, 4, 256], BF16) for i in range(2)]
                sbt = [C.sb("nsb%d" % i, [64, 512], F32) for i in range(2)]
                pb = [C.sb("npb%d" % i, [64, 512], BF16) for i in range(2)]
                pTs = [C.sb("npT%d" % i, [128, 4, 64], BF16) for i in range(2)]
                onb = [C.sb("nob%d" % i, [64, 256], BF16) for i in range(2)]
                mx, r_mx = C.sb("nmx", [64, 1], F32)
                rsum, r_rsum = C.sb("nrsum", [64, 1], F32)
                psS = [C.ps("npsS%d" % i, [128, 512], F32) for i in range(2)]
                psT = [C.ps("npsT%d" % i, [128, 1024], BF16) for i in range(2)]
                psO = [C.ps("npsO%d" % i, [128, 512], F32) for i in range(2)]
                fmv = fm[:, :, :].rearrange("h p t -> p h t")
                NSC = 128 ** -0.5
                it = 0
                ih = 0
                for (s0, sl) in seqs:
                    rows = sl // 64
                    for r in range(rows):
                        rs = min(max(r - 4, 0), rows - 8)
                        rho0 = 3 - ((r - 4) - rs)
                        tq = s0 + r * 64
                        tk = s0 + rs * 64
                        q_, r_q = q2[it % 2]
                        k_, r_k = k2[it % 2]
                        v_, r_v = v2[it % 2]
                        ob_, r_ob = onb[it % 2]
                        it += 1
                        P.dma("sp", lambda e, q_=q_, tq=tq: e.dma_start(out=q_[:], in_=fmv[:, 4:6, tq:tq + 64]), reads=[r_fm], writes=[r_q])
                        P.dma("sp", lambda e, k_=k_, tk=tk: e.dma_start(out=k_[:], in_=fmv[:, 6:8, tk:tk + 512]), reads=[r_fm], writes=[r_k])
                        P.dma("sp", lambda e, v_=v_, tk=tk: e.dma_start(out=v_[:], in_=nv[tk:tk + 512, :].rearrange("(c p) e -> p c e", p=128)), reads=[r_nv], writes=[r_v])
                        for h in range(2):
                            pS_, r_pS = psS[ih % 2]
                            pT_, r_pT = psT[ih % 2]
                            pO_, r_pO = psO[ih % 2]
                            sb_, r_sb = sbt[ih % 2]
                            p_, r_p = pb[ih % 2]
                            pt_, r_pt = pTs[ih % 2]
                            ih += 1
                            P.op("pe", lambda e, pS_=pS_, q_=q_, k_=k_, h=h: e.matmul(pS_[0:64, :], lhsT=q_[:, h, :], rhs=k_[:, h, :], start=True, stop=True), reads=[r_q, r_k], writes=[r_pS])
                            b0 = (h * 15 + rho0) * 64
                            P.op("dve", lambda e, pS_=pS_, sb_=sb_, b0=b0: e.scalar_tensor_tensor(out=sb_[:], in0=pS_[0:64, :], scalar=NSC, in1=Ztf[:, b0:b0 + 512], op0=ALU.mult, op1=ALU.add), reads=[r_pS, r_Zt], writes=[r_sb])
                            P.op("dve", lambda e, sb_=sb_: e.reduce_max(out=mx[:], in_=sb_[:], axis=AX.X), reads=[r_sb], writes=[r_mx])
                            P.op("dve", lambda e: e.tensor_scalar(out=mx[:], in0=mx[:], scalar1=-1.0, scalar2=None, op0=ALU.mult), reads=[r_mx], writes=[r_mx])
                            P.op("act", lambda e, sb_=sb_, p_=p_: e.activation(out=p_[:], in_=sb_[:], func=AF.Exp, bias=mx[:, 0:1], accum_out=rsum[:]), reads=[r_sb, r_mx], writes=[r_p, r_rsum])
                            for c4 in range(4):
                                P.op("pe", lambda e, pT_=pT_, p_=p_, c4=c4: e.transpose(out=pT_[:, c4 * 64:(c4 + 1) * 64], in_=p_[:, c4 * 128:(c4 + 1) * 128], identity=idb[0:64, 0:64]), reads=[r_p, r_idb], writes=[r_pT])
                            P.op("act", lambda e, pT_=pT_, pt_=pt_: e.activation(out=pt_[:].rearrange("p a b -> p (a b)"), in_=pT_[:, 0:256], func=AF.Copy), reads=[r_pT], writes=[r_pt])
                            for c4 in range(4):
                                P.op("pe", lambda e, pO_=pO_, pt_=pt_, v_=v_, c4=c4, h=h: e.matmul(pO_[0:64, 0:128], lhsT=pt_[:, c4, :], rhs=v_[:, c4, h * 128:(h + 1) * 128], start=(c4 == 0), stop=(c4 == 3)), reads=[r_pt, r_v], writes=[r_pO])
                            P.op("dve", lambda e: e.reciprocal(out=rsum[:], in_=rsum[:]), reads=[r_rsum], writes=[r_rsum])
                            P.op("dve", lambda e, pO_=pO_, ob_=ob_, h=h: e.tensor_scalar(out=ob_[:, h * 128:(h + 1) * 128], in0=pO_[0:64, 0:128], scalar1=rsum[:, 0:1], scalar2=None, op0=ALU.mult), reads=[r_pO, r_rsum], writes=[r_ob])
                        P.dma("sp", lambda e, ob_=ob_, tq=tq: e.dma_start(out=mix[tq:tq + 64, 256:512], in_=ob_[:]), reads=[r_ob], writes=[r_mix])
                P.barrier()
                P.flush()
    return nc


def build_l2(cfg, ntile_lim=None):
    D = cfg["D"]
    KD = D // 128
    NK = cfg["NKEYS"]
    NE = NK * NK
    NCORE = cfg["NC"]
    TPp = cfg["SEQ"] // NCORE
    TPC = TPp + cfg["DB"] * cfg["DS"] // NCORE
    ntile = TPC // 128
    nc = bass.Bass("TRN2", target_bir_lowering=False)
    x = nc.dram_tensor("x", [TPC, D], F32, kind="ExternalInput")
    mixo = nc.dram_tensor("mixo", [TPC, D], BF16, kind="ExternalInput")
    wo = nc.dram_tensor("wo", [D, D], F32, kind="ExternalInput")
    wq = nc.dram_tensor("wq", [D, 2048], F32, kind="ExternalInput")
    g1o = nc.dram_tensor("g1o", [2, D], F32, kind="ExternalInput")
    g2o = nc.dram_tensor("g2o", [2, D], F32, kind="ExternalInput")
    sc2o = nc.dram_tensor("sc2o", [2, D], F32, kind="ExternalInput")
    sh2o = nc.dram_tensor("sh2o", [2, D], F32, kind="ExternalInput")
    n2g = nc.dram_tensor("n2g", [1, D], F32, kind="ExternalInput")
    fg = nc.dram_tensor("fg", [1, D], F32, kind="ExternalInput")
    kTd = nc.dram_tensor("kT", [128, 16, NK], F32, kind="ExternalInput")
    u = nc.dram_tensor("u", [NE, D], F32, kind="ExternalInput")
    v = nc.dram_tensor("v", [NE, D], F32, kind="ExternalInput")
    io256 = nc.dram_tensor("io256", [1, 256], F32, kind="ExternalInput")
    y = nc.dram_tensor("y", [TPC, D], F32, kind="ExternalOutput")
    wob = nc.dram_tensor("wob", [D, D], BF16, kind="Internal")
    wqb = nc.dram_tensor("wqb", [D, 2048], BF16, kind="Internal")
    sc2s = nc.dram_tensor("sc2s", [2, D], F32, kind="Internal")
    h2d = nc.dram_tensor("h2d", [TPC, D], BF16, kind="Internal")
    r_y, r_wob, r_wqb, r_sc2s, r_h2d = [Res(n) for n in ("y", "wob", "wqb", "sc2s", "h2d")]
    wobv = wob[:, :].rearrange("(k p) n -> p k n", p=128)
    wqbv = wqb[:, :].rearrange("(k p) n -> p k n", p=128)
    with ExitStack() as st0:
        P = Prog(nc, st0)
        with ExitStack() as st:
            C = Ctx(nc, st, P)
            wf = [C.sb("wf%d" % i, [128, D], F32) for i in range(2)]
            wb_ = [C.sb("wb%d" % i, [128, D], BF16) for i in range(2)]
            n = 0
            for (src, dst, r_dst, ncols) in ((wo, wob, r_wob, D), (wq, wqb, r_wqb, 2048)):
                for k in range(KD):
                    a, r_a = wf[n % 2]
                    o, r_o = wb_[n % 2]
                    P.dma("sp", lambda e, a=a, k=k, src=src, ncols=ncols: e.dma_start(out=a[:, 0:ncols], in_=src[k * 128:(k + 1) * 128, :]), writes=[r_a])
                    if n % 2:
                        P.op("act", lambda e, a=a, o=o, ncols=ncols: e.activation(out=o[:, 0:ncols], in_=a[:, 0:ncols], func=AF.Copy), reads=[r_a], writes=[r_o])
                    else:
                        P.op("dve", lambda e, a=a, o=o, ncols=ncols: e.tensor_copy(out=o[:, 0:ncols], in_=a[:, 0:ncols]), reads=[r_a], writes=[r_o])
                    P.dma("sp", lambda e, o=o, k=k, dst=dst, ncols=ncols: e.dma_start(out=dst[k * 128:(k + 1) * 128, :], in_=o[:, 0:ncols]), reads=[r_o], writes=[r_dst])
                    n += 1
            s2, r_s2 = C.sb("s2", [2, D], F32)
            n2, r_n2 = C.sb("n2", [2, D], F32)
            P.dma("sp", lambda e: e.dma_start(out=s2[:], in_=sc2o[:, :]), writes=[r_s2])
            P.dma("sp", lambda e: e.dma_start(out=n2[:], in_=n2g[:, :].partition_broadcast(2)), writes=[r_n2])
            P.op("dve", lambda e: e.scalar_tensor_tensor(out=s2[:], in0=s2[:], scalar=1.0, in1=n2[:], op0=ALU.add, op1=ALU.mult), reads=[r_s2, r_n2], writes=[r_s2])
            P.dma("sp", lambda e: e.dma_start(out=sc2s[:, :], in_=s2[:]), reads=[r_s2], writes=[r_sc2s])
            P.barrier()
            P.flush()
        with ExitStack() as st:
            C = Ctx(nc, st, P)
            idf, r_idf, idb, r_idb = make_ident(P, C)
            xt, r_xt = C.sb("xt", [128, D], F32)
            t8, r_t8 = C.sb("t8", [128, D], BF16)
            tT, r_tT = C.sb("tT", [128, KD, 128], BF16)
            wblk = [C.sb("wblk%d" % i, [128, KD, 256], BF16) for i in range(2)]
            bc = [C.sb("bc%d" % i, [128, D], F32) for i in range(2)]
            uv = [C.sb("uv%d" % i, [128, D], F32) for i in range(2)]
            hb = [C.sb("hb%d" % i, [128, D], BF16) for i in range(2)]
            junk, r_junk = C.sb("junk", [128, D], BF16)
            tmpf = [C.sb("tmpf%d" % i, [128, 512], F32) for i in range(2)]
            qT, r_qT = C.sb("qT", [128, 16, 128], F32)
            scs, r_scs = C.sb("scs", [128, 16, NK], F32)
            kTt, r_kTt = C.sb("kTt", [128, 16, NK], F32)
            wk, r_wk = C.sb("wk", [128, 256], F32)
            v16, r_v16 = C.sb("v16", [128, 16, 16], F32)
            i16u, r_i16u = C.sb("i16u", [128, 16, 16], U32)
            i16f, r_i16f = C.sb("i16f", [128, 16, 16], F32)
            i1s, r_i1s = C.sb("i1s", [128, 16], F32)
            cand, r_cand = C.sb("cand", [128, 16, 16], F32)
            Eh, r_Eh = C.sb("Eh", [128, 16, 16], F32)
            sc16, r_sc16 = C.sb("sc16", [128, 8, 16], F32)
            ciu, r_ciu = C.sb("ciu", [128, 16], U32)
            cif, r_cif = C.sb("cif", [128, 16], F32)
            io, r_io = C.sb("io", [128, 256], F32)
            eid, r_eid = C.sb("eid", [128, 128], F32)
            gw, r_gw = C.sb("gw", [128, 8, 16], F32)
            nmx, r_nmx = C.sb("nmx", [128, 8], F32)
            gsum, r_gsum = C.sb("gsum", [128, 8], F32)
            idxT, r_idxT = C.sb("idxT", [128, 128], I32)
            gwT, r_gwT = C.sb("gwT", [128, 128], F32)
            ACTT, r_ACTT = C.sb("ACTT", [128, 128], F32)
            Wt, r_Wt = C.sb("Wt", [128, 128], F32)
            gl, r_gl = C.sb("gl", [128, 128], F32)
            Wsel = [C.sb("Wsel%d" % i, [128, 128], F32) for i in range(2)]
            Zc, r_Zc = C.sb("Zc", [128, 255], F32)
            ssq, r_ssq = C.sb("ssq", [128, 1], F32)
            rstd, r_rstd = C.sb("rstd", [128, 1], F32)
            ps = [C.ps("ps%d" % i, [128, 512], F32) for i in range(8)]
            P.op("pool", lambda e: e.memset(Zc[:], 0.0), writes=[r_Zc])
            P.op("pool", lambda e: e.memset(Zc[:, 127:128], 1.0), reads=[r_Zc], writes=[r_Zc])
            P.dma("sp", lambda e: e.dma_start(out=io[:], in_=io256[:, :].partition_broadcast(128)), writes=[r_io])
            P.dma("sp", lambda e: e.dma_start(out=kTt[:], in_=kTd[:, :, :]), writes=[r_kTt])
            cnt = dict(w=0, b=0, bc=0, uv=0, hb=0, t=0, ws=0)

            def rmsn(src_ap_fn):
                P.op("act", lambda e: e.activation(out=junk[:], in_=xt[:], func=AF.Square, accum_out=ssq[:]), reads=[r_xt], writes=[r_junk, r_ssq])
                P.op("dve", lambda e: e.tensor_scalar(out=rstd[:], in0=ssq[:], scalar1=1.0 / D, scalar2=EPS, op0=ALU.mult, op1=ALU.add), reads=[r_ssq], writes=[r_rstd])
                P.op("act", lambda e: e.activation(out=rstd[:], in_=rstd[:], func=AF.Sqrt), reads=[r_rstd], writes=[r_rstd])
                P.op("dve", lambda e: e.reciprocal(out=rstd[:], in_=rstd[:]), reads=[r_rstd], writes=[r_rstd])

            def transposes():
                for q in range(KD // 8):
                    pb_, r_pb = ps[6 + (q % 2)]
                    pv = pb_[:].bitcast(BF16)
                    for j in range(8):
                        k = q * 8 + j
                        P.op("pe", lambda e, pv=pv, j=j, k=k: e.transpose(out=pv[:, j * 128:(j + 1) * 128], in_=t8[:, k * 128:(k + 1) * 128], identity=idb[:]), reads=[r_t8, r_idb], writes=[r_pb])
                    if q % 2:
                        P.op("act", lambda e, pv=pv, q=q: e.activation(out=tT[:, q * 8:(q + 1) * 8, :].rearrange("p a b -> p (a b)"), in_=pv[:, :], func=AF.Copy), reads=[r_pb], writes=[r_tT])
                    else:
                        P.op("dve", lambda e, pv=pv, q=q: e.tensor_copy(out=tT[:, q * 8:(q + 1) * 8, :].rearrange("p a b -> p (a b)"), in_=pv[:, :]), reads=[r_pb], writes=[r_tT])

            def load_bc(src_ap):
                b_, r_b = bc[cnt["bc"] % 2]
                cnt["bc"] += 1
                P.dma("act", lambda e, b_=b_: e.dma_start(out=b_[:], in_=src_ap.partition_broadcast(128)), writes=[r_b])
                return b_, r_b

            def top16(src_ap, r_src, vout, r_vout, iout, r_iout, n):
                wkv = wk[:, 0:n]
                P.op("dve", lambda e: e.max(out=vout[:, 0:8], in_=src_ap), reads=[r_src], writes=[r_vout])
                P.op("dve", lambda e: e.max_index(out=iout[:, 0:8], in_max=vout[:, 0:8], in_values=src_ap), reads=[r_src, r_vout], writes=[r_iout])
                P.op("dve", lambda e: e.match_replace(out=wkv, in_to_replace=vout[:, 0:8], in_values=src_ap, imm_value=-1e30), reads=[r_src, r_vout], writes=[r_wk])
                P.op("dve", lambda e: e.max(out=vout[:, 8:16], in_=wkv), reads=[r_wk], writes=[r_vout])
                P.op("dve", lambda e: e.max_index(out=iout[:, 8:16], in_max=vout[:, 8:16], in_values=wkv), reads=[r_wk, r_vout], writes=[r_iout])

            nt_run = ntile if ntile_lim is None else ntile_lim
            for ti in range(nt_run):
                tok0 = ti * 128
                s = 0 if tok0 < TPp else 1
                for h4 in range(4):
                    P.dma("sp", lambda e, tok0=tok0, h4=h4: e.dma_start(out=xt[:, h4 * (D // 4):(h4 + 1) * (D // 4)], in_=x[tok0:tok0 + 128, h4 * (D // 4):(h4 + 1) * (D // 4)]), writes=[r_xt])
                P.dma("sp", lambda e, tok0=tok0: e.dma_start(out=t8[:], in_=mixo[tok0:tok0 + 128, :]), writes=[r_t8])
                transposes()
                b1, r_b1 = load_bc(g1o[s:s + 1, :])
                for cb in range(D // 256):
                    w_, r_w = wblk[cnt["w"] % 2]
                    cnt["w"] += 1
                    P.dma("sp", lambda e, w_=w_, cb=cb: e.dma_start(out=w_[:], in_=wobv[:, :, cb * 256:(cb + 1) * 256]), reads=[r_wob], writes=[r_w])
                    p_, r_p = ps[cnt["b"] % 6]
                    tf_, r_tf = tmpf[cnt["b"] % 2]
                    cnt["b"] += 1
                    for k in range(KD):
                        P.op("pe", lambda e, p_=p_, w_=w_, k=k: e.matmul(p_[:, 0:256], lhsT=tT[:, k, :], rhs=w_[:, k, :], start=(k == 0), stop=(k == KD - 1)), reads=[r_tT, r_w], writes=[r_p])
                    P.op("dve", lambda e, p_=p_, tf_=tf_, cb=cb, b1=b1: e.tensor_tensor(out=tf_[:, 0:256], in0=p_[:, 0:256], in1=b1[:, cb * 256:(cb + 1) * 256], op=ALU.mult), reads=[r_p, r_b1], writes=[r_tf])
                    P.op("pool", lambda e, tf_=tf_, cb=cb: e.tensor_tensor(out=xt[:, cb * 256:(cb + 1) * 256], in0=xt[:, cb * 256:(cb + 1) * 256], in1=tf_[:, 0:256], op=ALU.add), reads=[r_tf, r_xt], writes=[r_xt])
                rmsn(None)
                b2, r_b2 = load_bc(sc2s[s:s + 1, :])
                b3, r_b3 = load_bc(sh2o[s:s + 1, :])
                u0, r_u0 = uv[0]
                P.op("dve", lambda e, b2=b2: e.scalar_tensor_tensor(out=u0[:], in0=xt[:], scalar=rstd[:, 0:1], in1=b2[:], op0=ALU.mult, op1=ALU.mult), reads=[r_xt, r_rstd, r_b2], writes=[r_u0])
                P.op("dve", lambda e, b3=b3: e.tensor_tensor(out=t8[:], in0=u0[:], in1=b3[:], op=ALU.add), reads=[r_u0, r_b3], writes=[r_t8])
                P.dma("sp", lambda e, tok0=tok0: e.dma_start(out=h2d[tok0:tok0 + 128, :], in_=t8[:]), reads=[r_t8], writes=[r_h2d])
                transposes()
                for j4 in range(8):
                    w_, r_w = wblk[cnt["w"] % 2]
                    cnt["w"] += 1
                    P.dma("sp", lambda e, w_=w_, j4=j4: e.dma_start(out=w_[:], in_=wqbv[:, :, j4 * 256:(j4 + 1) * 256]), reads=[r_wqb], writes=[r_w])
                    for jj in range(2):
                        j = j4 * 2 + jj
                        p_, r_p = ps[cnt["b"] % 6]
                        cnt["b"] += 1
                        for k in range(KD):
                            P.op("pe", lambda e, p_=p_, w_=w_, k=k, jj=jj: e.matmul(p_[:, 0:128], lhsT=w_[:, k, jj * 128:(jj + 1) * 128], rhs=tT[:, k, :], start=(k == 0), stop=(k == KD - 1)), reads=[r_tT, r_w], writes=[r_p])
                        P.op("act", lambda e, p_=p_, j=j: e.activation(out=qT[:, j, :], in_=p_[:, 0:128], func=AF.Copy), reads=[r_p], writes=[r_qT])
                for j in range(16):
                    p_, r_p = ps[cnt["b"] % 6]
                    cnt["b"] += 1
                    P.op("pe", lambda e, p_=p_, j=j: e.matmul(p_[:, 0:NK], lhsT=qT[:, j, :], rhs=kTt[:, j, :], start=True, stop=True), reads=[r_qT, r_kTt], writes=[r_p])
                    P.op("dve", lambda e, p_=p_, j=j: e.tensor_copy(out=scs[:, j, :], in_=p_[:, 0:NK]), reads=[r_p], writes=[r_scs])
                for j in range(16):
                    top16(scs[:, j, :], r_scs, v16[:, j, :], r_v16, i16u[:, j, :], r_i16u, NK)
                P.op("dve", lambda e: e.tensor_copy(out=i16f[:], in_=i16u[:]), reads=[r_i16u], writes=[r_i16f])
                u0v = u0[:].rearrange("p (a b) -> p a b", a=16)
                for h in range(8):
                    j1, j2 = 2 * h, 2 * h + 1
                    P.op("dve", lambda e, j1=j1, j2=j2: e.tensor_tensor(out=cand[:], in0=v16[:, j1, :].unsqueeze(2).to_broadcast([128, 16, 16]), in1=v16[:, j2, :].unsqueeze(1).to_broadcast([128, 16, 16]), op=ALU.add), reads=[r_v16], writes=[r_cand])
                    P.op("dve", lambda e, j1=j1: e.tensor_scalar(out=i1s[:], in0=i16f[:, j1, :], scalar1=float(NK), scalar2=None, op0=ALU.mult), reads=[r_i16f], writes=[r_i1s])
                    P.op("dve", lambda e, j2=j2: e.tensor_tensor(out=Eh[:], in0=i1s[:].unsqueeze(2).to_broadcast([128, 16, 16]), in1=i16f[:, j2, :].unsqueeze(1).to_broadcast([128, 16, 16]), op=ALU.add), reads=[r_i1s, r_i16f], writes=[r_Eh])
                    top16(cand[:].rearrange("p a b -> p (a b)"), r_cand, sc16[:, h, :], r_sc16, ciu[:], r_ciu, 256)
                    P.op("dve", lambda e: e.tensor_copy(out=cif[:], in_=ciu[:]), reads=[r_ciu], writes=[r_cif])
                    P.op("dve", lambda e: e.tensor_tensor(out=u0v, in0=cif[:].unsqueeze(2).to_broadcast([128, 16, 256]), in1=io[:].unsqueeze(1).to_broadcast([128, 16, 256]), op=ALU.is_equal), reads=[r_cif, r_io], writes=[r_u0])
                    P.op("dve", lambda e: e.tensor_tensor(out=u0v, in0=u0v, in1=Eh[:].rearrange("p a b -> p (a b)").unsqueeze(1).to_broadcast([128, 16, 256]), op=ALU.mult), reads=[r_u0, r_Eh], writes=[r_u0])
                    P.op("dve", lambda e, h=h: e.reduce_sum(out=eid[:, h * 16:(h + 1) * 16], in_=u0v, axis=AX.X), reads=[r_u0], writes=[r_eid])
                P.op("dve", lambda e: e.tensor_scalar(out=nmx[:], in0=sc16[:, :, 0], scalar1=-1.0, scalar2=None, op0=ALU.mult), reads=[r_sc16], writes=[r_nmx])
                P.op("dve", lambda e: e.tensor_tensor(out=gw[:], in0=sc16[:], in1=nmx[:].unsqueeze(2).to_broadcast([128, 8, 16]), op=ALU.add), reads=[r_sc16, r_nmx], writes=[r_gw])
                P.op("act", lambda e: e.activation(out=gw[:], in_=gw[:], func=AF.Exp), reads=[r_gw], writes=[r_gw])
                P.op("dve", lambda e: e.reduce_sum(out=gsum[:], in_=gw[:], axis=AX.X), reads=[r_gw], writes=[r_gsum])
                P.op("dve", lambda e: e.reciprocal(out=gsum[:], in_=gsum[:]), reads=[r_gsum], writes=[r_gsum])
                P.op("dve", lambda e: e.tensor_tensor(out=gw[:], in0=gw[:], in1=gsum[:].unsqueeze(2).to_broadcast([128, 8, 16]), op=ALU.mult), reads=[r_gw, r_gsum], writes=[r_gw])
                p_, r_p = ps[cnt["b"] % 6]
                cnt["b"] += 1
                P.op("pe", lambda e, p_=p_: e.transpose(out=p_[:, 0:128], in_=eid[:], identity=idf[:]), reads=[r_eid, r_idf], writes=[r_p])
                P.op("pe", lambda e, p_=p_: e.transpose(out=p_[:, 128:256], in_=gw[:].rearrange("p a b -> p (a b)"), identity=idf[:]), reads=[r_gw, r_idf], writes=[r_p])
                P.op("dve", lambda e, p_=p_: e.tensor_copy(out=idxT[:], in_=p_[:, 0:128]), reads=[r_p], writes=[r_idxT])
                P.op("dve", lambda e, p_=p_: e.tensor_copy(out=gwT[:], in_=p_[:, 128:256]), reads=[r_p], writes=[r_gwT])
                for t in range(128):
                    U_, r_U = uv[cnt["uv"] % 2]
                    cnt["uv"] += 1
                    H_, r_H = hb[cnt["hb"] % 2]
                    cnt["hb"] += 1
                    P.dma("pool", lambda e, U_=U_, t=t: e.indirect_dma_start(out=U_[:], out_offset=None, in_=u[:, :], in_offset=bass.IndirectOffsetOnAxis(ap=idxT[:, t:t + 1], axis=0)), reads=[r_idxT], writes=[r_U])
                    P.dma("sp", lambda e, H_=H_, t=t, tok0=tok0: e.dma_start(out=H_[:], in_=h2d[tok0 + t:tok0 + t + 1, :].partition_broadcast(128)), reads=[r_h2d], writes=[r_H])
                    P.op("dve", lambda e, U_=U_, H_=H_, t=t: e.scalar_tensor_tensor(out=junk[:], in0=U_[:], scalar=1.0, in1=H_[:], op0=ALU.mult, op1=ALU.mult, accum_out=ACTT[:, t:t + 1]), reads=[r_U, r_H], writes=[r_junk, r_ACTT])
                P.op("dve", lambda e: e.tensor_tensor(out=gl[:], in0=ACTT[:], in1=ACTT[:], op=ALU.mult), reads=[r_ACTT], writes=[r_gl])
                P.op("dve", lambda e: e.tensor_scalar(out=gl[:], in0=gl[:], scalar1=0.044715, scalar2=1.0, op0=ALU.mult, op1=ALU.add), reads=[r_gl], writes=[r_gl])
                P.op("dve", lambda e: e.tensor_tensor(out=gl[:], in0=gl[:], in1=ACTT[:], op=ALU.mult), reads=[r_gl, r_ACTT], writes=[r_gl])
                P.op("act", lambda e: e.activation(out=gl[:], in_=gl[:], func=AF.Sigmoid, scale=1.5957691216057308), reads=[r_gl], writes=[r_gl])
                P.op("dve", lambda e: e.tensor_tensor(out=gl[:], in0=gl[:], in1=ACTT[:], op=ALU.mult), reads=[r_gl, r_ACTT], writes=[r_gl])
                P.op("dve", lambda e: e.tensor_tensor(out=Wt[:], in0=gl[:], in1=gwT[:], op=ALU.mult), reads=[r_gl, r_gwT], writes=[r_Wt])
                for t in range(128):
                    V_, r_V = uv[cnt["uv"] % 2]
                    cnt["uv"] += 1
                    ws_, r_ws = Wsel[cnt["ws"] % 2]
                    cnt["ws"] += 1
                    P.dma("pool", lambda e, V_=V_, t=t: e.indirect_dma_start(out=V_[:], out_offset=None, in_=v[:, :], in_offset=bass.IndirectOffsetOnAxis(ap=idxT[:, t:t + 1], axis=0)), reads=[r_idxT], writes=[r_V])
                    P.op("act", lambda e, ws_=ws_, t=t: e.activation(out=ws_[:], in_=Zc[:, 127 - t:255 - t], func=AF.Copy, scale=Wt[:, t:t + 1]), reads=[r_Zc, r_Wt], writes=[r_ws])
                    for cb in range(8):
                        p_, r_p = ps[cb]
                        P.op("pe", lambda e, p_=p_, ws_=ws_, V_=V_, cb=cb, t=t: e.matmul(p_[:, :], lhsT=ws_[:], rhs=V_[:, cb * 512:(cb + 1) * 512], start=(t == 0), stop=(t == 127)), reads=[r_ws, r_V], writes=[r_p])
                b4, r_b4 = load_bc(g2o[s:s + 1, :])
                for cb in range(8):
                    p_, r_p = ps[cb]
                    tf_, r_tf = tmpf[cb % 2]
                    P.op("dve", lambda e, p_=p_, tf_=tf_, cb=cb, b4=b4: e.tensor_tensor(out=tf_[:], in0=p_[:, :], in1=b4[:, cb * 512:(cb + 1) * 512], op=ALU.mult), reads=[r_p, r_b4], writes=[r_tf])
                    P.op("pool", lambda e, tf_=tf_, cb=cb: e.tensor_tensor(out=xt[:, cb * 512:(cb + 1) * 512], in0=xt[:, cb * 512:(cb + 1) * 512], in1=tf_[:], op=ALU.add), reads=[r_tf, r_xt], writes=[r_xt])
                rmsn(None)
                b5, r_b5 = load_bc(fg[0:1, :])
                u1, r_u1 = uv[1]
                P.op("dve", lambda e, b5=b5: e.scalar_tensor_tensor(out=u1[:], in0=xt[:], scalar=rstd[:, 0:1], in1=b5[:], op0=ALU.mult, op1=ALU.mult), reads=[r_xt, r_rstd, r_b5], writes=[r_u1])
                P.dma("sp", lambda e, tok0=tok0: e.dma_start(out=y[tok0:tok0 + 128, :], in_=u1[:]), reads=[r_u1], writes=[r_y])
            P.barrier()
            P.flush()
    return nc


def _T(a, KD):
    return np.ascontiguousarray(a.T.reshape(KD, 128, -1).transpose(1, 0, 2))


def _w_own(w_in, c, D):
    mw = D // 2
    g0 = 4 * mw
    n0 = g0 + 32
    sl = lambda base: w_in[:, base + c * 256: base + (c + 1) * 256]
    gates = w_in[:, [g0 + g * 8 + c for g in range(4)]]
    return np.ascontiguousarray(np.concatenate(
        [sl(0), sl(mw), sl(n0), sl(n0 + mw), sl(mw), sl(2 * mw), sl(3 * mw), sl(n0 + 2 * mw), gates], axis=1))


def _na_consts(rpb2):
    z = np.zeros((2, 15, 127), np.float32)
    z[:, :, 48:79] = rpb2
    cidx = np.arange(64)
    cs = np.clip(cidx - 8, 0, 48)
    ok = (cidx[None, :] >= cs[:, None]) & (cidx[None, :] < cs[:, None] + 16)
    m = np.full((64, 64), NEG, np.float32)
    m[ok] = 0.0
    return z.reshape(30, 127), m


def kernel(x_prompt, x_sample, c_prompt, c_sample, ada_w, ada_b, norm1_g, w_in, gate_b, mlstm_norm_g, na_rpb, w_out,
           norm2_g, peer_wq, peer_k1, peer_k2, peer_u, peer_v, final_g):
    cfg = dict(CFG)
    D, NCORE = cfg["D"], cfg["NC"]
    KD = D // 128
    f = lambda a: np.asarray(a, dtype=np.float32)
    x_all = np.ascontiguousarray(np.concatenate([f(x_prompt)[0], f(x_sample).reshape(-1, D)], axis=0))
    c_all = np.concatenate([f(c_prompt), f(c_sample)], axis=0)
    ncol = 6 * D // NCORE
    nc0 = build_l0(cfg)
    cT = _T(c_all, KD)
    in0 = [{"cT": cT, "w": np.ascontiguousarray(f(ada_w)[0][:, c * ncol:(c + 1) * ncol]),
            "b": np.ascontiguousarray(f(ada_b)[0][None, c * ncol:(c + 1) * ncol])} for c in range(NCORE)]
    r0 = run_bass_kernel_spmd(nc0, in0, core_ids=list(range(NCORE)))
    mod = np.concatenate([r0.results[c]["y"] for c in range(NCORE)], axis=1)
    sh1, sc1, g1, sh2, sc2, g2 = np.split(mod, 6, axis=1)
    nc1 = build_l1(cfg, phases="ABCD")
    n1g = np.ascontiguousarray(f(norm1_g)[0].reshape(KD, 128).T)
    sc1T, sh1T = _T(sc1, KD), _T(sh1, KD)
    in1 = []
    for c in range(NCORE):
        z, m = _na_consts(f(na_rpb)[0, 2 * c:2 * c + 2])
        in1.append({"x": x_all, "sc1T": sc1T, "sh1T": sh1T, "n1g": n1g, "w": _w_own(f(w_in)[0], c, D),
                    "gb": np.ascontiguousarray(f(gate_b)[0][[g * 8 + c for g in range(4)]][None, :]),
                    "mng": np.ascontiguousarray(f(mlstm_norm_g)[0][None, c * 256:(c + 1) * 256]), "zall": z, "nmask": m})
    r1 = run_bass_kernel_spmd(nc1, in1, core_ids=list(range(NCORE)))
    mixT = np.concatenate([np.asarray(r1.results[c]["mix"]) for c in range(NCORE)], axis=1)
    del in1, r1
    SEQ, DB, DS, NK = cfg["SEQ"], cfg["DB"], cfg["DS"], cfg["NKEYS"]
    TPp = SEQ // NCORE
    TSs = DB * DS // NCORE
    nc2 = build_l2(cfg)
    perm = np.concatenate([np.concatenate([np.arange(c * 256, (c + 1) * 256), D // 2 + np.arange(c * 256, (c + 1) * 256)]) for c in range(NCORE)])
    wo = np.ascontiguousarray(f(w_out)[0][perm, :])
    wq = np.ascontiguousarray(f(peer_wq)[0])
    kT = np.zeros((128, 16, NK), np.float32)
    k1, k2 = f(peer_k1)[0], f(peer_k2)[0]
    for h in range(8):
        kT[:, 2 * h, :] = k1[h].T
        kT[:, 2 * h + 1, :] = k2[h].T
    uu = np.ascontiguousarray(f(peer_u)[0])
    vv = np.ascontiguousarray(f(peer_v)[0])
    n2 = np.ascontiguousarray(f(norm2_g)[0][None, :])
    fgv = np.ascontiguousarray(f(final_g).reshape(1, D))
    io = np.arange(256, dtype=np.float32)[None]
    in2 = []
    for c in range(NCORE):
        rows = np.concatenate([np.arange(c * TPp, (c + 1) * TPp), SEQ + np.arange(c * TSs, (c + 1) * TSs)])
        sidx = [0, 1 + (c * TSs) // DS]
        in2.append({"x": np.ascontiguousarray(x_all[rows]), "mixo": np.ascontiguousarray(mixT[rows]), "wo": wo, "wq": wq,
                    "g1o": np.ascontiguousarray(g1[sidx]), "g2o": np.ascontiguousarray(g2[sidx]),
                    "sc2o": np.ascontiguousarray(sc2[sidx]), "sh2o": np.ascontiguousarray(sh2[sidx]),
                    "n2g": n2, "fg": fgv, "kT": kT, "u": uu, "v": vv, "io256": io})
    r2 = run_bass_kernel_spmd(nc2, in2, core_ids=list(range(NCORE)))
    y_prompt = np.zeros((1, SEQ, D), np.float32)
    y_sample = np.zeros((DB * DS, D), np.float32)
    for c in range(NCORE):
        yc = np.asarray(r2.results[c]["y"])
        y_prompt[0, c * TPp:(c + 1) * TPp] = yc[:TPp]
        y_sample[c * TSs:(c + 1) * TSs] = yc[TPp:]
    return (y_prompt, y_sample.reshape(DB, DS, D))
```

```python
import numpy as np
from contextlib import ExitStack
import ml_dtypes
import concourse.bass as bass
import concourse.mybir as mybir
from concourse.bass_utils import run_bass_kernel_spmd

F32 = mybir.dt.float32
BF16 = mybir.dt.bfloat16
I32 = mybir.dt.int32
U32 = mybir.dt.uint32
AF = mybir.ActivationFunctionType
ALU = mybir.AluOpType
AX = mybir.AxisListType
EPS = 1e-6
NEG = -30000.0

CFG = dict(D=4096, SEQ=16384, DB=4, DS=2048, NKEYS=128, NC=8)


class Res:
    __slots__ = ("name", "lw", "rd", "sem", "cnt")

    def __init__(self, name):
        self.name = name
        self.lw = None
        self.rd = {}
        self.sem = None
        self.cnt = 0


class Prog:
    ENG = ("pe", "act", "dve", "pool", "sp")

    def __init__(self, nc, stack):
        self.nc = nc
        self.stack = stack
        self.sems = {}
        self.lists = {e: [] for e in self.ENG}
        self.seq = {e: 0 for e in self.ENG}
        self.seen = {e: {} for e in self.ENG}
        self.final = {}
        for e in self.ENG:
            self._sem("E_" + e)
        self.ninst = 0
        self.dsems = []
        self.dsem_i = 0

    def _sem(self, key):
        if key not in self.sems:
            self.sems[key] = self.stack.enter_context(self.nc.semaphore(key))
            self.final[key] = 0
        return self.sems[key]

    def _deps(self, e, reads, writes):
        evs = {}

        def add(ev):
            if ev is None:
                return
            k, v = ev
            if evs.get(k, 0) < v:
                evs[k] = v
        for r in reads:
            add(r.lw)
        for w in writes:
            add(w.lw)
            for k, v in w.rd.items():
                add((k, v))
        out = []
        for k, v in evs.items():
            if e == "pe" and k == "E_pe":
                continue
            if self.seen[e].get(k, 0) >= v:
                continue
            self.seen[e][k] = v
            out.append((k, v))
        return out

    def _mark(self, ev, reads, writes):
        k, v = ev
        for r in reads:
            if r.rd.get(k, 0) < v:
                r.rd[k] = v
        for w in writes:
            w.lw = ev
            w.rd = {}

    def op(self, e, fn, reads=(), writes=()):
        for k, v in self._deps(e, reads, writes):
            self.lists[e].append(("w", k, v))
        self.seq[e] += 1
        ev = ("E_" + e, self.seq[e])
        self.final[ev[0]] = ev[1]
        self.lists[e].append(("i", fn, ev[0], 1))
        self._mark(ev, reads, writes)
        self.ninst += 1

    def dma(self, e, fn, reads=(), writes=(), dst=None):
        if dst is None:
            dst = writes[0]
        if dst.sem is None:
            dst.sem = "D_" + dst.name
            self._sem(dst.sem)
        for k, v in self._deps(e, reads, writes):
            self.lists[e].append(("w", k, v))
        dst.cnt += 16
        ev = (dst.sem, dst.cnt)
        self.final[dst.sem] = dst.cnt
        self.lists[e].append(("i", fn, dst.sem, 16))
        self._mark(ev, reads, writes)
        self.ninst += 1

    def barrier(self):
        for e in self.ENG:
            for k, v in self.final.items():
                if v > 0 and self.seen[e].get(k, 0) < v and not (k == "E_" + e):
                    self.seen[e][k] = v
                    self.lists[e].append(("w", k, v))

    def flush(self):
        nc = self.nc
        lists = self.lists
        sems = self.sems

        def replay(eng, lst):
            for it in lst:
                if it[0] == "w":
                    eng.wait_ge(sems[it[1]], it[2])
                else:
                    it[1](eng).then_inc(sems[it[2]], it[3])

        with nc.Block() as block:
            @block.tensor
            def _(eng):
                replay(eng, lists["pe"])

            @block.scalar
            def _(eng):
                replay(eng, lists["act"])

            @block.vector
            def _(eng):
                replay(eng, lists["dve"])

            @block.gpsimd
            def _(eng):
                replay(eng, lists["pool"])

            @block.sync
            def _(eng):
                replay(eng, lists["sp"])
        self.lists = {e: [] for e in self.ENG}


class Ctx:
    _n = [0]

    def __init__(self, nc, st, P):
        self.nc, self.st, self.P = nc, st, P
        Ctx._n[0] += 1
        self.pre = "c%d_" % Ctx._n[0]

    def sb(self, name, shape, dt):
        t = self.st.enter_context(self.nc.sbuf_tensor(self.pre + name, shape, dt))
        return t, Res(self.pre + name)

    def ps(self, name, shape, dt):
        t = self.st.enter_context(self.nc.psum_tensor(self.pre + name, shape, dt))
        return t, Res(self.pre + name)


def make_ident(P, C, n=128):
    idf, r_idf = C.sb("identf", [128, 128], F32)
    idb, r_idb = C.sb("identb", [128, 128], BF16)
    P.op("pool", lambda e: e.memset(idf[:], 0.0), writes=[r_idf])
    P.op("pool", lambda e: e.affine_select(out=idf[:], in_=idf[:], pattern=[[-1, 128]], compare_op=ALU.not_equal,
                                           fill=1.0, base=0, channel_multiplier=1), reads=[r_idf], writes=[r_idf])
    P.op("dve", lambda e: e.tensor_copy(out=idb[:], in_=idf[:]), reads=[r_idf], writes=[r_idb])
    return idf, r_idf, idb, r_idb


def build_l0(cfg):
    D = cfg["D"]
    KD = D // 128
    NS = 1 + cfg["DB"]
    NCOL = 6 * D // cfg["NC"]
    nc = bass.Bass("TRN2", target_bir_lowering=False)
    cT = nc.dram_tensor("cT", [128, KD, NS], F32, kind="ExternalInput")
    w = nc.dram_tensor("w", [D, NCOL], F32, kind="ExternalInput")
    b = nc.dram_tensor("b", [1, NCOL], F32, kind="ExternalInput")
    y = nc.dram_tensor("y", [NS, NCOL], F32, kind="ExternalOutput")
    r_y = Res("y")
    wv = w[:, :].rearrange("(k p) n -> p k n", p=128)
    with ExitStack() as st:
        P = Prog(nc, st)
        C = Ctx(nc, st, P)
        ct, r_ct = C.sb("ct", [128, KD, NS], F32)
        sg, r_sg = C.sb("sg", [128, KD, NS], F32)
        bt, r_bt = C.sb("bt", [NS, NCOL], F32)
        ot, r_ot = C.sb("ot", [NS, NCOL], F32)
        wt = [C.sb("wt%d" % i, [128, KD, 512], F32) for i in range(2)]
        pm = [C.ps("pm%d" % i, [128, 512], F32) for i in range(2)]
        P.dma("sp", lambda e: e.dma_start(out=ct[:], in_=cT[:, :, :]), writes=[r_ct])
        P.dma("sp", lambda e: e.dma_start(out=bt[:], in_=b[:, :].partition_broadcast(NS)), writes=[r_bt])
        P.op("act", lambda e: e.activation(out=sg[:], in_=ct[:], func=AF.Silu), reads=[r_ct], writes=[r_sg])
        nb = NCOL // 512
        for j in range(nb):
            wtj, r_w = wt[j % 2]
            pmj, r_p = pm[j % 2]
            for h in range(4):
                P.dma("sp" if h % 2 == 0 else "act", lambda e, wtj=wtj, j=j, h=h: e.dma_start(
                    out=wtj[:, h * (KD // 4):(h + 1) * (KD // 4), :], in_=wv[:, h * (KD // 4):(h + 1) * (KD // 4), j * 512:(j + 1) * 512]), writes=[r_w])
            for k in range(KD):
                P.op("pe", lambda e, wtj=wtj, pmj=pmj, k=k: e.matmul(pmj[0:NS, :], lhsT=sg[:, k, :], rhs=wtj[:, k, :],
                                                                     start=(k == 0), stop=(k == KD - 1)), reads=[r_sg, r_w], writes=[r_p])
            P.op("dve", lambda e, pmj=pmj, j=j: e.tensor_tensor(out=ot[:, j * 512:(j + 1) * 512], in0=pmj[0:NS, :], in1=bt[:, j * 512:(j + 1) * 512], op=ALU.add),
                 reads=[r_p, r_bt], writes=[r_ot])
        P.dma("sp", lambda e: e.dma_start(out=y[:, :], in_=ot[:]), reads=[r_ot], writes=[r_y])
        P.barrier()
        P.flush()
    return nc


WC = 2052


def seq_list(cfg):
    s = [(0, cfg["SEQ"])]
    for b in range(cfg["DB"]):
        s.append((cfg["SEQ"] + b * cfg["DS"], cfg["DS"]))
    return s


def build_l1(cfg, phases="ABCD"):
    D = cfg["D"]
    KD = D // 128
    NS = 1 + cfg["DB"]
    NT = cfg["SEQ"] + cfg["DB"] * cfg["DS"]
    seqs = seq_list(cfg)
    nc = bass.Bass("TRN2", target_bir_lowering=False)
    x = nc.dram_tensor("x", [NT, D], F32, kind="ExternalInput")
    sc1T = nc.dram_tensor("sc1T", [128, KD, NS], F32, kind="ExternalInput")
    sh1T = nc.dram_tensor("sh1T", [128, KD, NS], F32, kind="ExternalInput")
    n1g = nc.dram_tensor("n1g", [128, KD], F32, kind="ExternalInput")
    w = nc.dram_tensor("w", [D, WC], F32, kind="ExternalInput")
    gb = nc.dram_tensor("gb", [1, 4], F32, kind="ExternalInput")
    mng = nc.dram_tensor("mng", [1, 256], F32, kind="ExternalInput")
    zall = nc.dram_tensor("zall", [30, 127], F32, kind="ExternalInput")
    nmask = nc.dram_tensor("nmask", [64, 64], F32, kind="ExternalInput")
    mix = nc.dram_tensor("mix", [NT, 512], BF16, kind="ExternalOutput")
    dbg = "E" in phases
    okind = "ExternalOutput" if dbg else "Internal"
    wb = nc.dram_tensor("wb", [D, WC], BF16, kind="Internal")
    fm = nc.dram_tensor("fm", [8, 128, NT], BF16, kind=okind)
    kv = nc.dram_tensor("kv", [NT, 512], BF16, kind=okind)
    mo = nc.dram_tensor("mo", [NT, 256], F32, kind=okind)
    nv = nc.dram_tensor("nv", [NT, 256], BF16, kind=okind)
    gt = nc.dram_tensor("gt", [NT, 4], F32, kind=okind)
    hf = nc.dram_tensor("hf", [NT, 256], F32, kind="Internal")
    r_mix, r_wb, r_fm, r_kv, r_mo, r_nv, r_gt, r_hf = [Res(n) for n in ("mix", "wb", "fm", "kv", "mo", "nv", "gt", "hf")]
    wv = w[:, :].rearrange("(k p) n -> p k n", p=128)
    wbv = wb[:, :].rearrange("(k p) n -> p k n", p=128)

    with ExitStack() as st0:
        P = Prog(nc, st0)
        if "A" in phases:
            with ExitStack() as st:
                C = Ctx(nc, st, P)
                wf = [C.sb("wf%d" % i, [128, WC], F32) for i in range(2)]
                wo = [C.sb("wo%d" % i, [128, WC], BF16) for i in range(2)]
                for k in range(KD):
                    a, r_a = wf[k % 2]
                    o, r_o = wo[k % 2]
                    P.dma("sp", lambda e, a=a, k=k: e.dma_start(out=a[:], in_=w[k * 128:(k + 1) * 128, :]), writes=[r_a])
                    P.op("act" if k % 2 else "dve", (lambda e, a=a, o=o: e.activation(out=o[:], in_=a[:], func=AF.Copy)) if k % 2 else
                         (lambda e, a=a, o=o: e.tensor_copy(out=o[:], in_=a[:])), reads=[r_a], writes=[r_o])
                    P.dma("sp", lambda e, o=o, k=k: e.dma_start(out=wb[k * 128:(k + 1) * 128, :], in_=o[:]), reads=[r_o], writes=[r_wb])
                P.barrier()
                P.flush()
        if "B" in phases:
            with ExitStack() as st:
                C = Ctx(nc, st, P)
                idf, r_idf, idb, r_idb = make_ident(P, C)
                sct, r_sct = C.sb("sct", [128, KD, NS], F32)
                sht, r_sht = C.sb("sht", [128, KD, NS], F32)
                ngt, r_ngt = C.sb("ngt", [128, KD], F32)
                gbt, r_gbt = C.sb("gbt", [128, 4], F32)
                P.dma("sp", lambda e: e.dma_start(out=sct[:], in_=sc1T[:, :, :]), writes=[r_sct])
                P.dma("sp", lambda e: e.dma_start(out=sht[:], in_=sh1T[:, :, :]), writes=[r_sht])
                P.dma("sp", lambda e: e.dma_start(out=ngt[:], in_=n1g[:, :]), writes=[r_ngt])
                P.dma("sp", lambda e: e.dma_start(out=gbt[:], in_=gb[:, :].partition_broadcast(128)), writes=[r_gbt])
                P.op("dve", lambda e: e.tensor_scalar(out=sct[:], in0=sct[:], scalar1=1.0, scalar2=None, op0=ALU.add), reads=[r_sct], writes=[r_sct])
                for s in range(NS):
                    P.op("dve", lambda e, s=s: e.tensor_tensor(out=sct[:, :, s], in0=sct[:, :, s], in1=ngt[:], op=ALU.mult), reads=[r_sct, r_ngt], writes=[r_sct])
                xt = [C.sb("xt%d" % i, [128, D], F32) for i in range(2)]
                xn, r_xn = C.sb("xn", [128, D], BF16)
                jk, r_jk = C.sb("jk", [128, D], BF16)
                ssq, r_ssq = C.sb("ssq", [128, 1], F32)
                rstd, r_rstd = C.sb("rstd", [128, 1], F32)
                hT = [C.sb("hT%d" % i, [128, KD, 512], BF16) for i in range(2)]
                wfm = [C.sb("wfm%d" % i, [128, KD, 128], BF16) for i in range(2)]
                wtm = [C.sb("wtm%d" % i, [128, KD, 512], BF16) for i in range(2)]
                wg, r_wg = C.sb("wg", [128, KD, 4], BF16)
                ofm = [C.sb("ofm%d" % i, [128, 512], BF16) for i in range(2)]
                okv = [C.sb("okv%d" % i, [128, 512], BF16) for i in range(2)]
                omo = [C.sb("omo%d" % i, [128, 512], F32) for i in range(2)]
                og = [C.sb("og%d" % i, [128, 4], F32) for i in range(2)]
                pT = [C.ps("pT%d" % i, [128, 1024], BF16) for i in range(2)]
                pM = [C.ps("pM%d" % i, [128, 512], F32) for i in range(4)]
                P.dma("sp", lambda e: e.dma_start(out=wg[:], in_=wbv[:, :, 2048:2052]), reads=[r_wb], writes=[r_wg])
                ngrp = NT // 512
                cnt = dict(t=0, p=0, fm=0, tm=0, o=0)
                for g in range(ngrp):
                    hTg, r_hT = hT[g % 2]
                    for ti in range(4):
                        tok0 = g * 512 + ti * 128
                        s = [i for i, (a, l) in enumerate(seqs) if a <= tok0 < a + l][0]
                        xtt, r_xt = xt[cnt["t"] % 2]
                        cnt["t"] += 1
                        for h in range(4):
                            P.dma("sp", lambda e, xtt=xtt, tok0=tok0, h=h: e.dma_start(out=xtt[:, h * (D // 4):(h + 1) * (D // 4)], in_=x[tok0:tok0 + 128, h * (D // 4):(h + 1) * (D // 4)]), writes=[r_xt])
                        P.op("act", lambda e, xtt=xtt: e.activation(out=jk[:], in_=xtt[:], func=AF.Square, accum_out=ssq[:]), reads=[r_xt], writes=[r_jk, r_ssq])
                        P.op("dve", lambda e: e.tensor_scalar(out=rstd[:], in0=ssq[:], scalar1=1.0 / D, scalar2=EPS, op0=ALU.mult, op1=ALU.add), reads=[r_ssq], writes=[r_rstd])
                        P.op("act", lambda e: e.activation(out=rstd[:], in_=rstd[:], func=AF.Sqrt), reads=[r_rstd], writes=[r_rstd])
                        P.op("dve", lambda e: e.reciprocal(out=rstd[:], in_=rstd[:]), reads=[r_rstd], writes=[r_rstd])
                        P.op("act", lambda e, xtt=xtt: e.activation(out=xn[:], in_=xtt[:], func=AF.Copy, scale=rstd[:]), reads=[r_xt, r_rstd], writes=[r_xn])
                        for q in range(KD // 8):
                            pTt, r_pT = pT[cnt["p"] % 2]
                            cnt["p"] += 1
                            for j in range(8):
                                k = q * 8 + j
                                P.op("pe", lambda e, pTt=pTt, j=j, k=k: e.transpose(out=pTt[:, j * 128:(j + 1) * 128], in_=xn[:, k * 128:(k + 1) * 128], identity=idb[:]),
                                     reads=[r_xn, r_idb], writes=[r_pT])
                            for j in range(8):
                                k = q * 8 + j
                                if j % 2 == 0:
                                    P.op("act", lambda e, pTt=pTt, j=j, k=k, s=s, hTg=hTg, ti=ti: e.activation(out=hTg[:, k, ti * 128:(ti + 1) * 128], in_=pTt[:, j * 128:(j + 1) * 128],
                                                                                                          func=AF.Identity, scale=sct[:, k, s:s + 1], bias=sht[:, k, s:s + 1]),
                                         reads=[r_pT, r_sct, r_sht], writes=[r_hT])
                                else:
                                    P.op("dve", lambda e, pTt=pTt, j=j, k=k, s=s, hTg=hTg, ti=ti: e.tensor_scalar(out=hTg[:, k, ti * 128:(ti + 1) * 128], in0=pTt[:, j * 128:(j + 1) * 128],
                                                                                                             scalar1=sct[:, k, s:s + 1], scalar2=sht[:, k, s:s + 1], op0=ALU.mult, op1=ALU.add),
                                         reads=[r_pT, r_sct, r_sht], writes=[r_hT])
                    if cfg.get('lim', 9) < 2:
                        continue
                    for cb in range(8):
                        wt_, r_w = wfm[cnt["fm"] % 2]
                        cnt["fm"] += 1
                        P.dma("sp", lambda e, wt_=wt_, cb=cb: e.dma_start(out=wt_[:], in_=wbv[:, :, cb * 128:(cb + 1) * 128]), reads=[r_wb], writes=[r_w])
                        pm_, r_p = pM[cnt["o"] % 4]
                        o_, r_o = ofm[cnt["o"] % 2]
                        cnt["o"] += 1
                        for k in range(KD):
                            P.op("pe", lambda e, pm_=pm_, wt_=wt_, hTg=hTg, k=k: e.matmul(pm_[:, :], lhsT=wt_[:, k, :], rhs=hTg[:, k, :], start=(k == 0), stop=(k == KD - 1)),
                                 reads=[r_w, r_hT], writes=[r_p])
                        P.op("act", lambda e, pm_=pm_, o_=o_: e.activation(out=o_[:], in_=pm_[:, :], func=AF.Copy), reads=[r_p], writes=[r_o])
                        P.dma("sp", lambda e, o_=o_, cb=cb, g=g: e.dma_start(out=fm[cb, :, g * 512:(g + 1) * 512], in_=o_[:]), reads=[r_o], writes=[r_fm])
                    if cfg.get('lim', 9) < 2.5:
                        continue
                    for blk in range(2 if cfg.get('lim', 9) != 2.5 else 1):
                        wt_, r_w = wtm[cnt["tm"] % 2]
                        cnt["tm"] += 1
                        for h in range(2):
                            P.dma("sp", lambda e, wt_=wt_, blk=blk, h=h: e.dma_start(out=wt_[:, h * (KD // 2):(h + 1) * (KD // 2), :], in_=wbv[:, h * (KD // 2):(h + 1) * (KD // 2), 1024 + blk * 512:1536 + blk * 512]), reads=[r_wb], writes=[r_w])
                        for ti in range(4):
                            tok0 = g * 512 + ti * 128
                            pm_, r_p = pM[cnt["o"] % 4]
                            o_, r_o = okv[cnt["o"] % 2]
                            o2_, r_o2 = omo[cnt["o"] % 2]
                            cnt["o"] += 1
                            for k in range(KD):
                                P.op("pe", lambda e, pm_=pm_, wt_=wt_, hTg=hTg, k=k, ti=ti: e.matmul(pm_[:, :], lhsT=hTg[:, k, ti * 128:(ti + 1) * 128], rhs=wt_[:, k, :], start=(k == 0), stop=(k == KD - 1)),
                                     reads=[r_w, r_hT], writes=[r_p])
                            if blk == 0:
                                P.op("dve", lambda e, pm_=pm_, o_=o_: e.tensor_copy(out=o_[:], in_=pm_[:, :]), reads=[r_p], writes=[r_o])
                                P.dma("sp", lambda e, o_=o_, tok0=tok0: e.dma_start(out=kv[tok0:tok0 + 128, :], in_=o_[:]), reads=[r_o], writes=[r_kv])
                            else:
                                P.op("act", lambda e, pm_=pm_, o2_=o2_: e.activation(out=o2_[:], in_=pm_[:, :], func=AF.Copy), reads=[r_p], writes=[r_o2])
                                P.op("dve", lambda e, o2_=o2_, o_=o_: e.tensor_copy(out=o_[:, 0:256], in_=o2_[:, 256:512]), reads=[r_o2], writes=[r_o])
                                P.dma("sp", lambda e, o2_=o2_, tok0=tok0: e.dma_start(out=mo[tok0:tok0 + 128, :], in_=o2_[:, 0:256]), reads=[r_o2], writes=[r_mo])
                                P.dma("sp", lambda e, o_=o_, tok0=tok0: e.dma_start(out=nv[tok0:tok0 + 128, :], in_=o_[:, 0:256]), reads=[r_o], writes=[r_nv])
                    if cfg.get('lim', 9) < 4:
                        continue
                    for ti in range(4):
                        tok0 = g * 512 + ti * 128
                        pm_, r_p = pM[cnt["o"] % 4]
                        o_, r_o = og[cnt["o"] % 2]
                        cnt["o"] += 1
                        for k in range(KD):
                            P.op("pe", lambda e, pm_=pm_, hTg=hTg, k=k, ti=ti: e.matmul(pm_[:, 0:4], lhsT=hTg[:, k, ti * 128:(ti + 1) * 128], rhs=wg[:, k, :], start=(k == 0), stop=(k == KD - 1)),
                                 reads=[r_wg, r_hT], writes=[r_p])
                        P.op("dve", lambda e, pm_=pm_, o_=o_: e.tensor_tensor(out=o_[:], in0=pm_[:, 0:4], in1=gbt[:], op=ALU.add), reads=[r_p, r_gbt], writes=[r_o])
                        P.dma("sp", lambda e, o_=o_, tok0=tok0: e.dma_start(out=gt[tok0:tok0 + 128, :], in_=o_[:]), reads=[r_o], writes=[r_gt])
                P.barrier()
                P.flush()
        if "C" in phases:
            gt2 = nc.dram_tensor("gt2", [NT, 4], F32, kind="Internal")
            r_gt2 = Res("gt2")
            with ExitStack() as st:
                C = Ctx(nc, st, P)
                idf, r_idf, idb, r_idb = make_ident(P, C)
                NB = NT // 128
                g_in, r_gin = C.sb("g_in", [128, NB, 4], F32)
                g_a, r_ga = C.sb("g_a", [128, NB, 4], F32)
                g_b, r_gb = C.sb("g_b", [128, NB, 4], F32)
                P.dma("sp", lambda e: e.dma_start(out=g_in[:], in_=gt[:, :].rearrange("(n p) c -> p n c", p=128)), reads=[r_gt], writes=[r_gin])
                P.op("act", lambda e: e.activation(out=g_a[:], in_=g_in[:], func=AF.Abs), reads=[r_gin], writes=[r_ga])
                P.op("act", lambda e: e.activation(out=g_a[:], in_=g_a[:], func=AF.Exp, scale=-1.0), reads=[r_ga], writes=[r_ga])
                P.op("act", lambda e: e.activation(out=g_a[:], in_=g_a[:], func=AF.Ln, bias=1.0), reads=[r_ga], writes=[r_ga])
                P.op("dve", lambda e: e.tensor_scalar(out=g_b[:], in0=g_in[:], scalar1=0.0, scalar2=None, op0=ALU.min), reads=[r_gin], writes=[r_gb])
                P.op("dve", lambda e: e.tensor_tensor(out=g_b[:], in0=g_b[:], in1=g_a[:], op=ALU.subtract), reads=[r_gb, r_ga], writes=[r_gb])
                for col in (0, 2):
                    P.op("dve", lambda e, col=col: e.tensor_copy(out=g_b[:, :, col:col + 1], in_=g_in[:, :, col:col + 1]), reads=[r_gin, r_gb], writes=[r_gb])
                P.dma("sp", lambda e: e.dma_start(out=gt2[:, :].rearrange("(n p) c -> p n c", p=128), in_=g_b[:]), reads=[r_gb], writes=[r_gt2])
                tri = {}
                mneg = {}
                sel = {}
                for dn in ("f", "b"):
                    t_, r_t = C.sb("tri" + dn, [64, 64], F32)
                    m_, r_m = C.sb("mneg" + dn, [64, 64], F32)
                    s_, r_s = C.sb("sel" + dn, [64, 128], F32)
                    sgn = 1 if dn == "f" else -1
                    P.op("pool", lambda e, t_=t_: e.memset(t_[:], 1.0), writes=[r_t])
                    P.op("pool", lambda e, t_=t_, sgn=sgn: e.affine_select(out=t_[:], in_=t_[:], pattern=[[sgn, 64]], compare_op=ALU.is_ge, fill=0.0, base=0, channel_multiplier=-sgn), reads=[r_t], writes=[r_t])
                    P.op("pool", lambda e, m_=m_: e.memset(m_[:], 0.0), writes=[r_m])
                    P.op("pool", lambda e, m_=m_, sgn=sgn: e.affine_select(out=m_[:], in_=m_[:], pattern=[[-sgn, 64]], compare_op=ALU.is_ge, fill=NEG, base=0, channel_multiplier=sgn), reads=[r_m], writes=[r_m])
                    last = 63 if dn == "f" else 0
                    P.op("pool", lambda e, s_=s_: e.memset(s_[:], 0.0), writes=[r_s])
                    P.op("pool", lambda e, s_=s_, last=last: e.affine_select(out=s_[:], in_=s_[:], pattern=[[0, 128]], compare_op=ALU.not_equal, fill=1.0, base=-last, channel_multiplier=1), reads=[r_s], writes=[r_s])
                    tri[dn], mneg[dn], sel[dn] = (t_, r_t), (m_, r_m), (s_, r_s)
                ones, r_ones = C.sb("ones64", [64, 64], F32)
                P.op("pool", lambda e: e.memset(ones[:], 1.0), writes=[r_ones])
                mngb, r_mngb = C.sb("mngb", [64, 256], F32)
                P.dma("sp", lambda e: e.dma_start(out=mngb[:], in_=mng[:, :].partition_broadcast(64)), writes=[r_mngb])
                hbk = nc.dram_tensor("hbk", [NT, 256], F32, kind="Internal")
                r_hbk = Res("hbk")
                KS = 256 ** -0.5
                fmv = fm[:, :, :].rearrange("h p t -> p h t")

                def mk_tiles(dn):
                    T = {}
                    T["Cs"] = C.sb(dn + "Cs", [128, 2, 257], F32)
                    T["Cb"] = C.sb(dn + "Cb", [128, 2, 257], BF16)
                    T["mcur"] = C.sb(dn + "mcur", [128, 1], F32)
                    for nm, shp, dt in (("gch", [64, 4], F32), ("qTc", [128, 2, 64], BF16), ("kTc", [128, 2, 64], BF16), ("kvt", [64, 512], BF16), ("hout", [64, 256], F32)):
                        T[nm] = [C.sb(dn + nm + str(i), shp, dt) for i in range(2)]
                    for nm in ("a", "cmax", "ws", "ecl", "wint", "em", "ad", "rc", "wsk"):
                        T[nm] = C.sb(dn + "sm_" + nm, [64, 1], F32)
                    T["sc2"] = C.sb(dn + "sc2", [64, 2], F32)
                    T["ml2"] = C.sb(dn + "ml2", [128, 2], F32)
                    T["nml2"] = C.sb(dn + "nml2", [128, 2], F32)
                    T["wold"] = C.sb(dn + "wold", [128, 1], F32)
                    T["diagA"] = C.sb(dn + "diagA", [64, 64], F32)
                    T["am"] = C.sb(dn + "am", [64, 64], F32)
                    T["PT"] = C.sb(dn + "PT", [64, 64], BF16)
                    T["vs"] = C.sb(dn + "vs", [64, 257], BF16)
                    T["H1"] = C.sb(dn + "H1", [64, 257], F32)
                    T["Hh"] = C.sb(dn + "Hh", [64, 257], F32)
                    T["psA"] = C.ps(dn + "psA", [128, 512], F32)
                    T["psI"] = C.ps(dn + "psI", [128, 512], F32)
                    T["psQ"] = C.ps(dn + "psQ", [128, 512], F32)
                    T["psU"] = C.ps(dn + "psU", [128, 512], F32)
                    return T

                def chain(dn, T):
                    icol, fcol = (0, 1) if dn == "f" else (2, 3)
                    tri_, r_tri = tri[dn]
                    mneg_, r_mneg = mneg[dn]
                    sel_, r_sel = sel[dn]
                    hdst, r_hdst = (hf, r_hf) if dn == "f" else (hbk, r_hbk)
                    Cs, r_Cs = T["Cs"]
                    Cb, r_Cb = T["Cb"]
                    mcur, r_mcur = T["mcur"]
                    a_, r_a = T["a"]
                    cm_, r_cm = T["cmax"]
                    ws_, r_ws = T["ws"]
                    ecl_, r_ecl = T["ecl"]
                    wi_, r_wi = T["wint"]
                    em_, r_em = T["em"]
                    ad_, r_ad = T["ad"]
                    rc_, r_rc = T["rc"]
                    wsk_, r_wsk = T["wsk"]
                    sc2, r_sc2 = T["sc2"]
                    ml2, r_ml2 = T["ml2"]
                    nml2, r_nml2 = T["nml2"]
                    wold, r_wold = T["wold"]
                    diagA, r_diagA = T["diagA"]
                    am, r_am = T["am"]
                    PT, r_PT = T["PT"]
                    vs, r_vs = T["vs"]
                    H1, r_H1 = T["H1"]
                    Hh, r_Hh = T["Hh"]
                    psA, r_psA = T["psA"]
                    psI, r_psI = T["psI"]
                    psQ, r_psQ = T["psQ"]
                    psU, r_psU = T["psU"]
                    it = 0
                    for (s0, sl) in seqs:
                        P.op("pool", lambda e: e.memset(Cs[:], 0.0), writes=[r_Cs])
                        P.op("pool", lambda e: e.memset(Cb[:], 0.0), writes=[r_Cb])
                        P.op("pool", lambda e: e.memset(mcur[:], 0.0), writes=[r_mcur])
                        yield
                        nch = sl // 64
                        order = range(nch) if dn == "f" else range(nch - 1, -1, -1)
                        for ch in order:
                            tok0 = s0 + ch * 64
                            g_, r_g = T["gch"][it % 2]
                            q_, r_q = T["qTc"][it % 2]
                            k_, r_k = T["kTc"][it % 2]
                            kv_, r_kvt = T["kvt"][it % 2]
                            ho_, r_ho = T["hout"][it % 2]
                            it += 1
                            P.dma("sp", lambda e, g_=g_, tok0=tok0: e.dma_start(out=g_[:], in_=gt2[tok0:tok0 + 64, :]), reads=[r_gt2], writes=[r_g])
                            P.dma("sp", lambda e, q_=q_, tok0=tok0: e.dma_start(out=q_[:], in_=fmv[:, 0:2, tok0:tok0 + 64]), reads=[r_fm], writes=[r_q])
                            P.dma("sp", lambda e, k_=k_, tok0=tok0: e.dma_start(out=k_[:], in_=fmv[:, 2:4, tok0:tok0 + 64]), reads=[r_fm], writes=[r_k])
                            P.dma("sp", lambda e, kv_=kv_, tok0=tok0: e.dma_start(out=kv_[:], in_=kv[tok0:tok0 + 64, :]), reads=[r_kv], writes=[r_kvt])
                            yield
                            ops = []
                            A = ops.append
                            A(("pe", lambda e, g_=g_: e.matmul(psA[0:64, 0:1], lhsT=tri_[:], rhs=g_[:, fcol:fcol + 1], start=True, stop=True), [r_tri, r_g], [r_psA]))
                            A(("dve", lambda e, g_=g_: e.tensor_tensor(out=a_[:], in0=g_[:, icol:icol + 1], in1=psA[0:64, 0:1], op=ALU.subtract), [r_g, r_psA], [r_a]))
                            A(("dve", lambda e: e.tensor_scalar(out=diagA[:], in0=idf[0:64, 0:64], scalar1=a_[:, 0:1], scalar2=None, op0=ALU.mult), [r_idf, r_a], [r_diagA]))
                            A(("pe", lambda e: e.matmul(psA[0:64, 64:128], lhsT=ones[:], rhs=diagA[:], start=True, stop=True), [r_ones, r_diagA], [r_psA]))
                            A(("dve", lambda e: e.tensor_tensor(out=am[:], in0=psA[0:64, 64:128], in1=mneg_[:], op=ALU.add), [r_psA, r_mneg], [r_am]))
                            A(("dve", lambda e: e.reduce_max(out=cm_[:], in_=am[:], axis=AX.X), [r_am], [r_cm]))
                            A(("dve", lambda e: e.tensor_tensor(out=sc2[:, 0:1], in0=cm_[:], in1=mcur[0:64, :], op=ALU.max), [r_cm, r_mcur], [r_sc2]))
                            A(("dve", lambda e: e.tensor_tensor(out=sc2[:, 1:2], in0=sc2[:, 0:1], in1=psA[0:64, 0:1], op=ALU.add), [r_sc2, r_psA], [r_sc2]))
                            A(("pe", lambda e: e.matmul(psA[:, 128:130], lhsT=sel_[:], rhs=sc2[:], start=True, stop=True), [r_sel, r_sc2], [r_psA]))
                            A(("dve", lambda e: e.tensor_copy(out=ml2[:], in_=psA[:, 128:130]), [r_psA], [r_ml2]))
                            A(("dve", lambda e: e.tensor_scalar(out=nml2[:], in0=ml2[:], scalar1=-1.0, scalar2=None, op0=ALU.mult), [r_ml2], [r_nml2]))
                            A(("act", lambda e: e.activation(out=ws_[:], in_=a_[:], func=AF.Exp, bias=nml2[0:64, 0:1]), [r_a, r_nml2], [r_ws]))
                            A(("act", lambda e: e.activation(out=ecl_[:], in_=sc2[:, 0:1], func=AF.Exp, scale=-1.0, bias=ml2[0:64, 0:1]), [r_sc2, r_ml2], [r_ecl]))
                            A(("act", lambda e: e.activation(out=wi_[:], in_=sc2[:, 0:1], func=AF.Exp, scale=-1.0, bias=mcur[0:64, :]), [r_sc2, r_mcur], [r_wi]))
                            A(("act", lambda e: e.activation(out=em_[:], in_=sc2[:, 1:2], func=AF.Exp, scale=-1.0), [r_sc2], [r_em]))
                            A(("act", lambda e: e.activation(out=wold[:], in_=mcur[:], func=AF.Exp, bias=nml2[:, 0:1]), [r_mcur, r_nml2], [r_wold]))
                            A(("dve", lambda e: e.tensor_scalar(out=wsk_[:], in0=ws_[:], scalar1=KS, scalar2=None, op0=ALU.mult), [r_ws], [r_wsk]))
                            A(("dve", lambda e, kv_=kv_: e.tensor_scalar(out=vs[:, 0:256], in0=kv_[:, 256:512], scalar1=wsk_[:, 0:1], scalar2=None, op0=ALU.mult), [r_kvt, r_wsk], [r_vs]))
                            A(("dve", lambda e: e.tensor_copy(out=vs[:, 256:257], in_=wsk_[:]), [r_wsk, r_vs], [r_vs]))
                            for hh in range(2):
                                A(("pe", lambda e, k_=k_, q_=q_, hh=hh: e.matmul(psA[0:64, 256:320], lhsT=k_[:, hh, :], rhs=q_[:, hh, :], start=(hh == 0), stop=(hh == 1)), [r_k, r_q], [r_psA]))
                            A(("dve", lambda e: e.tensor_tensor(out=PT[:], in0=psA[0:64, 256:320], in1=tri_[:], op=ALU.mult), [r_psA, r_tri], [r_PT]))
                            A(("pe", lambda e: e.matmul(psI[0:64, 0:257], lhsT=PT[:], rhs=vs[:], start=True, stop=True), [r_PT, r_vs], [r_psI]))
                            for hh in range(2):
                                A(("pe", lambda e, q_=q_, hh=hh: e.matmul(psQ[0:64, 0:257], lhsT=q_[:, hh, :], rhs=Cb[:, hh, :], start=(hh == 0), stop=(hh == 1)), [r_q, r_Cb], [r_psQ]))
                            A(("dve", lambda e: e.tensor_scalar(out=H1[:], in0=psQ[0:64, 0:257], scalar1=wi_[:, 0:1], scalar2=None, op0=ALU.mult), [r_psQ, r_wi], [r_H1]))
                            A(("dve", lambda e: e.scalar_tensor_tensor(out=Hh[:], in0=psI[0:64, 0:257], scalar=ecl_[:, 0:1], in1=H1[:], op0=ALU.mult, op1=ALU.add), [r_psI, r_ecl, r_H1], [r_Hh]))
                            A(("act", lambda e: e.activation(out=ad_[:], in_=Hh[:, 256:257], func=AF.Abs), [r_Hh], [r_ad]))
                            A(("dve", lambda e: e.tensor_tensor(out=ad_[:], in0=ad_[:], in1=em_[:], op=ALU.max), [r_ad, r_em], [r_ad]))
                            A(("dve", lambda e: e.reciprocal(out=rc_[:], in_=ad_[:]), [r_ad], [r_rc]))
                            A(("dve", lambda e, ho_=ho_: e.tensor_scalar(out=ho_[:], in0=Hh[:, 0:256], scalar1=rc_[:, 0:1], scalar2=None, op0=ALU.mult), [r_Hh, r_rc], [r_ho]))
                            for hh in range(2):
                                A(("pe", lambda e, kv_=kv_, hh=hh: e.matmul(psU[:, 0:257], lhsT=kv_[:, hh * 128:(hh + 1) * 128], rhs=vs[:], start=True, stop=True), [r_kvt, r_vs], [r_psU]))
                                A(("dve", lambda e, hh=hh: e.scalar_tensor_tensor(out=Cs[:, hh, :], in0=Cs[:, hh, :], scalar=wold[:, 0:1], in1=psU[:, 0:257], op0=ALU.mult, op1=ALU.add), [r_Cs, r_wold, r_psU], [r_Cs]))
                            A(("act", lambda e: e.activation(out=Cb[:], in_=Cs[:], func=AF.Copy), [r_Cs], [r_Cb]))
                            A(("dve", lambda e: e.tensor_copy(out=mcur[:], in_=ml2[:, 1:2]), [r_ml2, r_mcur], [r_mcur]))
                            for (en, fn, rd, wr) in ops:
                                P.op(en, fn, reads=rd, writes=wr)
                                yield
                            P.dma("sp", lambda e, ho_=ho_, tok0=tok0: e.dma_start(out=hdst[tok0:tok0 + 64, :], in_=ho_[:]), reads=[r_ho], writes=[r_hdst])
                            yield

                gens = [chain("f", mk_tiles("f")), chain("b", mk_tiles("b"))]
                alive = [True, True]
                while any(alive):
                    for gi in range(2):
                        if alive[gi]:
                            try:
                                next(gens[gi])
                            except StopIteration:
                                alive[gi] = False
                P.barrier()
                P.flush()
            with ExitStack() as st:
                C = Ctx(nc, st, P)
                mngb, r_mngb = C.sb("mngb2", [128, 256], F32)
                P.dma("sp", lambda e: e.dma_start(out=mngb[:], in_=mng[:, :].partition_broadcast(128)), writes=[r_mngb])
                ta = [C.sb("ca%d" % i, [128, 256], F32) for i in range(2)]
                tb = [C.sb("cb%d" % i, [128, 256], F32) for i in range(2)]
                tm = [C.sb("cm%d" % i, [128, 256], F32) for i in range(2)]
                to = [C.sb("co%d" % i, [128, 256], BF16) for i in range(2)]
                jq, r_jq = C.sb("cjq", [128, 256], F32)
                ssq_, r_ssq = C.sb("cssq", [128, 1], F32)
                rs_, r_rs = C.sb("crs", [128, 1], F32)
                for i in range(NT // 128):
                    tok0 = i * 128
                    a_, r_a = ta[i % 2]
                    b_, r_b = tb[i % 2]
                    m_, r_m = tm[i % 2]
                    o_, r_o = to[i % 2]
                    P.dma("sp", lambda e, a_=a_, tok0=tok0: e.dma_start(out=a_[:], in_=hf[tok0:tok0 + 128, :]), reads=[r_hf], writes=[r_a])
                    P.dma("sp", lambda e, b_=b_, tok0=tok0: e.dma_start(out=b_[:], in_=hbk[tok0:tok0 + 128, :]), reads=[r_hbk], writes=[r_b])
                    P.dma("sp", lambda e, m_=m_, tok0=tok0: e.dma_start(out=m_[:], in_=mo[tok0:tok0 + 128, :]), reads=[r_mo], writes=[r_m])
                    P.op("dve", lambda e, a_=a_, b_=b_: e.tensor_tensor(out=a_[:], in0=a_[:], in1=b_[:], op=ALU.add), reads=[r_a, r_b], writes=[r_a])
                    P.op("act", lambda e, a_=a_: e.activation(out=jq[:], in_=a_[:], func=AF.Square, accum_out=ssq_[:]), reads=[r_a], writes=[r_jq, r_ssq])
                    P.op("dve", lambda e: e.tensor_scalar(out=rs_[:], in0=ssq_[:], scalar1=1.0 / 256, scalar2=EPS, op0=ALU.mult, op1=ALU.add), reads=[r_ssq], writes=[r_rs])
                    P.op("act", lambda e: e.activation(out=rs_[:], in_=rs_[:], func=AF.Sqrt), reads=[r_rs], writes=[r_rs])
                    P.op("dve", lambda e: e.reciprocal(out=rs_[:], in_=rs_[:]), reads=[r_rs], writes=[r_rs])
                    P.op("act", lambda e, m_=m_: e.activation(out=m_[:], in_=m_[:], func=AF.Sigmoid), reads=[r_m], writes=[r_m])
                    P.op("dve", lambda e, a_=a_: e.scalar_tensor_tensor(out=a_[:], in0=a_[:], scalar=rs_[:, 0:1], in1=mngb[:], op0=ALU.mult, op1=ALU.mult), reads=[r_a, r_rs, r_mngb], writes=[r_a])
                    P.op("dve", lambda e, a_=a_, m_=m_, o_=o_: e.tensor_tensor(out=o_[:], in0=a_[:], in1=m_[:], op=ALU.mult), reads=[r_a, r_m], writes=[r_o])
                    P.dma("sp", lambda e, o_=o_, tok0=tok0: e.dma_start(out=mix[tok0:tok0 + 128, 0:256], in_=o_[:]), reads=[r_o], writes=[r_mix])
                P.barrier()
                P.flush()
        if "D" in phases:
            with ExitStack() as st:
                C = Ctx(nc, st, P)
                idf, r_idf, idb, r_idb = make_ident(P, C)
                Zt, r_Zt = C.sb("Zt", [64, 30, 64], F32)
                mk_, r_mk = C.sb("nmaskt", [64, 64], F32)
                P.dma("sp", lambda e: e.dma_start(out=mk_[:], in_=nmask[:, :]), writes=[r_mk])
                for qc in range(64):
                    P.dma("sp" if qc % 2 else "act", lambda e, qc=qc: e.dma_start(out=Zt[qc:qc + 1, :, :], in_=zall[:, 63 - qc:127 - qc].unsqueeze(0)), writes=[r_Zt])
                for i in range(30):
                    P.op("dve", lambda e, i=i: e.tensor_tensor(out=Zt[:, i, :], in0=Zt[:, i, :], in1=mk_[:], op=ALU.add), reads=[r_Zt, r_mk], writes=[r_Zt])
                Ztf = Zt[:].rearrange("p a b -> p (a b)")
                q2 = [C.sb("nq%d" % i, [128, 2, 64], BF16) for i in range(2)]
                k2 = [C.sb("nk%d" % i, [128, 2, 512], BF16) for i in range(2)]
                v2 = [C.sb("nvv%d" % i, [128, 4, 256], BF16) for i in range(2)]
                sbt = [C.sb("nsb%d" % i, [64, 512], F32) for i in range(2)]
                pb = [C.sb("npb%d" % i, [64, 512], BF16) for i in range(2)]
                pTs = [C.sb("npT%d" % i, [128, 4, 64], BF16) for i in range(2)]
                onb = [C.sb("nob%d" % i, [64, 256], BF16) for i in range(2)]
                mx, r_mx = C.sb("nmx", [64, 1], F32)
                rsum, r_rsum = C.sb("nrsum", [64, 1], F32)
                psS = [C.ps("npsS%d" % i, [128, 512], F32) for i in range(2)]
                psT = [C.ps("npsT%d" % i, [128, 1024], BF16) for i in range(2)]
                psO = [C.ps("npsO%d" % i, [128, 512], F32) for i in range(2)]
                fmv = fm[:, :, :].rearrange("h p t -> p h t")
                NSC = 128 ** -0.5
                it = 0
                ih = 0
                for (s0, sl) in seqs:
                    rows = sl // 64
                    for r in range(rows):
                        rs = min(max(r - 4, 0), rows - 8)
                        rho0 = 3 - ((r - 4) - rs)
                        tq = s0 + r * 64
                        tk = s0 + rs * 64
                        q_, r_q = q2[it % 2]
                        k_, r_k = k2[it % 2]
                        v_, r_v = v2[it % 2]
                        ob_, r_ob = onb[it % 2]
                        it += 1
                        P.dma("sp", lambda e, q_=q_, tq=tq: e.dma_start(out=q_[:], in_=fmv[:, 4:6, tq:tq + 64]), reads=[r_fm], writes=[r_q])
                        P.dma("sp", lambda e, k_=k_, tk=tk: e.dma_start(out=k_[:], in_=fmv[:, 6:8, tk:tk + 512]), reads=[r_fm], writes=[r_k])
                        P.dma("sp", lambda e, v_=v_, tk=tk: e.dma_start(out=v_[:], in_=nv[tk:tk + 512, :].rearrange("(c p) e -> p c e", p=128)), reads=[r_nv], writes=[r_v])
                        for h in range(2):
                            pS_, r_pS = psS[ih % 2]
                            pT_, r_pT = psT[ih % 2]
                            pO_, r_pO = psO[ih % 2]
                            sb_, r_sb = sbt[ih % 2]
                            p_, r_p = pb[ih % 2]
                            pt_, r_pt = pTs[ih % 2]
                            ih += 1
                            P.op("pe", lambda e, pS_=pS_, q_=q_, k_=k_, h=h: e.matmul(pS_[0:64, :], lhsT=q_[:, h, :], rhs=k_[:, h, :], start=True, stop=True), reads=[r_q, r_k], writes=[r_pS])
                            b0 = (h * 15 + rho0) * 64
                            P.op("dve", lambda e, pS_=pS_, sb_=sb_, b0=b0: e.scalar_tensor_tensor(out=sb_[:], in0=pS_[0:64, :], scalar=NSC, in1=Ztf[:, b0:b0 + 512], op0=ALU.mult, op1=ALU.add), reads=[r_pS, r_Zt], writes=[r_sb])
                            P.op("dve", lambda e, sb_=sb_: e.reduce_max(out=mx[:], in_=sb_[:], axis=AX.X), reads=[r_sb], writes=[r_mx])
                            P.op("dve", lambda e: e.tensor_scalar(out=mx[:], in0=mx[:], scalar1=-1.0, scalar2=None, op0=ALU.mult), reads=[r_mx], writes=[r_mx])
                            P.op("act", lambda e, sb_=sb_, p_=p_: e.activation(out=p_[:], in_=sb_[:], func=AF.Exp, bias=mx[:, 0:1], accum_out=rsum[:]), reads=[r_sb, r_mx], writes=[r_p, r_rsum])
                            for c4 in range(4):
                                P.op("pe", lambda e, pT_=pT_, p_=p_, c4=c4: e.transpose(out=pT_[:, c4 * 64:(c4 + 1) * 64], in_=p_[:, c4 * 128:(c4 + 1) * 128], identity=idb[0:64, 0:64]), reads=[r_p, r_idb], writes=[r_pT])
                            P.op("act", lambda e, pT_=pT_, pt_=pt_: e.activation(out=pt_[:].rearrange("p a b -> p (a b)"), in_=pT_[:, 0:256], func=AF.Copy), reads=[r_pT], writes=[r_pt])
                            for c4 in range(4):
                                P.op("pe", lambda e, pO_=pO_, pt_=pt_, v_=v_, c4=c4, h=h: e.matmul(pO_[0:64, 0:128], lhsT=pt_[:, c4, :], rhs=v_[:, c4, h * 128:(h + 1) * 128], start=(c4 == 0), stop=(c4 == 3)), reads=[r_pt, r_v], writes=[r_pO])
                            P.op("dve", lambda e: e.reciprocal(out=rsum[:], in_=rsum[:]), reads=[r_rsum], writes=[r_rsum])
                            P.op("dve", lambda e, pO_=pO_, ob_=ob_, h=h: e.tensor_scalar(out=ob_[:, h * 128:(h + 1) * 128], in0=pO_[0:64, 0:128], scalar1=rsum[:, 0:1], scalar2=None, op0=ALU.mult), reads=[r_pO, r_rsum], writes=[r_ob])
                        P.dma("sp", lambda e, ob_=ob_, tq=tq: e.dma_start(out=mix[tq:tq + 64, 256:512], in_=ob_[:]), reads=[r_ob], writes=[r_mix])
                P.barrier()
                P.flush()
    return nc


def build_l2(cfg, ntile_lim=None):
    D = cfg["D"]
    KD = D // 128
    NK = cfg["NKEYS"]
    NE = NK * NK
    NCORE = cfg["NC"]
    TPp = cfg["SEQ"] // NCORE
    TPC = TPp + cfg["DB"] * cfg["DS"] // NCORE
    ntile = TPC // 128
    nc = bass.Bass("TRN2", target_bir_lowering=False)
    x = nc.dram_tensor("x", [TPC, D], F32, kind="ExternalInput")
    mixo = nc.dram_tensor("mixo", [TPC, D], BF16, kind="ExternalInput")
    wo = nc.dram_tensor("wo", [D, D], F32, kind="ExternalInput")
    wq = nc.dram_tensor("wq", [D, 2048], F32, kind="ExternalInput")
    g1o = nc.dram_tensor("g1o", [2, D], F32, kind="ExternalInput")
    g2o = nc.dram_tensor("g2o", [2, D], F32, kind="ExternalInput")
    sc2o = nc.dram_tensor("sc2o", [2, D], F32, kind="ExternalInput")
    sh2o = nc.dram_tensor("sh2o", [2, D], F32, kind="ExternalInput")
    n2g = nc.dram_tensor("n2g", [1, D], F32, kind="ExternalInput")
    fg = nc.dram_tensor("fg", [1, D], F32, kind="ExternalInput")
    kTd = nc.dram_tensor("kT", [128, 16, NK], F32, kind="ExternalInput")
    u = nc.dram_tensor("u", [NE, D], F32, kind="ExternalInput")
    v = nc.dram_tensor("v", [NE, D], F32, kind="ExternalInput")
    io256 = nc.dram_tensor("io256", [1, 256], F32, kind="ExternalInput")
    y = nc.dram_tensor("y", [TPC, D], F32, kind="ExternalOutput")
    wob = nc.dram_tensor("wob", [D, D], BF16, kind="Internal")
    wqb = nc.dram_tensor("wqb", [D, 2048], BF16, kind="Internal")
    sc2s = nc.dram_tensor("sc2s", [2, D], F32, kind="Internal")
    h2d = nc.dram_tensor("h2d", [TPC, D], BF16, kind="Internal")
    r_y, r_wob, r_wqb, r_sc2s, r_h2d = [Res(n) for n in ("y", "wob", "wqb", "sc2s", "h2d")]
    wobv = wob[:, :].rearrange("(k p) n -> p k n", p=128)
    wqbv = wqb[:, :].rearrange("(k p) n -> p k n", p=128)
    with ExitStack() as st0:
        P = Prog(nc, st0)
        with ExitStack() as st:
            C = Ctx(nc, st, P)
            wf = [C.sb("wf%d" % i, [128, D], F32) for i in range(2)]
            wb_ = [C.sb("wb%d" % i, [128, D], BF16) for i in range(2)]
            n = 0
            for (src, dst, r_dst, ncols) in ((wo, wob, r_wob, D), (wq, wqb, r_wqb, 2048)):
                for k in range(KD):
                    a, r_a = wf[n % 2]
                    o, r_o = wb_[n % 2]
                    P.dma("sp", lambda e, a=a, k=k, src=src, ncols=ncols: e.dma_start(out=a[:, 0:ncols], in_=src[k * 128:(k + 1) * 128, :]), writes=[r_a])
                    if n % 2:
                        P.op("act", lambda e, a=a, o=o, ncols=ncols: e.activation(out=o[:, 0:ncols], in_=a[:, 0:ncols], func=AF.Copy), reads=[r_a], writes=[r_o])
                    else:
                        P.op("dve", lambda e, a=a, o=o, ncols=ncols: e.tensor_copy(out=o[:, 0:ncols], in_=a[:, 0:ncols]), reads=[r_a], writes=[r_o])
                    P.dma("sp", lambda e, o=o, k=k, dst=dst, ncols=ncols: e.dma_start(out=dst[k * 128:(k + 1) * 128, :], in_=o[:, 0:ncols]), reads=[r_o], writes=[r_dst])
                    n += 1
            s2, r_s2 = C.sb("s2", [2, D], F32)
            n2, r_n2 = C.sb("n2", [2, D], F32)
            P.dma("sp", lambda e: e.dma_start(out=s2[:], in_=sc2o[:, :]), writes=[r_s2])
            P.dma("sp", lambda e: e.dma_start(out=n2[:], in_=n2g[:, :].partition_broadcast(2)), writes=[r_n2])
            P.op("dve", lambda e: e.scalar_tensor_tensor(out=s2[:], in0=s2[:], scalar=1.0, in1=n2[:], op0=ALU.add, op1=ALU.mult), reads=[r_s2, r_n2], writes=[r_s2])
            P.dma("sp", lambda e: e.dma_start(out=sc2s[:, :], in_=s2[:]), reads=[r_s2], writes=[r_sc2s])
            P.barrier()
            P.flush()
        with ExitStack() as st:
            C = Ctx(nc, st, P)
            idf, r_idf, idb, r_idb = make_ident(P, C)
            xt, r_xt = C.sb("xt", [128, D], F32)
            t8, r_t8 = C.sb("t8", [128, D], BF16)
            tT, r_tT = C.sb("tT", [128, KD, 128], BF16)
            wblk = [C.sb("wblk%d" % i, [128, KD, 256], BF16) for i in range(2)]
            bc = [C.sb("bc%d" % i, [128, D], F32) for i in range(1)]
            uv = [C.sb("uv%d" % i, [128, D], F32) for i in range(2)]
            vv_ = [C.sb("vv%d" % i, [128, D], F32) for i in range(2)]
            vb_ = [C.sb("vb%d" % i, [128, D], BF16) for i in range(2)]
            hb = [C.sb("hb%d" % i, [128, D], BF16) for i in range(2)]
            junk, r_junk = t8, r_t8
            tmpf = [C.sb("tmpf%d" % i, [128, 512], F32) for i in range(2)]
            qT, r_qT = vv_[0][0][:, 0:2048].rearrange("p (a b) -> p a b", a=16), vv_[0][1]
            scs, r_scs = vv_[0][0][:, 2048:2048 + 16 * NK].rearrange("p (a b) -> p a b", a=16), vv_[0][1]
            kTt, r_kTt = C.sb("kTt", [128, 16, NK], F32)
            wk, r_wk = C.sb("wk", [128, 256], F32)
            v16, r_v16 = C.sb("v16", [128, 16, 16], F32)
            i16u, r_i16u = C.sb("i16u", [128, 16, 16], U32)
            i16f, r_i16f = C.sb("i16f", [128, 16, 16], F32)
            i1s, r_i1s = C.sb("i1s", [128, 16], F32)
            cand, r_cand = C.sb("cand", [128, 16, 16], F32)
            Eh, r_Eh = C.sb("Eh", [128, 16, 16], F32)
            sc16, r_sc16 = C.sb("sc16", [128, 8, 16], F32)
            ciu, r_ciu = C.sb("ciu", [128, 16], U32)
            cif, r_cif = C.sb("cif", [128, 16], F32)
            io, r_io = C.sb("io", [128, 256], F32)
            eid, r_eid = C.sb("eid", [128, 128], F32)
            gw, r_gw = C.sb("gw", [128, 8, 16], F32)
            nmx, r_nmx = C.sb("nmx", [128, 8], F32)
            gsum, r_gsum = C.sb("gsum", [128, 8], F32)
            idxT, r_idxT = C.sb("idxT", [128, 128], I32)
            gwT, r_gwT = C.sb("gwT", [128, 128], F32)
            ACTT, r_ACTT = C.sb("ACTT", [128, 128], F32)
            Wt, r_Wt = C.sb("Wt", [128, 128], F32)
            gl, r_gl = C.sb("gl", [128, 128], F32)
            Wsel = [C.sb("Wsel%d" % i, [128, 128], BF16) for i in range(2)]
            sm_ = {n_: C.sb("sm_" + n_, [128, 1], F32) for n_ in ("x2", "u", "sg", "w")}
            Zc, r_Zc = C.sb("Zc", [128, 255], F32)
            ssq, r_ssq = C.sb("ssq", [128, 1], F32)
            rstd, r_rstd = C.sb("rstd", [128, 1], F32)
            ps = [C.ps("ps%d" % i, [128, 512], F32) for i in range(8)]
            P.op("pool", lambda e: e.memset(Zc[:], 0.0), writes=[r_Zc])
            P.op("pool", lambda e: e.memset(Zc[:, 127:128], 1.0), reads=[r_Zc], writes=[r_Zc])
            P.dma("sp", lambda e: e.dma_start(out=io[:], in_=io256[:, :].partition_broadcast(128)), writes=[r_io])
            P.dma("sp", lambda e: e.dma_start(out=kTt[:], in_=kTd[:, :, :]), writes=[r_kTt])
            cnt = dict(w=0, b=0, bc=0, uv=0, hb=0, t=0, ws=0)

            def rmsn(src_ap_fn):
                P.op("act", lambda e: e.activation(out=t8[:], in_=xt[:], func=AF.Square, accum_out=ssq[:]), reads=[r_xt], writes=[r_t8, r_ssq])
                P.op("dve", lambda e: e.tensor_scalar(out=rstd[:], in0=ssq[:], scalar1=1.0 / D, scalar2=EPS, op0=ALU.mult, op1=ALU.add), reads=[r_ssq], writes=[r_rstd])
                P.op("act", lambda e: e.activation(out=rstd[:], in_=rstd[:], func=AF.Sqrt), reads=[r_rstd], writes=[r_rstd])
                P.op("dve", lambda e: e.reciprocal(out=rstd[:], in_=rstd[:]), reads=[r_rstd], writes=[r_rstd])

            def transposes():
                for q in range(KD // 8):
                    pb_, r_pb = ps[6 + (q % 2)]
                    pv = pb_[:].bitcast(BF16)
                    for j in range(8):
                        k = q * 8 + j
                        P.op("pe", lambda e, pv=pv, j=j, k=k: e.transpose(out=pv[:, j * 128:(j + 1) * 128], in_=t8[:, k * 128:(k + 1) * 128], identity=idb[:]), reads=[r_t8, r_idb], writes=[r_pb])
                    if q % 2:
                        P.op("act", lambda e, pv=pv, q=q: e.activation(out=tT[:, q * 8:(q + 1) * 8, :].rearrange("p a b -> p (a b)"), in_=pv[:, :], func=AF.Copy), reads=[r_pb], writes=[r_tT])
                    else:
                        P.op("dve", lambda e, pv=pv, q=q: e.tensor_copy(out=tT[:, q * 8:(q + 1) * 8, :].rearrange("p a b -> p (a b)"), in_=pv[:, :]), reads=[r_pb], writes=[r_tT])

            def load_bc(src_ap):
                b_, r_b = bc[0]
                cnt["bc"] += 1
                P.dma("act", lambda e, b_=b_: e.dma_start(out=b_[:], in_=src_ap.partition_broadcast(128)), writes=[r_b])
                return b_, r_b

            def top16(src_ap, r_src, vout, r_vout, iout, r_iout, n):
                wkv = wk[:, 0:n]
                P.op("dve", lambda e: e.max(out=vout[:, 0:8], in_=src_ap), reads=[r_src], writes=[r_vout])
                P.op("dve", lambda e: e.max_index(out=iout[:, 0:8], in_max=vout[:, 0:8], in_values=src_ap), reads=[r_src, r_vout], writes=[r_iout])
                P.op("dve", lambda e: e.match_replace(out=wkv, in_to_replace=vout[:, 0:8], in_values=src_ap, imm_value=-1e30), reads=[r_src, r_vout], writes=[r_wk])
                P.op("dve", lambda e: e.max(out=vout[:, 8:16], in_=wkv), reads=[r_wk], writes=[r_vout])
                P.op("dve", lambda e: e.max_index(out=iout[:, 8:16], in_max=vout[:, 8:16], in_values=wkv), reads=[r_wk, r_vout], writes=[r_iout])

            nt_run = ntile if ntile_lim is None else ntile_lim
            for ti in range(nt_run):
                tok0 = ti * 128
                s = 0 if tok0 < TPp else 1
                for h4 in range(4):
                    P.dma("sp", lambda e, tok0=tok0, h4=h4: e.dma_start(out=xt[:, h4 * (D // 4):(h4 + 1) * (D // 4)], in_=x[tok0:tok0 + 128, h4 * (D // 4):(h4 + 1) * (D // 4)]), writes=[r_xt])
                P.dma("sp", lambda e, tok0=tok0: e.dma_start(out=t8[:], in_=mixo[tok0:tok0 + 128, :]), writes=[r_t8])
                transposes()
                b1, r_b1 = load_bc(g1o[s:s + 1, :])
                for cb in range(D // 256):
                    w_, r_w = wblk[cnt["w"] % 2]
                    cnt["w"] += 1
                    P.dma("sp", lambda e, w_=w_, cb=cb: e.dma_start(out=w_[:], in_=wobv[:, :, cb * 256:(cb + 1) * 256]), reads=[r_wob], writes=[r_w])
                    p_, r_p = ps[cnt["b"] % 6]
                    tf_, r_tf = tmpf[cnt["b"] % 2]
                    cnt["b"] += 1
                    for k in range(KD):
                        P.op("pe", lambda e, p_=p_, w_=w_, k=k: e.matmul(p_[:, 0:256], lhsT=tT[:, k, :], rhs=w_[:, k, :], start=(k == 0), stop=(k == KD - 1)), reads=[r_tT, r_w], writes=[r_p])
                    P.op("dve", lambda e, p_=p_, tf_=tf_, cb=cb, b1=b1: e.tensor_tensor(out=tf_[:, 0:256], in0=p_[:, 0:256], in1=b1[:, cb * 256:(cb + 1) * 256], op=ALU.mult), reads=[r_p, r_b1], writes=[r_tf])
                    P.op("pool", lambda e, tf_=tf_, cb=cb: e.tensor_tensor(out=xt[:, cb * 256:(cb + 1) * 256], in0=xt[:, cb * 256:(cb + 1) * 256], in1=tf_[:, 0:256], op=ALU.add), reads=[r_tf, r_xt], writes=[r_xt])
                rmsn(None)
                b2, r_b2 = load_bc(sc2s[s:s + 1, :])
                u0, r_u0 = uv[0]
                P.op("dve", lambda e, b2=b2: e.scalar_tensor_tensor(out=u0[:], in0=xt[:], scalar=rstd[:, 0:1], in1=b2[:], op0=ALU.mult, op1=ALU.mult), reads=[r_xt, r_rstd, r_b2], writes=[r_u0])
                b3, r_b3 = load_bc(sh2o[s:s + 1, :])
                P.op("dve", lambda e, b3=b3: e.tensor_tensor(out=t8[:], in0=u0[:], in1=b3[:], op=ALU.add), reads=[r_u0, r_b3], writes=[r_t8])
                P.dma("sp", lambda e, tok0=tok0: e.dma_start(out=h2d[tok0:tok0 + 128, :], in_=t8[:]), reads=[r_t8], writes=[r_h2d])
                transposes()
                for j4 in range(8):
                    w_, r_w = wblk[cnt["w"] % 2]
                    cnt["w"] += 1
                    P.dma("sp", lambda e, w_=w_, j4=j4: e.dma_start(out=w_[:], in_=wqbv[:, :, j4 * 256:(j4 + 1) * 256]), reads=[r_wqb], writes=[r_w])
                    for jj in range(2):
                        j = j4 * 2 + jj
                        p_, r_p = ps[cnt["b"] % 6]
                        cnt["b"] += 1
                        for k in range(KD):
                            P.op("pe", lambda e, p_=p_, w_=w_, k=k, jj=jj: e.matmul(p_[:, 0:128], lhsT=w_[:, k, jj * 128:(jj + 1) * 128], rhs=tT[:, k, :], start=(k == 0), stop=(k == KD - 1)), reads=[r_tT, r_w], writes=[r_p])
                        P.op("act", lambda e, p_=p_, j=j: e.activation(out=qT[:, j, :], in_=p_[:, 0:128], func=AF.Copy), reads=[r_p], writes=[r_qT])
                for j in range(16):
                    p_, r_p = ps[cnt["b"] % 6]
                    cnt["b"] += 1
                    P.op("pe", lambda e, p_=p_, j=j: e.matmul(p_[:, 0:NK], lhsT=qT[:, j, :], rhs=kTt[:, j, :], start=True, stop=True), reads=[r_qT, r_kTt], writes=[r_p])
                    P.op("dve", lambda e, p_=p_, j=j: e.tensor_copy(out=scs[:, j, :], in_=p_[:, 0:NK]), reads=[r_p], writes=[r_scs])
                for j in range(16):
                    top16(scs[:, j, :], r_scs, v16[:, j, :], r_v16, i16u[:, j, :], r_i16u, NK)
                P.op("dve", lambda e: e.tensor_copy(out=i16f[:], in_=i16u[:]), reads=[r_i16u], writes=[r_i16f])
                u0v = u0[:].rearrange("p (a b) -> p a b", a=16)
                for h in range(8):
                    j1, j2 = 2 * h, 2 * h + 1
                    P.op("dve", lambda e, j1=j1, j2=j2: e.tensor_tensor(out=cand[:], in0=v16[:, j1, :].unsqueeze(2).to_broadcast([128, 16, 16]), in1=v16[:, j2, :].unsqueeze(1).to_broadcast([128, 16, 16]), op=ALU.add), reads=[r_v16], writes=[r_cand])
                    P.op("dve", lambda e, j1=j1: e.tensor_scalar(out=i1s[:], in0=i16f[:, j1, :], scalar1=float(NK), scalar2=None, op0=ALU.mult), reads=[r_i16f], writes=[r_i1s])
                    P.op("dve", lambda e, j2=j2: e.tensor_tensor(out=Eh[:], in0=i1s[:].unsqueeze(2).to_broadcast([128, 16, 16]), in1=i16f[:, j2, :].unsqueeze(1).to_broadcast([128, 16, 16]), op=ALU.add), reads=[r_i1s, r_i16f], writes=[r_Eh])
                    top16(cand[:].rearrange("p a b -> p (a b)"), r_cand, sc16[:, h, :], r_sc16, ciu[:], r_ciu, 256)
                    P.op("dve", lambda e: e.tensor_copy(out=cif[:], in_=ciu[:]), reads=[r_ciu], writes=[r_cif])
                    P.op("dve", lambda e: e.tensor_tensor(out=u0v, in0=cif[:].unsqueeze(2).to_broadcast([128, 16, 256]), in1=io[:].unsqueeze(1).to_broadcast([128, 16, 256]), op=ALU.is_equal), reads=[r_cif, r_io], writes=[r_u0])
                    P.op("dve", lambda e: e.tensor_tensor(out=u0v, in0=u0v, in1=Eh[:].rearrange("p a b -> p (a b)").unsqueeze(1).to_broadcast([128, 16, 256]), op=ALU.mult), reads=[r_u0, r_Eh], writes=[r_u0])
                    P.op("dve", lambda e, h=h: e.reduce_sum(out=eid[:, h * 16:(h + 1) * 16], in_=u0v, axis=AX.X), reads=[r_u0], writes=[r_eid])
                P.op("dve", lambda e: e.tensor_scalar(out=nmx[:], in0=sc16[:, :, 0], scalar1=-1.0, scalar2=None, op0=ALU.mult), reads=[r_sc16], writes=[r_nmx])
                P.op("dve", lambda e: e.tensor_tensor(out=gw[:], in0=sc16[:], in1=nmx[:].unsqueeze(2).to_broadcast([128, 8, 16]), op=ALU.add), reads=[r_sc16, r_nmx], writes=[r_gw])
                P.op("act", lambda e: e.activation(out=gw[:], in_=gw[:], func=AF.Exp), reads=[r_gw], writes=[r_gw])
                P.op("dve", lambda e: e.reduce_sum(out=gsum[:], in_=gw[:], axis=AX.X), reads=[r_gw], writes=[r_gsum])
                P.op("dve", lambda e: e.reciprocal(out=gsum[:], in_=gsum[:]), reads=[r_gsum], writes=[r_gsum])
                P.op("dve", lambda e: e.tensor_tensor(out=gw[:], in0=gw[:], in1=gsum[:].unsqueeze(2).to_broadcast([128, 8, 16]), op=ALU.mult), reads=[r_gw, r_gsum], writes=[r_gw])
                p_, r_p = ps[cnt["b"] % 6]
                cnt["b"] += 1
                P.op("pe", lambda e, p_=p_: e.transpose(out=p_[:, 0:128], in_=eid[:], identity=idf[:]), reads=[r_eid, r_idf], writes=[r_p])
                P.op("pe", lambda e, p_=p_: e.transpose(out=p_[:, 128:256], in_=gw[:].rearrange("p a b -> p (a b)"), identity=idf[:]), reads=[r_gw, r_idf], writes=[r_p])
                P.op("dve", lambda e, p_=p_: e.tensor_copy(out=idxT[:], in_=p_[:, 0:128]), reads=[r_p], writes=[r_idxT])
                P.op("dve", lambda e, p_=p_: e.tensor_copy(out=gwT[:], in_=p_[:, 128:256]), reads=[r_p], writes=[r_gwT])
                x2_, r_x2 = sm_["x2"]
                uu_, r_uu = sm_["u"]
                sg_, r_sg = sm_["sg"]
                w1_, r_w1 = sm_["w"]
                for t in range(128):
                    U_, r_U = uv[t % 2]
                    V_, r_V = vv_[t % 2]
                    Vb_, r_Vb = vb_[t % 2]
                    H_, r_H = hb[t % 2]
                    ws_, r_ws = Wsel[t % 2]
                    P.dma("pool", lambda e, U_=U_, t=t: e.indirect_dma_start(out=U_[:], out_offset=None, in_=u[:, :], in_offset=bass.IndirectOffsetOnAxis(ap=idxT[:, t:t + 1], axis=0)), reads=[r_idxT], writes=[r_U])
                    P.dma("sp", lambda e, H_=H_, t=t, tok0=tok0: e.dma_start(out=H_[:], in_=h2d[tok0 + t:tok0 + t + 1, :].partition_broadcast(128)), reads=[r_h2d], writes=[r_H])
                    P.dma("pool", lambda e, V_=V_, t=t: e.indirect_dma_start(out=V_[:], out_offset=None, in_=v[:, :], in_offset=bass.IndirectOffsetOnAxis(ap=idxT[:, t:t + 1], axis=0)), reads=[r_idxT], writes=[r_V])
                    P.op("dve", lambda e, U_=U_, H_=H_, t=t: e.scalar_tensor_tensor(out=U_[:], in0=U_[:], scalar=1.0, in1=H_[:], op0=ALU.mult, op1=ALU.mult, accum_out=ACTT[:, t:t + 1]), reads=[r_U, r_H], writes=[r_U, r_ACTT])
                    P.op("dve", lambda e, t=t: e.tensor_tensor(out=x2_[:], in0=ACTT[:, t:t + 1], in1=ACTT[:, t:t + 1], op=ALU.mult), reads=[r_ACTT], writes=[r_x2])
                    P.op("dve", lambda e: e.tensor_scalar(out=x2_[:], in0=x2_[:], scalar1=0.044715, scalar2=1.0, op0=ALU.mult, op1=ALU.add), reads=[r_x2], writes=[r_x2])
                    P.op("dve", lambda e, t=t: e.tensor_tensor(out=uu_[:], in0=x2_[:], in1=ACTT[:, t:t + 1], op=ALU.mult), reads=[r_x2, r_ACTT], writes=[r_uu])
                    P.op("act", lambda e: e.activation(out=sg_[:], in_=uu_[:], func=AF.Sigmoid, scale=1.5957691216057308), reads=[r_uu], writes=[r_sg])
                    P.op("dve", lambda e, t=t: e.scalar_tensor_tensor(out=w1_[:], in0=sg_[:], scalar=ACTT[:, t:t + 1], in1=gwT[:, t:t + 1], op0=ALU.mult, op1=ALU.mult), reads=[r_sg, r_ACTT, r_gwT], writes=[r_w1])
                    P.op("act", lambda e, ws_=ws_, t=t: e.activation(out=ws_[:], in_=Zc[:, 127 - t:255 - t], func=AF.Copy, scale=w1_[:, 0:1]), reads=[r_Zc, r_w1], writes=[r_ws])
                    P.op("act", lambda e, V_=V_, Vb_=Vb_: e.activation(out=Vb_[:], in_=V_[:], func=AF.Copy), reads=[r_V], writes=[r_Vb])
                    for cb in range(8):
                        p_, r_p = ps[cb]
                        P.op("pe", lambda e, p_=p_, ws_=ws_, Vb_=Vb_, cb=cb, t=t: e.matmul(p_[:, :], lhsT=ws_[:], rhs=Vb_[:, cb * 512:(cb + 1) * 512], start=(t == 0), stop=(t == 127)), reads=[r_ws, r_Vb], writes=[r_p])
                b4, r_b4 = load_bc(g2o[s:s + 1, :])
                for cb in range(8):
                    p_, r_p = ps[cb]
                    tf_, r_tf = tmpf[cb % 2]
                    P.op("dve", lambda e, p_=p_, tf_=tf_, cb=cb, b4=b4: e.tensor_tensor(out=tf_[:], in0=p_[:, :], in1=b4[:, cb * 512:(cb + 1) * 512], op=ALU.mult), reads=[r_p, r_b4], writes=[r_tf])
                    P.op("pool", lambda e, tf_=tf_, cb=cb: e.tensor_tensor(out=xt[:, cb * 512:(cb + 1) * 512], in0=xt[:, cb * 512:(cb + 1) * 512], in1=tf_[:], op=ALU.add), reads=[r_tf, r_xt], writes=[r_xt])
                rmsn(None)
                b5, r_b5 = load_bc(fg[0:1, :])
                u1, r_u1 = uv[1]
                P.op("dve", lambda e, b5=b5: e.scalar_tensor_tensor(out=u1[:], in0=xt[:], scalar=rstd[:, 0:1], in1=b5[:], op0=ALU.mult, op1=ALU.mult), reads=[r_xt, r_rstd, r_b5], writes=[r_u1])
                P.dma("sp", lambda e, tok0=tok0: e.dma_start(out=y[tok0:tok0 + 128, :], in_=u1[:]), reads=[r_u1], writes=[r_y])
            P.barrier()
            P.flush()
    return nc


def _T(a, KD):
    return np.ascontiguousarray(a.T.reshape(KD, 128, -1).transpose(1, 0, 2))


def _w_own(w_in, c, D):
    mw = D // 2
    g0 = 4 * mw
    n0 = g0 + 32
    sl = lambda base: w_in[:, base + c * 256: base + (c + 1) * 256]
    gates = w_in[:, [g0 + g * 8 + c for g in range(4)]]
    return np.ascontiguousarray(np.concatenate(
        [sl(0), sl(mw), sl(n0), sl(n0 + mw), sl(mw), sl(2 * mw), sl(3 * mw), sl(n0 + 2 * mw), gates], axis=1))


def _na_consts(rpb2):
    z = np.zeros((2, 15, 127), np.float32)
    z[:, :, 48:79] = rpb2
    cidx = np.arange(64)
    cs = np.clip(cidx - 8, 0, 48)
    ok = (cidx[None, :] >= cs[:, None]) & (cidx[None, :] < cs[:, None] + 16)
    m = np.full((64, 64), NEG, np.float32)
    m[ok] = 0.0
    return z.reshape(30, 127), m


def kernel(x_prompt, x_sample, c_prompt, c_sample, ada_w, ada_b, norm1_g, w_in, gate_b, mlstm_norm_g, na_rpb, w_out,
           norm2_g, peer_wq, peer_k1, peer_k2, peer_u, peer_v, final_g):
    cfg = dict(CFG)
    D, NCORE = cfg["D"], cfg["NC"]
    KD = D // 128
    f = lambda a: np.asarray(a, dtype=np.float32)
    x_all = np.ascontiguousarray(np.concatenate([f(x_prompt)[0], f(x_sample).reshape(-1, D)], axis=0))
    c_all = np.concatenate([f(c_prompt), f(c_sample)], axis=0)
    ncol = 6 * D // NCORE
    nc0 = build_l0(cfg)
    cT = _T(c_all, KD)
    in0 = [{"cT": cT, "w": np.ascontiguousarray(f(ada_w)[0][:, c * ncol:(c + 1) * ncol]),
            "b": np.ascontiguousarray(f(ada_b)[0][None, c * ncol:(c + 1) * ncol])} for c in range(NCORE)]
    r0 = run_bass_kernel_spmd(nc0, in0, core_ids=list(range(NCORE)))
    mod = np.concatenate([r0.results[c]["y"] for c in range(NCORE)], axis=1)
    sh1, sc1, g1, sh2, sc2, g2 = np.split(mod, 6, axis=1)
    nc1 = build_l1(cfg, phases="ABCD")
    n1g = np.ascontiguousarray(f(norm1_g)[0].reshape(KD, 128).T)
    sc1T, sh1T = _T(sc1, KD), _T(sh1, KD)
    in1 = []
    for c in range(NCORE):
        z, m = _na_consts(f(na_rpb)[0, 2 * c:2 * c + 2])
        in1.append({"x": x_all, "sc1T": sc1T, "sh1T": sh1T, "n1g": n1g, "w": _w_own(f(w_in)[0], c, D),
                    "gb": np.ascontiguousarray(f(gate_b)[0][[g * 8 + c for g in range(4)]][None, :]),
                    "mng": np.ascontiguousarray(f(mlstm_norm_g)[0][None, c * 256:(c + 1) * 256]), "zall": z, "nmask": m})
    r1 = run_bass_kernel_spmd(nc1, in1, core_ids=list(range(NCORE)))
    mixT = np.concatenate([np.asarray(r1.results[c]["mix"]) for c in range(NCORE)], axis=1)
    del in1, r1
    SEQ, DB, DS, NK = cfg["SEQ"], cfg["DB"], cfg["DS"], cfg["NKEYS"]
    TPp = SEQ // NCORE
    TSs = DB * DS // NCORE
    nc2 = build_l2(cfg)
    perm = np.concatenate([np.concatenate([np.arange(c * 256, (c + 1) * 256), D // 2 + np.arange(c * 256, (c + 1) * 256)]) for c in range(NCORE)])
    wo = np.ascontiguousarray(f(w_out)[0][perm, :])
    wq = np.ascontiguousarray(f(peer_wq)[0])
    kT = np.zeros((128, 16, NK), np.float32)
    k1, k2 = f(peer_k1)[0], f(peer_k2)[0]
    for h in range(8):
        kT[:, 2 * h, :] = k1[h].T
        kT[:, 2 * h + 1, :] = k2[h].T
    uu = np.ascontiguousarray(f(peer_u)[0])
    vv = np.ascontiguousarray(f(peer_v)[0])
    n2 = np.ascontiguousarray(f(norm2_g)[0][None, :])
    fgv = np.ascontiguousarray(f(final_g).reshape(1, D))
    io = np.arange(256, dtype=np.float32)[None]
    in2 = []
    for c in range(NCORE):
        rows = np.concatenate([np.arange(c * TPp, (c + 1) * TPp), SEQ + np.arange(c * TSs, (c + 1) * TSs)])
        sidx = [0, 1 + (c * TSs) // DS]
        in2.append({"x": np.ascontiguousarray(x_all[rows]), "mixo": np.ascontiguousarray(mixT[rows]), "wo": wo, "wq": wq,
                    "g1o": np.ascontiguousarray(g1[sidx]), "g2o": np.ascontiguousarray(g2[sidx]),
                    "sc2o": np.ascontiguousarray(sc2[sidx]), "sh2o": np.ascontiguousarray(sh2[sidx]),
                    "n2g": n2, "fg": fgv, "kT": kT, "u": uu, "v": vv, "io256": io})
    r2 = run_bass_kernel_spmd(nc2, in2, core_ids=list(range(NCORE)))
    y_prompt = np.zeros((1, SEQ, D), np.float32)
    y_sample = np.zeros((DB * DS, D), np.float32)
    for c in range(NCORE):
        yc = np.asarray(r2.results[c]["y"])
        y_prompt[0, c * TPp:(c + 1) * TPp] = yc[:TPp]
        y_sample[c * TSs:(c + 1) * TSs] = yc[TPp:]
    return (y_prompt, y_sample.reshape(DB, DS, D))
```

```python
import numpy as np
from contextlib import ExitStack
import ml_dtypes
import concourse.bass as bass
import concourse.mybir as mybir
from concourse.bass_utils import run_bass_kernel_spmd

F32 = mybir.dt.float32
BF16 = mybir.dt.bfloat16
I32 = mybir.dt.int32
U32 = mybir.dt.uint32
AF = mybir.ActivationFunctionType
ALU = mybir.AluOpType
AX = mybir.AxisListType
EPS = 1e-6
NEG = -30000.0

CFG = dict(D=4096, SEQ=16384, DB=4, DS=2048, NKEYS=128, NC=8)


class Res:
    __slots__ = ("name", "lw", "rd", "sem", "cnt")

    def __init__(self, name):
        self.name = name
        self.lw = None
        self.rd = {}
        self.sem = None
        self.cnt = 0


class Prog:
    ENG = ("pe", "act", "dve", "pool", "sp")

    def __init__(self, nc, stack):
        self.nc = nc
        self.stack = stack
        self.sems = {}
        self.lists = {e: [] for e in self.ENG}
        self.seq = {e: 0 for e in self.ENG}
        self.seen = {e: {} for e in self.ENG}
        self.final = {}
        for e in self.ENG:
            self._sem("E_" + e)
        self.ninst = 0
        self.dsems = []
        self.dsem_i = 0

    def _sem(self, key):
        if key not in self.sems:
            self.sems[key] = self.stack.enter_context(self.nc.semaphore(key))
            self.final[key] = 0
        return self.sems[key]

    def _deps(self, e, reads, writes):
        evs = {}

        def add(ev):
            if ev is None:
                return
            k, v = ev
            if evs.get(k, 0) < v:
                evs[k] = v
        for r in reads:
            add(r.lw)
        for w in writes:
            add(w.lw)
            for k, v in w.rd.items():
                add((k, v))
        out = []
        for k, v in evs.items():
            if e == "pe" and k == "E_pe":
                continue
            if self.seen[e].get(k, 0) >= v:
                continue
            self.seen[e][k] = v
            out.append((k, v))
        return out

    def _mark(self, ev, reads, writes):
        k, v = ev
        for r in reads:
            if r.rd.get(k, 0) < v:
                r.rd[k] = v
        for w in writes:
            w.lw = ev
            w.rd = {}

    def op(self, e, fn, reads=(), writes=()):
        for k, v in self._deps(e, reads, writes):
            self.lists[e].append(("w", k, v))
        self.seq[e] += 1
        ev = ("E_" + e, self.seq[e])
        self.final[ev[0]] = ev[1]
        self.lists[e].append(("i", fn, ev[0], 1))
        self._mark(ev, reads, writes)
        self.ninst += 1

    def dma(self, e, fn, reads=(), writes=(), dst=None):
        if dst is None:
            dst = writes[0]
        if dst.sem is None:
            dst.sem = "D_" + dst.name
            self._sem(dst.sem)
        for k, v in self._deps(e, reads, writes):
            self.lists[e].append(("w", k, v))
        dst.cnt += 16
        ev = (dst.sem, dst.cnt)
        self.final[dst.sem] = dst.cnt
        self.lists[e].append(("i", fn, dst.sem, 16))
        self._mark(ev, reads, writes)
        self.ninst += 1

    def barrier(self):
        for e in self.ENG:
            for k, v in self.final.items():
                if v > 0 and self.seen[e].get(k, 0) < v and not (k == "E_" + e):
                    self.seen[e][k] = v
                    self.lists[e].append(("w", k, v))

    def flush(self):
        nc = self.nc
        lists = self.lists
        sems = self.sems

        def replay(eng, lst):
            for it in lst:
                if it[0] == "w":
                    eng.wait_ge(sems[it[1]], it[2])
                else:
                    it[1](eng).then_inc(sems[it[2]], it[3])

        with nc.Block() as block:
            @block.tensor
            def _(eng):
                replay(eng, lists["pe"])

            @block.scalar
            def _(eng):
                replay(eng, lists["act"])

            @block.vector
            def _(eng):
                replay(eng, lists["dve"])

            @block.gpsimd
            def _(eng):
                replay(eng, lists["pool"])

            @block.sync
            def _(eng):
                replay(eng, lists["sp"])
        self.lists = {e: [] for e in self.ENG}


class Ctx:
    _n = [0]

    def __init__(self, nc, st, P):
        self.nc, self.st, self.P = nc, st, P
        Ctx._n[0] += 1
        self.pre = "c%d_" % Ctx._n[0]

    def sb(self, name, shape, dt):
        t = self.st.enter_context(self.nc.sbuf_tensor(self.pre + name, shape, dt))
        return t, Res(self.pre + name)

    def ps(self, name, shape, dt):
        t = self.st.enter_context(self.nc.psum_tensor(self.pre + name, shape, dt))
        return t, Res(self.pre + name)


def make_ident(P, C, n=128):
    idf, r_idf = C.sb("identf", [128, 128], F32)
    idb, r_idb = C.sb("identb", [128, 128], BF16)
    P.op("pool", lambda e: e.memset(idf[:], 0.0), writes=[r_idf])
    P.op("pool", lambda e: e.affine_select(out=idf[:], in_=idf[:], pattern=[[-1, 128]], compare_op=ALU.not_equal,
                                           fill=1.0, base=0, channel_multiplier=1), reads=[r_idf], writes=[r_idf])
    P.op("dve", lambda e: e.tensor_copy(out=idb[:], in_=idf[:]), reads=[r_idf], writes=[r_idb])
    return idf, r_idf, idb, r_idb


def build_l0(cfg):
    D = cfg["D"]
    KD = D // 128
    NS = 1 + cfg["DB"]
    NCOL = 6 * D // cfg["NC"]
    nc = bass.Bass("TRN2", target_bir_lowering=False)
    cT = nc.dram_tensor("cT", [128, KD, NS], F32, kind="ExternalInput")
    w = nc.dram_tensor("w", [D, NCOL], F32, kind="ExternalInput")
    b = nc.dram_tensor("b", [1, NCOL], F32, kind="ExternalInput")
    y = nc.dram_tensor("y", [NS, NCOL], F32, kind="ExternalOutput")
    r_y = Res("y")
    wv = w[:, :].rearrange("(k p) n -> p k n", p=128)
    with ExitStack() as st:
        P = Prog(nc, st)
        C = Ctx(nc, st, P)
        ct, r_ct = C.sb("ct", [128, KD, NS], F32)
        sg, r_sg = C.sb("sg", [128, KD, NS], F32)
        bt, r_bt = C.sb("bt", [NS, NCOL], F32)
        ot, r_ot = C.sb("ot", [NS, NCOL], F32)
        wt = [C.sb("wt%d" % i, [128, KD, 512], F32) for i in range(2)]
        pm = [C.ps("pm%d" % i, [128, 512], F32) for i in range(2)]
        P.dma("sp", lambda e: e.dma_start(out=ct[:], in_=cT[:, :, :]), writes=[r_ct])
        P.dma("sp", lambda e: e.dma_start(out=bt[:], in_=b[:, :].partition_broadcast(NS)), writes=[r_bt])
        P.op("act", lambda e: e.activation(out=sg[:], in_=ct[:], func=AF.Silu), reads=[r_ct], writes=[r_sg])
        nb = NCOL // 512
        for j in range(nb):
            wtj, r_w = wt[j % 2]
            pmj, r_p = pm[j % 2]
            for h in range(4):
                P.dma("sp" if h % 2 == 0 else "act", lambda e, wtj=wtj, j=j, h=h: e.dma_start(
                    out=wtj[:, h * (KD // 4):(h + 1) * (KD // 4), :], in_=wv[:, h * (KD // 4):(h + 1) * (KD // 4), j * 512:(j + 1) * 512]), writes=[r_w])
            for k in range(KD):
                P.op("pe", lambda e, wtj=wtj, pmj=pmj, k=k: e.matmul(pmj[0:NS, :], lhsT=sg[:, k, :], rhs=wtj[:, k, :],
                                                                     start=(k == 0), stop=(k == KD - 1)), reads=[r_sg, r_w], writes=[r_p])
            P.op("dve", lambda e, pmj=pmj, j=j: e.tensor_tensor(out=ot[:, j * 512:(j + 1) * 512], in0=pmj[0:NS, :], in1=bt[:, j * 512:(j + 1) * 512], op=ALU.add),
                 reads=[r_p, r_bt], writes=[r_ot])
        P.dma("sp", lambda e: e.dma_start(out=y[:, :], in_=ot[:]), reads=[r_ot], writes=[r_y])
        P.barrier()
        P.flush()
    return nc


WC = 2052


def seq_list(cfg):
    s = [(0, cfg["SEQ"])]
    for b in range(cfg["DB"]):
        s.append((cfg["SEQ"] + b * cfg["DS"], cfg["DS"]))
    return s


def build_l1(cfg, phases="ABCD"):
    D = cfg["D"]
    KD = D // 128
    NS = 1 + cfg["DB"]
    NT = cfg["SEQ"] + cfg["DB"] * cfg["DS"]
    seqs = seq_list(cfg)
    nc = bass.Bass("TRN2", target_bir_lowering=False)
    x = nc.dram_tensor("x", [NT, D], F32, kind="ExternalInput")
    sc1T = nc.dram_tensor("sc1T", [128, KD, NS], F32, kind="ExternalInput")
    sh1T = nc.dram_tensor("sh1T", [128, KD, NS], F32, kind="ExternalInput")
    n1g = nc.dram_tensor("n1g", [128, KD], F32, kind="ExternalInput")
    w = nc.dram_tensor("w", [D, WC], F32, kind="ExternalInput")
    gb = nc.dram_tensor("gb", [1, 4], F32, kind="ExternalInput")
    mng = nc.dram_tensor("mng", [1, 256], F32, kind="ExternalInput")
    zall = nc.dram_tensor("zall", [30, 127], F32, kind="ExternalInput")
    nmask = nc.dram_tensor("nmask", [64, 64], F32, kind="ExternalInput")
    mix = nc.dram_tensor("mix", [NT, 512], BF16, kind="ExternalOutput")
    dbg = "E" in phases
    okind = "ExternalOutput" if dbg else "Internal"
    wb = nc.dram_tensor("wb", [D, WC], BF16, kind="Internal")
    fm = nc.dram_tensor("fm", [8, 128, NT], BF16, kind=okind)
    kv = nc.dram_tensor("kv", [NT, 512], BF16, kind=okind)
    mo = nc.dram_tensor("mo", [NT, 256], F32, kind=okind)
    nv = nc.dram_tensor("nv", [NT, 256], BF16, kind=okind)
    gt = nc.dram_tensor("gt", [NT, 4], F32, kind=okind)
    hf = nc.dram_tensor("hf", [NT, 256], F32, kind="Internal")
    r_mix, r_wb, r_fm, r_kv, r_mo, r_nv, r_gt, r_hf = [Res(n) for n in ("mix", "wb", "fm", "kv", "mo", "nv", "gt", "hf")]
    wv = w[:, :].rearrange("(k p) n -> p k n", p=128)
    wbv = wb[:, :].rearrange("(k p) n -> p k n", p=128)

    with ExitStack() as st0:
        P = Prog(nc, st0)
        if "A" in phases:
            with ExitStack() as st:
                C = Ctx(nc, st, P)
                wf = [C.sb("wf%d" % i, [128, WC], F32) for i in range(2)]
                wo = [C.sb("wo%d" % i, [128, WC], BF16) for i in range(2)]
                for k in range(KD):
                    a, r_a = wf[k % 2]
                    o, r_o = wo[k % 2]
                    P.dma("sp", lambda e, a=a, k=k: e.dma_start(out=a[:], in_=w[k * 128:(k + 1) * 128, :]), writes=[r_a])
                    P.op("act" if k % 2 else "dve", (lambda e, a=a, o=o: e.activation(out=o[:], in_=a[:], func=AF.Copy)) if k % 2 else
                         (lambda e, a=a, o=o: e.tensor_copy(out=o[:], in_=a[:])), reads=[r_a], writes=[r_o])
                    P.dma("sp", lambda e, o=o, k=k: e.dma_start(out=wb[k * 128:(k + 1) * 128, :], in_=o[:]), reads=[r_o], writes=[r_wb])
                P.barrier()
                P.flush()
        if "B" in phases:
            with ExitStack() as st:
                C = Ctx(nc, st, P)
                idf, r_idf, idb, r_idb = make_ident(P, C)
                sct, r_sct = C.sb("sct", [128, KD, NS], F32)
                sht, r_sht = C.sb("sht", [128, KD, NS], F32)
                ngt, r_ngt = C.sb("ngt", [128, KD], F32)
                gbt, r_gbt = C.sb("gbt", [128, 4], F32)
                P.dma("sp", lambda e: e.dma_start(out=sct[:], in_=sc1T[:, :, :]), writes=[r_sct])
                P.dma("sp", lambda e: e.dma_start(out=sht[:], in_=sh1T[:, :, :]), writes=[r_sht])
                P.dma("sp", lambda e: e.dma_start(out=ngt[:], in_=n1g[:, :]), writes=[r_ngt])
                P.dma("sp", lambda e: e.dma_start(out=gbt[:], in_=gb[:, :].partition_broadcast(128)), writes=[r_gbt])
                P.op("dve", lambda e: e.tensor_scalar(out=sct[:], in0=sct[:], scalar1=1.0, scalar2=None, op0=ALU.add), reads=[r_sct], writes=[r_sct])
                for s in range(NS):
                    P.op("dve", lambda e, s=s: e.tensor_tensor(out=sct[:, :, s], in0=sct[:, :, s], in1=ngt[:], op=ALU.mult), reads=[r_sct, r_ngt], writes=[r_sct])
                xt = [C.sb("xt%d" % i, [128, D], F32) for i in range(2)]
                xn, r_xn = C.sb("xn", [128, D], BF16)
                jk, r_jk = C.sb("jk", [128, D], BF16)
                ssq, r_ssq = C.sb("ssq", [128, 1], F32)
                rstd, r_rstd = C.sb("rstd", [128, 1], F32)
                hT = [C.sb("hT%d" % i, [128, KD, 512], BF16) for i in range(2)]
                wfm = [C.sb("wfm%d" % i, [128, KD, 128], BF16) for i in range(2)]
                wtm = [C.sb("wtm%d" % i, [128, KD, 512], BF16) for i in range(2)]
                wg, r_wg = C.sb("wg", [128, KD, 4], BF16)
                ofm = [C.sb("ofm%d" % i, [128, 512], BF16) for i in range(2)]
                okv = [C.sb("okv%d" % i, [128, 512], BF16) for i in range(2)]
                omo = [C.sb("omo%d" % i, [128, 512], F32) for i in range(2)]
                og = [C.sb("og%d" % i, [128, 4], F32) for i in range(2)]
                pT = [C.ps("pT%d" % i, [128, 1024], BF16) for i in range(2)]
                pM = [C.ps("pM%d" % i, [128, 512], F32) for i in range(4)]
                P.dma("sp", lambda e: e.dma_start(out=wg[:], in_=wbv[:, :, 2048:2052]), reads=[r_wb], writes=[r_wg])
                ngrp = NT // 512
                cnt = dict(t=0, p=0, fm=0, tm=0, o=0)
                for g in range(ngrp):
                    hTg, r_hT = hT[g % 2]
                    for ti in range(4):
                        tok0 = g * 512 + ti * 128
                        s = [i for i, (a, l) in enumerate(seqs) if a <= tok0 < a + l][0]
                        xtt, r_xt = xt[cnt["t"] % 2]
                        cnt["t"] += 1
                        for h in range(4):
                            P.dma("sp", lambda e, xtt=xtt, tok0=tok0, h=h: e.dma_start(out=xtt[:, h * (D // 4):(h + 1) * (D // 4)], in_=x[tok0:tok0 + 128, h * (D // 4):(h + 1) * (D // 4)]), writes=[r_xt])
                        P.op("act", lambda e, xtt=xtt: e.activation(out=jk[:], in_=xtt[:], func=AF.Square, accum_out=ssq[:]), reads=[r_xt], writes=[r_jk, r_ssq])
                        P.op("dve", lambda e: e.tensor_scalar(out=rstd[:], in0=ssq[:], scalar1=1.0 / D, scalar2=EPS, op0=ALU.mult, op1=ALU.add), reads=[r_ssq], writes=[r_rstd])
                        P.op("act", lambda e: e.activation(out=rstd[:], in_=rstd[:], func=AF.Sqrt), reads=[r_rstd], writes=[r_rstd])
                        P.op("dve", lambda e: e.reciprocal(out=rstd[:], in_=rstd[:]), reads=[r_rstd], writes=[r_rstd])
                        P.op("act", lambda e, xtt=xtt: e.activation(out=xn[:], in_=xtt[:], func=AF.Copy, scale=rstd[:]), reads=[r_xt, r_rstd], writes=[r_xn])
                        for q in range(KD // 8):
                            pTt, r_pT = pT[cnt["p"] % 2]
                            cnt["p"] += 1
                            for j in range(8):
                                k = q * 8 + j
                                P.op("pe", lambda e, pTt=pTt, j=j, k=k: e.transpose(out=pTt[:, j * 128:(j + 1) * 128], in_=xn[:, k * 128:(k + 1) * 128], identity=idb[:]),
                                     reads=[r_xn, r_idb], writes=[r_pT])
                            for j in range(8):
                                k = q * 8 + j
                                if j % 2 == 0:
                                    P.op("act", lambda e, pTt=pTt, j=j, k=k, s=s, hTg=hTg, ti=ti: e.activation(out=hTg[:, k, ti * 128:(ti + 1) * 128], in_=pTt[:, j * 128:(j + 1) * 128],
                                                                                                          func=AF.Identity, scale=sct[:, k, s:s + 1], bias=sht[:, k, s:s + 1]),
                                         reads=[r_pT, r_sct, r_sht], writes=[r_hT])
                                else:
                                    P.op("dve", lambda e, pTt=pTt, j=j, k=k, s=s, hTg=hTg, ti=ti: e.tensor_scalar(out=hTg[:, k, ti * 128:(ti + 1) * 128], in0=pTt[:, j * 128:(j + 1) * 128],
                                                                                                             scalar1=sct[:, k, s:s + 1], scalar2=sht[:, k, s:s + 1], op0=ALU.mult, op1=ALU.add),
                                         reads=[r_pT, r_sct, r_sht], writes=[r_hT])
                    if cfg.get('lim', 9) < 2:
                        continue
                    for cb in range(8):
                        wt_, r_w = wfm[cnt["fm"] % 2]
                        cnt["fm"] += 1
                        P.dma("sp", lambda e, wt_=wt_, cb=cb: e.dma_start(out=wt_[:], in_=wbv[:, :, cb * 128:(cb + 1) * 128]), reads=[r_wb], writes=[r_w])
                        pm_, r_p = pM[cnt["o"] % 4]
                        o_, r_o = ofm[cnt["o"] % 2]
                        cnt["o"] += 1
                        for k in range(KD):
                            P.op("pe", lambda e, pm_=pm_, wt_=wt_, hTg=hTg, k=k: e.matmul(pm_[:, :], lhsT=wt_[:, k, :], rhs=hTg[:, k, :], start=(k == 0), stop=(k == KD - 1)),
                                 reads=[r_w, r_hT], writes=[r_p])
                        P.op("act", lambda e, pm_=pm_, o_=o_: e.activation(out=o_[:], in_=pm_[:, :], func=AF.Copy), reads=[r_p], writes=[r_o])
                        P.dma("sp", lambda e, o_=o_, cb=cb, g=g: e.dma_start(out=fm[cb, :, g * 512:(g + 1) * 512], in_=o_[:]), reads=[r_o], writes=[r_fm])
                    if cfg.get('lim', 9) < 2.5:
                        continue
                    for blk in range(2 if cfg.get('lim', 9) != 2.5 else 1):
                        wt_, r_w = wtm[cnt["tm"] % 2]
                        cnt["tm"] += 1
                        for h in range(2):
                            P.dma("sp", lambda e, wt_=wt_, blk=blk, h=h: e.dma_start(out=wt_[:, h * (KD // 2):(h + 1) * (KD // 2), :], in_=wbv[:, h * (KD // 2):(h + 1) * (KD // 2), 1024 + blk * 512:1536 + blk * 512]), reads=[r_wb], writes=[r_w])
                        for ti in range(4):
                            tok0 = g * 512 + ti * 128
                            pm_, r_p = pM[cnt["o"] % 4]
                            o_, r_o = okv[cnt["o"] % 2]
                            o2_, r_o2 = omo[cnt["o"] % 2]
                            cnt["o"] += 1
                            for k in range(KD):
                                P.op("pe", lambda e, pm_=pm_, wt_=wt_, hTg=hTg, k=k, ti=ti: e.matmul(pm_[:, :], lhsT=hTg[:, k, ti * 128:(ti + 1) * 128], rhs=wt_[:, k, :], start=(k == 0), stop=(k == KD - 1)),
                                     reads=[r_w, r_hT], writes=[r_p])
                            if blk == 0:
                                P.op("dve", lambda e, pm_=pm_, o_=o_: e.tensor_copy(out=o_[:], in_=pm_[:, :]), reads=[r_p], writes=[r_o])
                                P.dma("sp", lambda e, o_=o_, tok0=tok0: e.dma_start(out=kv[tok0:tok0 + 128, :], in_=o_[:]), reads=[r_o], writes=[r_kv])
                            else:
                                P.op("act", lambda e, pm_=pm_, o2_=o2_: e.activation(out=o2_[:], in_=pm_[:, :], func=AF.Copy), reads=[r_p], writes=[r_o2])
                                P.op("dve", lambda e, o2_=o2_, o_=o_: e.tensor_copy(out=o_[:, 0:256], in_=o2_[:, 256:512]), reads=[r_o2], writes=[r_o])
                                P.dma("sp", lambda e, o2_=o2_, tok0=tok0: e.dma_start(out=mo[tok0:tok0 + 128, :], in_=o2_[:, 0:256]), reads=[r_o2], writes=[r_mo])
                                P.dma("sp", lambda e, o_=o_, tok0=tok0: e.dma_start(out=nv[tok0:tok0 + 128, :], in_=o_[:, 0:256]), reads=[r_o], writes=[r_nv])
                    if cfg.get('lim', 9) < 4:
                        continue
                    for ti in range(4):
                        tok0 = g * 512 + ti * 128
                        pm_, r_p = pM[cnt["o"] % 4]
                        o_, r_o = og[cnt["o"] % 2]
                        cnt["o"] += 1
                        for k in range(KD):
                            P.op("pe", lambda e, pm_=pm_, hTg=hTg, k=k, ti=ti: e.matmul(pm_[:, 0:4], lhsT=hTg[:, k, ti * 128:(ti + 1) * 128], rhs=wg[:, k, :], start=(k == 0), stop=(k == KD - 1)),
                                 reads=[r_wg, r_hT], writes=[r_p])
                        P.op("dve", lambda e, pm_=pm_, o_=o_: e.tensor_tensor(out=o_[:], in0=pm_[:, 0:4], in1=gbt[:], op=ALU.add), reads=[r_p, r_gbt], writes=[r_o])
                        P.dma("sp", lambda e, o_=o_, tok0=tok0: e.dma_start(out=gt[tok0:tok0 + 128, :], in_=o_[:]), reads=[r_o], writes=[r_gt])
                P.barrier()
                P.flush()
        if "C" in phases:
            gt2 = nc.dram_tensor("gt2", [NT, 4], F32, kind="Internal")
            r_gt2 = Res("gt2")
            with ExitStack() as st:
                C = Ctx(nc, st, P)
                idf, r_idf, idb, r_idb = make_ident(P, C)
                NB = NT // 128
                g_in, r_gin = C.sb("g_in", [128, NB, 4], F32)
                g_a, r_ga = C.sb("g_a", [128, NB, 4], F32)
                g_b, r_gb = C.sb("g_b", [128, NB, 4], F32)
                P.dma("sp", lambda e: e.dma_start(out=g_in[:], in_=gt[:, :].rearrange("(n p) c -> p n c", p=128)), reads=[r_gt], writes=[r_gin])
                P.op("act", lambda e: e.activation(out=g_a[:], in_=g_in[:], func=AF.Abs), reads=[r_gin], writes=[r_ga])
                P.op("act", lambda e: e.activation(out=g_a[:], in_=g_a[:], func=AF.Exp, scale=-1.0), reads=[r_ga], writes=[r_ga])
                P.op("act", lambda e: e.activation(out=g_a[:], in_=g_a[:], func=AF.Ln, bias=1.0), reads=[r_ga], writes=[r_ga])
                P.op("dve", lambda e: e.tensor_scalar(out=g_b[:], in0=g_in[:], scalar1=0.0, scalar2=None, op0=ALU.min), reads=[r_gin], writes=[r_gb])
                P.op("dve", lambda e: e.tensor_tensor(out=g_b[:], in0=g_b[:], in1=g_a[:], op=ALU.subtract), reads=[r_gb, r_ga], writes=[r_gb])
                for col in (0, 2):
                    P.op("dve", lambda e, col=col: e.tensor_copy(out=g_b[:, :, col:col + 1], in_=g_in[:, :, col:col + 1]), reads=[r_gin, r_gb], writes=[r_gb])
                P.dma("sp", lambda e: e.dma_start(out=gt2[:, :].rearrange("(n p) c -> p n c", p=128), in_=g_b[:]), reads=[r_gb], writes=[r_gt2])
                tri = {}
                mneg = {}
                sel = {}
                for dn in ("f", "b"):
                    t_, r_t = C.sb("tri" + dn, [64, 64], F32)
                    m_, r_m = C.sb("mneg" + dn, [64, 64], F32)
                    s_, r_s = C.sb("sel" + dn, [64, 128], F32)
                    sgn = 1 if dn == "f" else -1
                    P.op("pool", lambda e, t_=t_: e.memset(t_[:], 1.0), writes=[r_t])
                    P.op("pool", lambda e, t_=t_, sgn=sgn: e.affine_select(out=t_[:], in_=t_[:], pattern=[[sgn, 64]], compare_op=ALU.is_ge, fill=0.0, base=0, channel_multiplier=-sgn), reads=[r_t], writes=[r_t])
                    P.op("pool", lambda e, m_=m_: e.memset(m_[:], 0.0), writes=[r_m])
                    P.op("pool", lambda e, m_=m_, sgn=sgn: e.affine_select(out=m_[:], in_=m_[:], pattern=[[-sgn, 64]], compare_op=ALU.is_ge, fill=NEG, base=0, channel_multiplier=sgn), reads=[r_m], writes=[r_m])
                    last = 63 if dn == "f" else 0
                    P.op("pool", lambda e, s_=s_: e.memset(s_[:], 0.0), writes=[r_s])
                    P.op("pool", lambda e, s_=s_, last=last: e.affine_select(out=s_[:], in_=s_[:], pattern=[[0, 128]], compare_op=ALU.not_equal, fill=1.0, base=-last, channel_multiplier=1), reads=[r_s], writes=[r_s])
                    tri[dn], mneg[dn], sel[dn] = (t_, r_t), (m_, r_m), (s_, r_s)
                ones, r_ones = C.sb("ones64", [64, 64], F32)
                P.op("pool", lambda e: e.memset(ones[:], 1.0), writes=[r_ones])
                mngb, r_mngb = C.sb("mngb", [64, 256], F32)
                P.dma("sp", lambda e: e.dma_start(out=mngb[:], in_=mng[:, :].partition_broadcast(64)), writes=[r_mngb])
                hbk = nc.dram_tensor("hbk", [NT, 256], F32, kind="Internal")
                r_hbk = Res("hbk")
                KS = 256 ** -0.5
                fmv = fm[:, :, :].rearrange("h p t -> p h t")

                def mk_tiles(dn):
                    T = {}
                    T["Cs"] = C.sb(dn + "Cs", [128, 2, 257], F32)
                    T["Cb"] = C.sb(dn + "Cb", [128, 2, 257], BF16)
                    T["mcur"] = C.sb(dn + "mcur", [128, 1], F32)
                    for nm, shp, dt in (("gch", [64, 4], F32), ("qTc", [128, 2, 64], BF16), ("kTc", [128, 2, 64], BF16), ("kvt", [64, 512], BF16), ("hout", [64, 256], F32)):
                        T[nm] = [C.sb(dn + nm + str(i), shp, dt) for i in range(2)]
                    for nm in ("a", "cmax", "ws", "ecl", "wint", "em", "ad", "rc", "wsk"):
                        T[nm] = C.sb(dn + "sm_" + nm, [64, 1], F32)
                    T["sc2"] = C.sb(dn + "sc2", [64, 2], F32)
                    T["ml2"] = C.sb(dn + "ml2", [128, 2], F32)
                    T["nml2"] = C.sb(dn + "nml2", [128, 2], F32)
                    T["wold"] = C.sb(dn + "wold", [128, 1], F32)
                    T["diagA"] = C.sb(dn + "diagA", [64, 64], F32)
                    T["am"] = C.sb(dn + "am", [64, 64], F32)
                    T["PT"] = C.sb(dn + "PT", [64, 64], BF16)
                    T["vs"] = C.sb(dn + "vs", [64, 257], BF16)
                    T["H1"] = C.sb(dn + "H1", [64, 257], F32)
                    T["Hh"] = C.sb(dn + "Hh", [64, 257], F32)
                    T["psA"] = C.ps(dn + "psA", [128, 512], F32)
                    T["psI"] = C.ps(dn + "psI", [128, 512], F32)
                    T["psQ"] = C.ps(dn + "psQ", [128, 512], F32)
                    T["psU"] = C.ps(dn + "psU", [128, 512], F32)
                    return T

                def chain(dn, T):
                    icol, fcol = (0, 1) if dn == "f" else (2, 3)
                    tri_, r_tri = tri[dn]
                    mneg_, r_mneg = mneg[dn]
                    sel_, r_sel = sel[dn]
                    hdst, r_hdst = (hf, r_hf) if dn == "f" else (hbk, r_hbk)
                    Cs, r_Cs = T["Cs"]
                    Cb, r_Cb = T["Cb"]
                    mcur, r_mcur = T["mcur"]
                    a_, r_a = T["a"]
                    cm_, r_cm = T["cmax"]
                    ws_, r_ws = T["ws"]
                    ecl_, r_ecl = T["ecl"]
                    wi_, r_wi = T["wint"]
                    em_, r_em = T["em"]
                    ad_, r_ad = T["ad"]
                    rc_, r_rc = T["rc"]
                    wsk_, r_wsk = T["wsk"]
                    sc2, r_sc2 = T["sc2"]
                    ml2, r_ml2 = T["ml2"]
                    nml2, r_nml2 = T["nml2"]
                    wold, r_wold = T["wold"]
                    diagA, r_diagA = T["diagA"]
                    am, r_am = T["am"]
                    PT, r_PT = T["PT"]
                    vs, r_vs = T["vs"]
                    H1, r_H1 = T["H1"]
                    Hh, r_Hh = T["Hh"]
                    psA, r_psA = T["psA"]
                    psI, r_psI = T["psI"]
                    psQ, r_psQ = T["psQ"]
                    psU, r_psU = T["psU"]
                    it = 0
                    for (s0, sl) in seqs:
                        P.op("pool", lambda e: e.memset(Cs[:], 0.0), writes=[r_Cs])
                        P.op("pool", lambda e: e.memset(Cb[:], 0.0), writes=[r_Cb])
                        P.op("pool", lambda e: e.memset(mcur[:], 0.0), writes=[r_mcur])
                        yield
                        nch = sl // 64
                        order = range(nch) if dn == "f" else range(nch - 1, -1, -1)
                        for ch in order:
                            tok0 = s0 + ch * 64
                            g_, r_g = T["gch"][it % 2]
                            q_, r_q = T["qTc"][it % 2]
                            k_, r_k = T["kTc"][it % 2]
                            kv_, r_kvt = T["kvt"][it % 2]
                            ho_, r_ho = T["hout"][it % 2]
                            it += 1
                            P.dma("sp", lambda e, g_=g_, tok0=tok0: e.dma_start(out=g_[:], in_=gt2[tok0:tok0 + 64, :]), reads=[r_gt2], writes=[r_g])
                            P.dma("sp", lambda e, q_=q_, tok0=tok0: e.dma_start(out=q_[:], in_=fmv[:, 0:2, tok0:tok0 + 64]), reads=[r_fm], writes=[r_q])
                            P.dma("sp", lambda e, k_=k_, tok0=tok0: e.dma_start(out=k_[:], in_=fmv[:, 2:4, tok0:tok0 + 64]), reads=[r_fm], writes=[r_k])
                            P.dma("sp", lambda e, kv_=kv_, tok0=tok0: e.dma_start(out=kv_[:], in_=kv[tok0:tok0 + 64, :]), reads=[r_kv], writes=[r_kvt])
                            yield
                            ops = []
                            A = ops.append
                            A(("pe", lambda e, g_=g_: e.matmul(psA[0:64, 0:1], lhsT=tri_[:], rhs=g_[:, fcol:fcol + 1], start=True, stop=True), [r_tri, r_g], [r_psA]))
                            A(("dve", lambda e, g_=g_: e.tensor_tensor(out=a_[:], in0=g_[:, icol:icol + 1], in1=psA[0:64, 0:1], op=ALU.subtract), [r_g, r_psA], [r_a]))
                            A(("dve", lambda e: e.tensor_scalar(out=diagA[:], in0=idf[0:64, 0:64], scalar1=a_[:, 0:1], scalar2=None, op0=ALU.mult), [r_idf, r_a], [r_diagA]))
                            A(("pe", lambda e: e.matmul(psA[0:64, 64:128], lhsT=ones[:], rhs=diagA[:], start=True, stop=True), [r_ones, r_diagA], [r_psA]))
                            A(("dve", lambda e: e.tensor_tensor(out=am[:], in0=psA[0:64, 64:128], in1=mneg_[:], op=ALU.add), [r_psA, r_mneg], [r_am]))
                            A(("dve", lambda e: e.reduce_max(out=cm_[:], in_=am[:], axis=AX.X), [r_am], [r_cm]))
                            A(("dve", lambda e: e.tensor_tensor(out=sc2[:, 0:1], in0=cm_[:], in1=mcur[0:64, :], op=ALU.max), [r_cm, r_mcur], [r_sc2]))
                            A(("dve", lambda e: e.tensor_tensor(out=sc2[:, 1:2], in0=sc2[:, 0:1], in1=psA[0:64, 0:1], op=ALU.add), [r_sc2, r_psA], [r_sc2]))
                            A(("pe", lambda e: e.matmul(psA[:, 128:130], lhsT=sel_[:], rhs=sc2[:], start=True, stop=True), [r_sel, r_sc2], [r_psA]))
                            A(("dve", lambda e: e.tensor_copy(out=ml2[:], in_=psA[:, 128:130]), [r_psA], [r_ml2]))
                            A(("dve", lambda e: e.tensor_scalar(out=nml2[:], in0=ml2[:], scalar1=-1.0, scalar2=None, op0=ALU.mult), [r_ml2], [r_nml2]))
                            A(("act", lambda e: e.activation(out=ws_[:], in_=a_[:], func=AF.Exp, bias=nml2[0:64, 0:1]), [r_a, r_nml2], [r_ws]))
                            A(("act", lambda e: e.activation(out=ecl_[:], in_=sc2[:, 0:1], func=AF.Exp, scale=-1.0, bias=ml2[0:64, 0:1]), [r_sc2, r_ml2], [r_ecl]))
                            A(("act", lambda e: e.activation(out=wi_[:], in_=sc2[:, 0:1], func=AF.Exp, scale=-1.0, bias=mcur[0:64, :]), [r_sc2, r_mcur], [r_wi]))
                            A(("act", lambda e: e.activation(out=em_[:], in_=sc2[:, 1:2], func=AF.Exp, scale=-1.0), [r_sc2], [r_em]))
                            A(("act", lambda e: e.activation(out=wold[:], in_=mcur[:], func=AF.Exp, bias=nml2[:, 0:1]), [r_mcur, r_nml2], [r_wold]))
                            A(("dve", lambda e: e.tensor_scalar(out=wsk_[:], in0=ws_[:], scalar1=KS, scalar2=None, op0=ALU.mult), [r_ws], [r_wsk]))
                            A(("dve", lambda e, kv_=kv_: e.tensor_scalar(out=vs[:, 0:256], in0=kv_[:, 256:512], scalar1=wsk_[:, 0:1], scalar2=None, op0=ALU.mult), [r_kvt, r_wsk], [r_vs]))
                            A(("dve", lambda e: e.tensor_copy(out=vs[:, 256:257], in_=wsk_[:]), [r_wsk, r_vs], [r_vs]))
                            for hh in range(2):
                                A(("pe", lambda e, k_=k_, q_=q_, hh=hh: e.matmul(psA[0:64, 256:320], lhsT=k_[:, hh, :], rhs=q_[:, hh, :], start=(hh == 0), stop=(hh == 1)), [r_k, r_q], [r_psA]))
                            A(("dve", lambda e: e.tensor_tensor(out=PT[:], in0=psA[0:64, 256:320], in1=tri_[:], op=ALU.mult), [r_psA, r_tri], [r_PT]))
                            A(("pe", lambda e: e.matmul(psI[0:64, 0:257], lhsT=PT[:], rhs=vs[:], start=True, stop=True), [r_PT, r_vs], [r_psI]))
                            for hh in range(2):
                                A(("pe", lambda e, q_=q_, hh=hh: e.matmul(psQ[0:64, 0:257], lhsT=q_[:, hh, :], rhs=Cb[:, hh, :], start=(hh == 0), stop=(hh == 1)), [r_q, r_Cb], [r_psQ]))
                            A(("dve", lambda e: e.tensor_scalar(out=H1[:], in0=psQ[0:64, 0:257], scalar1=wi_[:, 0:1], scalar2=None, op0=ALU.mult), [r_psQ, r_wi], [r_H1]))
                            A(("dve", lambda e: e.scalar_tensor_tensor(out=Hh[:], in0=psI[0:64, 0:257], scalar=ecl_[:, 0:1], in1=H1[:], op0=ALU.mult, op1=ALU.add), [r_psI, r_ecl, r_H1], [r_Hh]))
                            A(("act", lambda e: e.activation(out=ad_[:], in_=Hh[:, 256:257], func=AF.Abs), [r_Hh], [r_ad]))
                            A(("dve", lambda e: e.tensor_tensor(out=ad_[:], in0=ad_[:], in1=em_[:], op=ALU.max), [r_ad, r_em], [r_ad]))
                            A(("dve", lambda e: e.reciprocal(out=rc_[:], in_=ad_[:]), [r_ad], [r_rc]))
                            A(("dve", lambda e, ho_=ho_: e.tensor_scalar(out=ho_[:], in0=Hh[:, 0:256], scalar1=rc_[:, 0:1], scalar2=None, op0=ALU.mult), [r_Hh, r_rc], [r_ho]))
                            for hh in range(2):
                                A(("pe", lambda e, kv_=kv_, hh=hh: e.matmul(psU[:, 0:257], lhsT=kv_[:, hh * 128:(hh + 1) * 128], rhs=vs[:], start=True, stop=True), [r_kvt, r_vs], [r_psU]))
                                A(("dve", lambda e, hh=hh: e.scalar_tensor_tensor(out=Cs[:, hh, :], in0=Cs[:, hh, :], scalar=wold[:, 0:1], in1=psU[:, 0:257], op0=ALU.mult, op1=ALU.add), [r_Cs, r_wold, r_psU], [r_Cs]))
                            A(("act", lambda e: e.activation(out=Cb[:], in_=Cs[:], func=AF.Copy), [r_Cs], [r_Cb]))
                            A(("dve", lambda e: e.tensor_copy(out=mcur[:], in_=ml2[:, 1:2]), [r_ml2, r_mcur], [r_mcur]))
                            for (en, fn, rd, wr) in ops:
                                P.op(en, fn, reads=rd, writes=wr)
                                yield
                            P.dma("sp", lambda e, ho_=ho_, tok0=tok0: e.dma_start(out=hdst[tok0:tok0 + 64, :], in_=ho_[:]), reads=[r_ho], writes=[r_hdst])
                            yield

                gens = [chain("f", mk_tiles("f")), chain("b", mk_tiles("b"))]
                alive = [True, True]
                while any(alive):
                    for gi in range(2):
                        if alive[gi]:
                            try:
                                next(gens[gi])
                            except StopIteration:
                                alive[gi] = False
                P.barrier()
                P.flush()
            with ExitStack() as st:
                C = Ctx(nc, st, P)
                mngb, r_mngb = C.sb("mngb2", [128, 256], F32)
                P.dma("sp", lambda e: e.dma_start(out=mngb[:], in_=mng[:, :].partition_broadcast(128)), writes=[r_mngb])
                ta = [C.sb("ca%d" % i, [128, 256], F32) for i in range(2)]
                tb = [C.sb("cb%d" % i, [128, 256], F32) for i in range(2)]
                tm = [C.sb("cm%d" % i, [128, 256], F32) for i in range(2)]
                to = [C.sb("co%d" % i, [128, 256], BF16) for i in range(2)]
                jq, r_jq = C.sb("cjq", [128, 256], F32)
                ssq_, r_ssq = C.sb("cssq", [128, 1], F32)
                rs_, r_rs = C.sb("crs", [128, 1], F32)
                for i in range(NT // 128):
                    tok0 = i * 128
                    a_, r_a = ta[i % 2]
                    b_, r_b = tb[i % 2]
                    m_, r_m = tm[i % 2]
                    o_, r_o = to[i % 2]
                    P.dma("sp", lambda e, a_=a_, tok0=tok0: e.dma_start(out=a_[:], in_=hf[tok0:tok0 + 128, :]), reads=[r_hf], writes=[r_a])
                    P.dma("sp", lambda e, b_=b_, tok0=tok0: e.dma_start(out=b_[:], in_=hbk[tok0:tok0 + 128, :]), reads=[r_hbk], writes=[r_b])
                    P.dma("sp", lambda e, m_=m_, tok0=tok0: e.dma_start(out=m_[:], in_=mo[tok0:tok0 + 128, :]), reads=[r_mo], writes=[r_m])
                    P.op("dve", lambda e, a_=a_, b_=b_: e.tensor_tensor(out=a_[:], in0=a_[:], in1=b_[:], op=ALU.add), reads=[r_a, r_b], writes=[r_a])
                    P.op("act", lambda e, a_=a_: e.activation(out=jq[:], in_=a_[:], func=AF.Square, accum_out=ssq_[:]), reads=[r_a], writes=[r_jq, r_ssq])
                    P.op("dve", lambda e: e.tensor_scalar(out=rs_[:], in0=ssq_[:], scalar1=1.0 / 256, scalar2=EPS, op0=ALU.mult, op1=ALU.add), reads=[r_ssq], writes=[r_rs])
                    P.op("act", lambda e: e.activation(out=rs_[:], in_=rs_[:], func=AF.Sqrt), reads=[r_rs], writes=[r_rs])
                    P.op("dve", lambda e: e.reciprocal(out=rs_[:], in_=rs_[:]), reads=[r_rs], writes=[r_rs])
                    P.op("act", lambda e, m_=m_: e.activation(out=m_[:], in_=m_[:], func=AF.Sigmoid), reads=[r_m], writes=[r_m])
                    P.op("dve", lambda e, a_=a_: e.scalar_tensor_tensor(out=a_[:], in0=a_[:], scalar=rs_[:, 0:1], in1=mngb[:], op0=ALU.mult, op1=ALU.mult), reads=[r_a, r_rs, r_mngb], writes=[r_a])
                    P.op("dve", lambda e, a_=a_, m_=m_, o_=o_: e.tensor_tensor(out=o_[:], in0=a_[:], in1=m_[:], op=ALU.mult), reads=[r_a, r_m], writes=[r_o])
                    P.dma("sp", lambda e, o_=o_, tok0=tok0: e.dma_start(out=mix[tok0:tok0 + 128, 0:256], in_=o_[:]), reads=[r_o], writes=[r_mix])
                P.barrier()
                P.flush()
        if "D" in phases:
            with ExitStack() as st:
                C = Ctx(nc, st, P)
                idf, r_idf, idb, r_idb = make_ident(P, C)
                Zt, r_Zt = C.sb("Zt", [64, 30, 64], F32)
                mk_, r_mk = C.sb("nmaskt", [64, 64], F32)
                P.dma("sp", lambda e: e.dma_start(out=mk_[:], in_=nmask[:, :]), writes=[r_mk])
                for qc in range(64):
                    P.dma("sp" if qc % 2 else "act", lambda e, qc=qc: e.dma_start(out=Zt[qc:qc + 1, :, :], in_=zall[:, 63 - qc:127 - qc].unsqueeze(0)), writes=[r_Zt])
                for i in range(30):
                    P.op("dve", lambda e, i=i: e.tensor_tensor(out=Zt[:, i, :], in0=Zt[:, i, :], in1=mk_[:], op=ALU.add), reads=[r_Zt, r_mk], writes=[r_Zt])
                Ztf = Zt[:].rearrange("p a b -> p (a b)")
                fmv = fm[:, :, :].rearrange("h p t -> p h t")
                NSC = 128 ** -0.5

                def na_chain(h):
                    pre = "h%d" % h
                    q2 = [C.sb(pre + "nq%d" % i, [128, 64], BF16) for i in range(2)]
                    k2 = [C.sb(pre + "nk%d" % i, [128, 512], BF16) for i in range(2)]
                    v2 = [C.sb(pre + "nvv%d" % i, [128, 4, 128], BF16) for i in range(2)]
                    sbt = [C.sb(pre + "nsb%d" % i, [64, 512], F32) for i in range(2)]
                    pb = [C.sb(pre + "npb%d" % i, [64, 512], BF16) for i in range(2)]
                    pTs = [C.sb(pre + "npT%d" % i, [128, 4, 64], BF16) for i in range(2)]
                    onb = [C.sb(pre + "nob%d" % i, [64, 128], BF16) for i in range(2)]
                    mxs = [C.sb(pre + "nmx%d" % i, [64, 1], F32) for i in range(2)]
                    rss = [C.sb(pre + "nrs%d" % i, [64, 1], F32) for i in range(2)]
                    psS = [C.ps(pre + "npsS%d" % i, [128, 512], F32) for i in range(2)]
                    psT, r_pT = C.ps(pre + "npsT", [128, 1024], BF16)
                    psO, r_pO = C.ps(pre + "npsO", [128, 512], F32)
                    it = 0
                    for (s0, sl) in seqs:
                        rows = sl // 64
                        for r in range(rows):
                            rs = min(max(r - 4, 0), rows - 8)
                            rho0 = 3 - ((r - 4) - rs)
                            tq = s0 + r * 64
                            tk = s0 + rs * 64
                            q_, r_q = q2[it % 2]
                            k_, r_k = k2[it % 2]
                            v_, r_v = v2[it % 2]
                            ob_, r_ob = onb[it % 2]
                            pS_, r_pS = psS[it % 2]
                            sb_, r_sb = sbt[it % 2]
                            p_, r_p = pb[it % 2]
                            pt_, r_pt = pTs[it % 2]
                            mx, r_mx = mxs[it % 2]
                            rsum, r_rsum = rss[it % 2]
                            it += 1
                            P.dma("sp", lambda e, q_=q_, tq=tq: e.dma_start(out=q_[:], in_=fm[4 + h, :, tq:tq + 64]), reads=[r_fm], writes=[r_q])
                            P.dma("sp", lambda e, k_=k_, tk=tk: e.dma_start(out=k_[:], in_=fm[6 + h, :, tk:tk + 512]), reads=[r_fm], writes=[r_k])
                            P.dma("sp", lambda e, v_=v_, tk=tk: e.dma_start(out=v_[:], in_=nv[tk:tk + 512, h * 128:(h + 1) * 128].rearrange("(c p) e -> p c e", p=128)), reads=[r_nv], writes=[r_v])
                            yield
                            b0 = (h * 15 + rho0) * 64
                            ops = []
                            A = ops.append
                            A(("pe", lambda e, pS_=pS_, q_=q_, k_=k_: e.matmul(pS_[0:64, :], lhsT=q_[:], rhs=k_[:], start=True, stop=True), [r_q, r_k], [r_pS]))
                            A(("dve", lambda e, pS_=pS_, sb_=sb_, b0=b0: e.scalar_tensor_tensor(out=sb_[:], in0=pS_[0:64, :], scalar=NSC, in1=Ztf[:, b0:b0 + 512], op0=ALU.mult, op1=ALU.add), [r_pS, r_Zt], [r_sb]))
                            A(("dve", lambda e, sb_=sb_, mx=mx: e.reduce_max(out=mx[:], in_=sb_[:], axis=AX.X), [r_sb], [r_mx]))
                            A(("dve", lambda e, mx=mx: e.tensor_scalar(out=mx[:], in0=mx[:], scalar1=-1.0, scalar2=None, op0=ALU.mult), [r_mx], [r_mx]))
                            A(("act", lambda e, sb_=sb_, p_=p_, mx=mx, rsum=rsum: e.activation(out=p_[:], in_=sb_[:], func=AF.Exp, bias=mx[:, 0:1], accum_out=rsum[:]), [r_sb, r_mx], [r_p, r_rsum]))
                            for c4 in range(4):
                                A(("pe", lambda e, p_=p_, c4=c4: e.transpose(out=psT[:, c4 * 64:(c4 + 1) * 64], in_=p_[:, c4 * 128:(c4 + 1) * 128], identity=idb[0:64, 0:64]), [r_p, r_idb], [r_pT]))
                            A(("act", lambda e, pt_=pt_: e.activation(out=pt_[:].rearrange("p a b -> p (a b)"), in_=psT[:, 0:256], func=AF.Copy), [r_pT], [r_pt]))
                            for c4 in range(4):
                                A(("pe", lambda e, pt_=pt_, v_=v_, c4=c4: e.matmul(psO[0:64, 0:128], lhsT=pt_[:, c4, :], rhs=v_[:, c4, :], start=(c4 == 0), stop=(c4 == 3)), [r_pt, r_v], [r_pO]))
                            A(("dve", lambda e, rsum=rsum: e.reciprocal(out=rsum[:], in_=rsum[:]), [r_rsum], [r_rsum]))
                            A(("dve", lambda e, ob_=ob_, rsum=rsum: e.tensor_scalar(out=ob_[:], in0=psO[0:64, 0:128], scalar1=rsum[:, 0:1], scalar2=None, op0=ALU.mult), [r_pO, r_rsum], [r_ob]))
                            for (en, fn, rd, wr) in ops:
                                P.op(en, fn, reads=rd, writes=wr)
                                yield
                            P.dma("sp", lambda e, ob_=ob_, tq=tq: e.dma_start(out=mix[tq:tq + 64, 256 + h * 128:256 + (h + 1) * 128], in_=ob_[:]), reads=[r_ob], writes=[r_mix])
                            yield

                gens = [na_chain(0), na_chain(1)]
                alive = [True, True]
                while any(alive):
                    for gi in range(2):
                        if alive[gi]:
                            try:
                                next(gens[gi])
                            except StopIteration:
                                alive[gi] = False
                P.barrier()
                P.flush()
    return nc


def build_l2(cfg, ntile_lim=None):
    D = cfg["D"]
    KD = D // 128
    NK = cfg["NKEYS"]
    NE = NK * NK
    NCORE = cfg["NC"]
    TPp = cfg["SEQ"] // NCORE
    TPC = TPp + cfg["DB"] * cfg["DS"] // NCORE
    ntile = TPC // 128
    nc = bass.Bass("TRN2", target_bir_lowering=False)
    x = nc.dram_tensor("x", [TPC, D], F32, kind="ExternalInput")
    mixo = nc.dram_tensor("mixo", [TPC, D], BF16, kind="ExternalInput")
    wo = nc.dram_tensor("wo", [D, D], F32, kind="ExternalInput")
    wq = nc.dram_tensor("wq", [D, 2048], F32, kind="ExternalInput")
    g1o = nc.dram_tensor("g1o", [2, D], F32, kind="ExternalInput")
    g2o = nc.dram_tensor("g2o", [2, D], F32, kind="ExternalInput")
    sc2o = nc.dram_tensor("sc2o", [2, D], F32, kind="ExternalInput")
    sh2o = nc.dram_tensor("sh2o", [2, D], F32, kind="ExternalInput")
    n2g = nc.dram_tensor("n2g", [1, D], F32, kind="ExternalInput")
    fg = nc.dram_tensor("fg", [1, D], F32, kind="ExternalInput")
    kTd = nc.dram_tensor("kT", [128, 16, NK], F32, kind="ExternalInput")
    u = nc.dram_tensor("u", [NE, D], F32, kind="ExternalInput")
    v = nc.dram_tensor("v", [NE, D], F32, kind="ExternalInput")
    io256 = nc.dram_tensor("io256", [1, 256], F32, kind="ExternalInput")
    y = nc.dram_tensor("y", [TPC, D], F32, kind="ExternalOutput")
    wob = nc.dram_tensor("wob", [D, D], BF16, kind="Internal")
    wqb = nc.dram_tensor("wqb", [D, 2048], BF16, kind="Internal")
    sc2s = nc.dram_tensor("sc2s", [2, D], F32, kind="Internal")
    h2d = nc.dram_tensor("h2d", [TPC, D], BF16, kind="Internal")
    ub = nc.dram_tensor("ub", [NE, D], BF16, kind="Internal")
    vbd = nc.dram_tensor("vbd", [NE, D], BF16, kind="Internal")
    r_y, r_wob, r_wqb, r_sc2s, r_h2d, r_ub, r_vbd = [Res(n) for n in ("y", "wob", "wqb", "sc2s", "h2d", "ub", "vbd")]
    wobv = wob[:, :].rearrange("(k p) n -> p k n", p=128)
    wqbv = wqb[:, :].rearrange("(k p) n -> p k n", p=128)
    with ExitStack() as st0:
        P = Prog(nc, st0)
        with ExitStack() as st:
            C = Ctx(nc, st, P)
            wf = [C.sb("wf%d" % i, [128, D], F32) for i in range(2)]
            wb_ = [C.sb("wb%d" % i, [128, D], BF16) for i in range(2)]
            n = 0
            for (src, dst, r_dst, ncols, nblk) in ((wo, wob, r_wob, D, KD), (wq, wqb, r_wqb, 2048, KD), (u, ub, r_ub, D, NE // 128), (v, vbd, r_vbd, D, NE // 128)):
                for k in range(nblk):
                    a, r_a = wf[n % 2]
                    o, r_o = wb_[n % 2]
                    P.dma("sp" if n % 2 else "pool", lambda e, a=a, k=k, src=src, ncols=ncols: e.dma_start(out=a[:, 0:ncols], in_=src[k * 128:(k + 1) * 128, :]), writes=[r_a])
                    if n % 2:
                        P.op("act", lambda e, a=a, o=o, ncols=ncols: e.activation(out=o[:, 0:ncols], in_=a[:, 0:ncols], func=AF.Copy), reads=[r_a], writes=[r_o])
                    else:
                        P.op("dve", lambda e, a=a, o=o, ncols=ncols: e.tensor_copy(out=o[:, 0:ncols], in_=a[:, 0:ncols]), reads=[r_a], writes=[r_o])
                    P.dma("sp", lambda e, o=o, k=k, dst=dst, ncols=ncols: e.dma_start(out=dst[k * 128:(k + 1) * 128, :], in_=o[:, 0:ncols]), reads=[r_o], writes=[r_dst])
                    n += 1
            s2, r_s2 = C.sb("s2", [2, D], F32)
            n2, r_n2 = C.sb("n2", [2, D], F32)
            P.dma("sp", lambda e: e.dma_start(out=s2[:], in_=sc2o[:, :]), writes=[r_s2])
            P.dma("sp", lambda e: e.dma_start(out=n2[:], in_=n2g[:, :].partition_broadcast(2)), writes=[r_n2])
            P.op("dve", lambda e: e.scalar_tensor_tensor(out=s2[:], in0=s2[:], scalar=1.0, in1=n2[:], op0=ALU.add, op1=ALU.mult), reads=[r_s2, r_n2], writes=[r_s2])
            P.dma("sp", lambda e: e.dma_start(out=sc2s[:, :], in_=s2[:]), reads=[r_s2], writes=[r_sc2s])
            P.barrier()
            P.flush()
        with ExitStack() as st:
            C = Ctx(nc, st, P)
            idf, r_idf, idb, r_idb = make_ident(P, C)
            xt, r_xt = C.sb("xt", [128, D], F32)
            t8, r_t8 = C.sb("t8", [128, D], BF16)
            tT, r_tT = C.sb("tT", [128, KD, 128], BF16)
            wblk = [C.sb("wblk%d" % i, [128, KD, 256], BF16) for i in range(2)]
            bc = [C.sb("bc%d" % i, [128, D], F32) for i in range(1)]
            uv = [C.sb("uv%d" % i, [128, D], F32) for i in range(2)]
            NBUF = 3
            ubt = [C.sb("ubt%d" % i, [128, D], BF16) for i in range(NBUF)]
            vbt = [C.sb("vbt%d" % i, [128, D], BF16) for i in range(NBUF)]
            hb = [C.sb("hb%d" % i, [128, D], BF16) for i in range(2)]
            junk, r_junk = t8, r_t8
            tmpf = [C.sb("tmpf%d" % i, [128, 512], F32) for i in range(2)]
            qT, r_qT = uv[1][0][:, 0:2048].rearrange("p (a b) -> p a b", a=16), uv[1][1]
            scs, r_scs = uv[1][0][:, 2048:2048 + 16 * NK].rearrange("p (a b) -> p a b", a=16), uv[1][1]
            kTt, r_kTt = C.sb("kTt", [128, 16, NK], F32)
            wk, r_wk = C.sb("wk", [128, 256], F32)
            v16, r_v16 = C.sb("v16", [128, 16, 16], F32)
            i16u, r_i16u = C.sb("i16u", [128, 16, 16], U32)
            i16f, r_i16f = C.sb("i16f", [128, 16, 16], F32)
            i1s, r_i1s = C.sb("i1s", [128, 16], F32)
            cand, r_cand = C.sb("cand", [128, 16, 16], F32)
            Eh, r_Eh = C.sb("Eh", [128, 16, 16], F32)
            sc16, r_sc16 = C.sb("sc16", [128, 8, 16], F32)
            ciu, r_ciu = C.sb("ciu", [128, 16], U32)
            cif, r_cif = C.sb("cif", [128, 16], F32)
            io, r_io = C.sb("io", [128, 256], F32)
            eid, r_eid = C.sb("eid", [128, 128], F32)
            gw, r_gw = C.sb("gw", [128, 8, 16], F32)
            nmx, r_nmx = C.sb("nmx", [128, 8], F32)
            gsum, r_gsum = C.sb("gsum", [128, 8], F32)
            idxT, r_idxT = C.sb("idxT", [128, 128], I32)
            gwT, r_gwT = C.sb("gwT", [128, 128], F32)
            ACTT, r_ACTT = C.sb("ACTT", [128, 128], F32)
            Wt, r_Wt = C.sb("Wt", [128, 128], F32)
            gl, r_gl = C.sb("gl", [128, 128], F32)
            Wsel = [C.sb("Wsel%d" % i, [128, 128], BF16) for i in range(2)]
            sm_ = {n_: C.sb("sm_" + n_, [128, 1], F32) for n_ in ("x2", "u", "sg", "w")}
            Zc, r_Zc = C.sb("Zc", [128, 255], F32)
            ssq, r_ssq = C.sb("ssq", [128, 1], F32)
            rstd, r_rstd = C.sb("rstd", [128, 1], F32)
            ps = [C.ps("ps%d" % i, [128, 512], F32) for i in range(8)]
            P.op("pool", lambda e: e.memset(Zc[:], 0.0), writes=[r_Zc])
            P.op("pool", lambda e: e.memset(Zc[:, 127:128], 1.0), reads=[r_Zc], writes=[r_Zc])
            P.dma("sp", lambda e: e.dma_start(out=io[:], in_=io256[:, :].partition_broadcast(128)), writes=[r_io])
            P.dma("sp", lambda e: e.dma_start(out=kTt[:], in_=kTd[:, :, :]), writes=[r_kTt])
            cnt = dict(w=0, b=0, bc=0, uv=0, hb=0, t=0, ws=0)

            def rmsn(src_ap_fn):
                P.op("act", lambda e: e.activation(out=t8[:], in_=xt[:], func=AF.Square, accum_out=ssq[:]), reads=[r_xt], writes=[r_t8, r_ssq])
                P.op("dve", lambda e: e.tensor_scalar(out=rstd[:], in0=ssq[:], scalar1=1.0 / D, scalar2=EPS, op0=ALU.mult, op1=ALU.add), reads=[r_ssq], writes=[r_rstd])
                P.op("act", lambda e: e.activation(out=rstd[:], in_=rstd[:], func=AF.Sqrt), reads=[r_rstd], writes=[r_rstd])
                P.op("dve", lambda e: e.reciprocal(out=rstd[:], in_=rstd[:]), reads=[r_rstd], writes=[r_rstd])

            def transposes():
                for q in range(KD // 8):
                    pb_, r_pb = ps[6 + (q % 2)]
                    pv = pb_[:].bitcast(BF16)
                    for j in range(8):
                        k = q * 8 + j
                        P.op("pe", lambda e, pv=pv, j=j, k=k: e.transpose(out=pv[:, j * 128:(j + 1) * 128], in_=t8[:, k * 128:(k + 1) * 128], identity=idb[:]), reads=[r_t8, r_idb], writes=[r_pb])
                    if q % 2:
                        P.op("act", lambda e, pv=pv, q=q: e.activation(out=tT[:, q * 8:(q + 1) * 8, :].rearrange("p a b -> p (a b)"), in_=pv[:, :], func=AF.Copy), reads=[r_pb], writes=[r_tT])
                    else:
                        P.op("dve", lambda e, pv=pv, q=q: e.tensor_copy(out=tT[:, q * 8:(q + 1) * 8, :].rearrange("p a b -> p (a b)"), in_=pv[:, :]), reads=[r_pb], writes=[r_tT])

            def load_bc(src_ap):
                b_, r_b = bc[0]
                cnt["bc"] += 1
                P.dma("act", lambda e, b_=b_: e.dma_start(out=b_[:], in_=src_ap.partition_broadcast(128)), writes=[r_b])
                return b_, r_b

            def top16(src_ap, r_src, vout, r_vout, iout, r_iout, n):
                wkv = wk[:, 0:n]
                P.op("dve", lambda e: e.max(out=vout[:, 0:8], in_=src_ap), reads=[r_src], writes=[r_vout])
                P.op("dve", lambda e: e.max_index(out=iout[:, 0:8], in_max=vout[:, 0:8], in_values=src_ap), reads=[r_src, r_vout], writes=[r_iout])
                P.op("dve", lambda e: e.match_replace(out=wkv, in_to_replace=vout[:, 0:8], in_values=src_ap, imm_value=-1e30), reads=[r_src, r_vout], writes=[r_wk])
                P.op("dve", lambda e: e.max(out=vout[:, 8:16], in_=wkv), reads=[r_wk], writes=[r_vout])
                P.op("dve", lambda e: e.max_index(out=iout[:, 8:16], in_max=vout[:, 8:16], in_values=wkv), reads=[r_wk, r_vout], writes=[r_iout])

            nt_run = ntile if ntile_lim is None else ntile_lim
            for ti in range(nt_run):
                tok0 = ti * 128
                s = 0 if tok0 < TPp else 1
                for h4 in range(4):
                    P.dma("sp", lambda e, tok0=tok0, h4=h4: e.dma_start(out=xt[:, h4 * (D // 4):(h4 + 1) * (D // 4)], in_=x[tok0:tok0 + 128, h4 * (D // 4):(h4 + 1) * (D // 4)]), writes=[r_xt])
                P.dma("sp", lambda e, tok0=tok0: e.dma_start(out=t8[:], in_=mixo[tok0:tok0 + 128, :]), writes=[r_t8])
                transposes()
                b1, r_b1 = load_bc(g1o[s:s + 1, :])
                for cb in range(D // 256):
                    w_, r_w = wblk[cnt["w"] % 2]
                    cnt["w"] += 1
                    P.dma("sp", lambda e, w_=w_, cb=cb: e.dma_start(out=w_[:], in_=wobv[:, :, cb * 256:(cb + 1) * 256]), reads=[r_wob], writes=[r_w])
                    p_, r_p = ps[cnt["b"] % 6]
                    tf_, r_tf = tmpf[cnt["b"] % 2]
                    cnt["b"] += 1
                    for k in range(KD):
                        P.op("pe", lambda e, p_=p_, w_=w_, k=k: e.matmul(p_[:, 0:256], lhsT=tT[:, k, :], rhs=w_[:, k, :], start=(k == 0), stop=(k == KD - 1)), reads=[r_tT, r_w], writes=[r_p])
                    P.op("dve", lambda e, p_=p_, tf_=tf_, cb=cb, b1=b1: e.tensor_tensor(out=tf_[:, 0:256], in0=p_[:, 0:256], in1=b1[:, cb * 256:(cb + 1) * 256], op=ALU.mult), reads=[r_p, r_b1], writes=[r_tf])
                    P.op("pool", lambda e, tf_=tf_, cb=cb: e.tensor_tensor(out=xt[:, cb * 256:(cb + 1) * 256], in0=xt[:, cb * 256:(cb + 1) * 256], in1=tf_[:, 0:256], op=ALU.add), reads=[r_tf, r_xt], writes=[r_xt])
                rmsn(None)
                b2, r_b2 = load_bc(sc2s[s:s + 1, :])
                u0, r_u0 = uv[0]
                P.op("dve", lambda e, b2=b2: e.scalar_tensor_tensor(out=u0[:], in0=xt[:], scalar=rstd[:, 0:1], in1=b2[:], op0=ALU.mult, op1=ALU.mult), reads=[r_xt, r_rstd, r_b2], writes=[r_u0])
                b3, r_b3 = load_bc(sh2o[s:s + 1, :])
                P.op("dve", lambda e, b3=b3: e.tensor_tensor(out=t8[:], in0=u0[:], in1=b3[:], op=ALU.add), reads=[r_u0, r_b3], writes=[r_t8])
                P.dma("sp", lambda e, tok0=tok0: e.dma_start(out=h2d[tok0:tok0 + 128, :], in_=t8[:]), reads=[r_t8], writes=[r_h2d])
                transposes()
                for j4 in range(8):
                    w_, r_w = wblk[cnt["w"] % 2]
                    cnt["w"] += 1
                    P.dma("sp", lambda e, w_=w_, j4=j4: e.dma_start(out=w_[:], in_=wqbv[:, :, j4 * 256:(j4 + 1) * 256]), reads=[r_wqb], writes=[r_w])
                    for jj in range(2):
                        j = j4 * 2 + jj
                        p_, r_p = ps[cnt["b"] % 6]
                        cnt["b"] += 1
                        for k in range(KD):
                            P.op("pe", lambda e, p_=p_, w_=w_, k=k, jj=jj: e.matmul(p_[:, 0:128], lhsT=w_[:, k, jj * 128:(jj + 1) * 128], rhs=tT[:, k, :], start=(k == 0), stop=(k == KD - 1)), reads=[r_tT, r_w], writes=[r_p])
                        P.op("act", lambda e, p_=p_, j=j: e.activation(out=qT[:, j, :], in_=p_[:, 0:128], func=AF.Copy), reads=[r_p], writes=[r_qT])
                for j in range(16):
                    p_, r_p = ps[cnt["b"] % 6]
                    cnt["b"] += 1
                    P.op("pe", lambda e, p_=p_, j=j: e.matmul(p_[:, 0:NK], lhsT=qT[:, j, :], rhs=kTt[:, j, :], start=True, stop=True), reads=[r_qT, r_kTt], writes=[r_p])
                    P.op("dve", lambda e, p_=p_, j=j: e.tensor_copy(out=scs[:, j, :], in_=p_[:, 0:NK]), reads=[r_p], writes=[r_scs])
                for j in range(16):
                    top16(scs[:, j, :], r_scs, v16[:, j, :], r_v16, i16u[:, j, :], r_i16u, NK)
                P.op("dve", lambda e: e.tensor_copy(out=i16f[:], in_=i16u[:]), reads=[r_i16u], writes=[r_i16f])
                u0v = u0[:].rearrange("p (a b) -> p a b", a=16)
                for h in range(8):
                    j1, j2 = 2 * h, 2 * h + 1
                    P.op("dve", lambda e, j1=j1, j2=j2: e.tensor_tensor(out=cand[:], in0=v16[:, j1, :].unsqueeze(2).to_broadcast([128, 16, 16]), in1=v16[:, j2, :].unsqueeze(1).to_broadcast([128, 16, 16]), op=ALU.add), reads=[r_v16], writes=[r_cand])
                    P.op("dve", lambda e, j1=j1: e.tensor_scalar(out=i1s[:], in0=i16f[:, j1, :], scalar1=float(NK), scalar2=None, op0=ALU.mult), reads=[r_i16f], writes=[r_i1s])
                    P.op("dve", lambda e, j2=j2: e.tensor_tensor(out=Eh[:], in0=i1s[:].unsqueeze(2).to_broadcast([128, 16, 16]), in1=i16f[:, j2, :].unsqueeze(1).to_broadcast([128, 16, 16]), op=ALU.add), reads=[r_i1s, r_i16f], writes=[r_Eh])
                    top16(cand[:].rearrange("p a b -> p (a b)"), r_cand, sc16[:, h, :], r_sc16, ciu[:], r_ciu, 256)
                    P.op("dve", lambda e: e.tensor_copy(out=cif[:], in_=ciu[:]), reads=[r_ciu], writes=[r_cif])
                    P.op("dve", lambda e: e.tensor_tensor(out=u0v, in0=cif[:].unsqueeze(2).to_broadcast([128, 16, 256]), in1=io[:].unsqueeze(1).to_broadcast([128, 16, 256]), op=ALU.is_equal), reads=[r_cif, r_io], writes=[r_u0])
                    P.op("dve", lambda e: e.tensor_tensor(out=u0v, in0=u0v, in1=Eh[:].rearrange("p a b -> p (a b)").unsqueeze(1).to_broadcast([128, 16, 256]), op=ALU.mult), reads=[r_u0, r_Eh], writes=[r_u0])
                    P.op("dve", lambda e, h=h: e.reduce_sum(out=eid[:, h * 16:(h + 1) * 16], in_=u0v, axis=AX.X), reads=[r_u0], writes=[r_eid])
                P.op("dve", lambda e: e.tensor_scalar(out=nmx[:], in0=sc16[:, :, 0], scalar1=-1.0, scalar2=None, op0=ALU.mult), reads=[r_sc16], writes=[r_nmx])
                P.op("dve", lambda e: e.tensor_tensor(out=gw[:], in0=sc16[:], in1=nmx[:].unsqueeze(2).to_broadcast([128, 8, 16]), op=ALU.add), reads=[r_sc16, r_nmx], writes=[r_gw])
                P.op("act", lambda e: e.activation(out=gw[:], in_=gw[:], func=AF.Exp), reads=[r_gw], writes=[r_gw])
                P.op("dve", lambda e: e.reduce_sum(out=gsum[:], in_=gw[:], axis=AX.X), reads=[r_gw], writes=[r_gsum])
                P.op("dve", lambda e: e.reciprocal(out=gsum[:], in_=gsum[:]), reads=[r_gsum], writes=[r_gsum])
                P.op("dve", lambda e: e.tensor_tensor(out=gw[:], in0=gw[:], in1=gsum[:].unsqueeze(2).to_broadcast([128, 8, 16]), op=ALU.mult), reads=[r_gw, r_gsum], writes=[r_gw])
                p_, r_p = ps[cnt["b"] % 6]
                cnt["b"] += 1
                P.op("pe", lambda e, p_=p_: e.transpose(out=p_[:, 0:128], in_=eid[:], identity=idf[:]), reads=[r_eid, r_idf], writes=[r_p])
                P.op("pe", lambda e, p_=p_: e.transpose(out=p_[:, 128:256], in_=gw[:].rearrange("p a b -> p (a b)"), identity=idf[:]), reads=[r_gw, r_idf], writes=[r_p])
                P.op("dve", lambda e, p_=p_: e.tensor_copy(out=idxT[:], in_=p_[:, 0:128]), reads=[r_p], writes=[r_idxT])
                P.op("dve", lambda e, p_=p_: e.tensor_copy(out=gwT[:], in_=p_[:, 128:256]), reads=[r_p], writes=[r_gwT])
                x2_, r_x2 = sm_["x2"]
                uu_, r_uu = sm_["u"]
                sg_, r_sg = sm_["sg"]
                w1_, r_w1 = sm_["w"]
                for t in range(128):
                    U_, r_U = ubt[t % NBUF]
                    V_, r_V = vbt[t % NBUF]
                    H_, r_H = hb[t % 2]
                    ws_, r_ws = Wsel[t % 2]
                    P.dma("pool", lambda e, U_=U_, t=t: e.indirect_dma_start(out=U_[:], out_offset=None, in_=ub[:, :], in_offset=bass.IndirectOffsetOnAxis(ap=idxT[:, t:t + 1], axis=0)), reads=[r_idxT, r_ub], writes=[r_U])
                    P.dma("sp", lambda e, H_=H_, t=t, tok0=tok0: e.dma_start(out=H_[:], in_=h2d[tok0 + t:tok0 + t + 1, :].partition_broadcast(128)), reads=[r_h2d], writes=[r_H])
                    P.dma("pool", lambda e, V_=V_, t=t: e.indirect_dma_start(out=V_[:], out_offset=None, in_=vbd[:, :], in_offset=bass.IndirectOffsetOnAxis(ap=idxT[:, t:t + 1], axis=0)), reads=[r_idxT, r_vbd], writes=[r_V])
                    P.op("dve", lambda e, U_=U_, H_=H_, t=t: e.scalar_tensor_tensor(out=U_[:], in0=U_[:], scalar=1.0, in1=H_[:], op0=ALU.mult, op1=ALU.mult, accum_out=ACTT[:, t:t + 1]), reads=[r_U, r_H], writes=[r_U, r_ACTT])
                    P.op("dve", lambda e, t=t: e.tensor_tensor(out=x2_[:], in0=ACTT[:, t:t + 1], in1=ACTT[:, t:t + 1], op=ALU.mult), reads=[r_ACTT], writes=[r_x2])
                    P.op("dve", lambda e: e.tensor_scalar(out=x2_[:], in0=x2_[:], scalar1=0.044715, scalar2=1.0, op0=ALU.mult, op1=ALU.add), reads=[r_x2], writes=[r_x2])
                    P.op("dve", lambda e, t=t: e.tensor_tensor(out=uu_[:], in0=x2_[:], in1=ACTT[:, t:t + 1], op=ALU.mult), reads=[r_x2, r_ACTT], writes=[r_uu])
                    P.op("act", lambda e: e.activation(out=sg_[:], in_=uu_[:], func=AF.Sigmoid, scale=1.5957691216057308), reads=[r_uu], writes=[r_sg])
                    P.op("dve", lambda e, t=t: e.scalar_tensor_tensor(out=w1_[:], in0=sg_[:], scalar=ACTT[:, t:t + 1], in1=gwT[:, t:t + 1], op0=ALU.mult, op1=ALU.mult), reads=[r_sg, r_ACTT, r_gwT], writes=[r_w1])
                    P.op("act", lambda e, ws_=ws_, t=t: e.activation(out=ws_[:], in_=Zc[:, 127 - t:255 - t], func=AF.Copy, scale=w1_[:, 0:1]), reads=[r_Zc, r_w1], writes=[r_ws])
                    for cb in range(8):
                        p_, r_p = ps[cb]
                        P.op("pe", lambda e, p_=p_, ws_=ws_, V_=V_, cb=cb, t=t: e.matmul(p_[:, :], lhsT=ws_[:], rhs=V_[:, cb * 512:(cb + 1) * 512], start=(t == 0), stop=(t == 127)), reads=[r_ws, r_V], writes=[r_p])
                b4, r_b4 = load_bc(g2o[s:s + 1, :])
                for cb in range(8):
                    p_, r_p = ps[cb]
                    tf_, r_tf = tmpf[cb % 2]
                    P.op("dve", lambda e, p_=p_, tf_=tf_, cb=cb, b4=b4: e.tensor_tensor(out=tf_[:], in0=p_[:, :], in1=b4[:, cb * 512:(cb + 1) * 512], op=ALU.mult), reads=[r_p, r_b4], writes=[r_tf])
                    P.op("pool", lambda e, tf_=tf_, cb=cb: e.tensor_tensor(out=xt[:, cb * 512:(cb + 1) * 512], in0=xt[:, cb * 512:(cb + 1) * 512], in1=tf_[:], op=ALU.add), reads=[r_tf, r_xt], writes=[r_xt])
                rmsn(None)
                b5, r_b5 = load_bc(fg[0:1, :])
                u1, r_u1 = uv[1]
                P.op("dve", lambda e, b5=b5: e.scalar_tensor_tensor(out=u1[:], in0=xt[:], scalar=rstd[:, 0:1], in1=b5[:], op0=ALU.mult, op1=ALU.mult), reads=[r_xt, r_rstd, r_b5], writes=[r_u1])
                P.dma("sp", lambda e, tok0=tok0: e.dma_start(out=y[tok0:tok0 + 128, :], in_=u1[:]), reads=[r_u1], writes=[r_y])
            P.barrier()
            P.flush()
    return nc


def _T(a, KD):
    return np.ascontiguousarray(a.T.reshape(KD, 128, -1).transpose(1, 0, 2))


def _w_own(w_in, c, D):
    mw = D // 2
    g0 = 4 * mw
    n0 = g0 + 32
    sl = lambda base: w_in[:, base + c * 256: base + (c + 1) * 256]
    gates = w_in[:, [g0 + g * 8 + c for g in range(4)]]
    return np.ascontiguousarray(np.concatenate(
        [sl(0), sl(mw), sl(n0), sl(n0 + mw), sl(mw), sl(2 * mw), sl(3 * mw), sl(n0 + 2 * mw), gates], axis=1))


def _na_consts(rpb2):
    z = np.zeros((2, 15, 127), np.float32)
    z[:, :, 48:79] = rpb2
    cidx = np.arange(64)
    cs = np.clip(cidx - 8, 0, 48)
    ok = (cidx[None, :] >= cs[:, None]) & (cidx[None, :] < cs[:, None] + 16)
    m = np.full((64, 64), NEG, np.float32)
    m[ok] = 0.0
    return z.reshape(30, 127), m


def kernel(x_prompt, x_sample, c_prompt, c_sample, ada_w, ada_b, norm1_g, w_in, gate_b, mlstm_norm_g, na_rpb, w_out,
           norm2_g, peer_wq, peer_k1, peer_k2, peer_u, peer_v, final_g):
    cfg = dict(CFG)
    D, NCORE = cfg["D"], cfg["NC"]
    KD = D // 128
    f = lambda a: np.asarray(a, dtype=np.float32)
    x_all = np.ascontiguousarray(np.concatenate([f(x_prompt)[0], f(x_sample).reshape(-1, D)], axis=0))
    c_all = np.concatenate([f(c_prompt), f(c_sample)], axis=0)
    ncol = 6 * D // NCORE
    nc0 = build_l0(cfg)
    cT = _T(c_all, KD)
    in0 = [{"cT": cT, "w": np.ascontiguousarray(f(ada_w)[0][:, c * ncol:(c + 1) * ncol]),
            "b": np.ascontiguousarray(f(ada_b)[0][None, c * ncol:(c + 1) * ncol])} for c in range(NCORE)]
    r0 = run_bass_kernel_spmd(nc0, in0, core_ids=list(range(NCORE)))
    mod = np.concatenate([r0.results[c]["y"] for c in range(NCORE)], axis=1)
    sh1, sc1, g1, sh2, sc2, g2 = np.split(mod, 6, axis=1)
    nc1 = build_l1(cfg, phases="ABCD")
    n1g = np.ascontiguousarray(f(norm1_g)[0].reshape(KD, 128).T)
    sc1T, sh1T = _T(sc1, KD), _T(sh1, KD)
    in1 = []
    for c in range(NCORE):
        z, m = _na_consts(f(na_rpb)[0, 2 * c:2 * c + 2])
        in1.append({"x": x_all, "sc1T": sc1T, "sh1T": sh1T, "n1g": n1g, "w": _w_own(f(w_in)[0], c, D),
                    "gb": np.ascontiguousarray(f(gate_b)[0][[g * 8 + c for g in range(4)]][None, :]),
                    "mng": np.ascontiguousarray(f(mlstm_norm_g)[0][None, c * 256:(c + 1) * 256]), "zall": z, "nmask": m})
    r1 = run_bass_kernel_spmd(nc1, in1, core_ids=list(range(NCORE)))
    mixT = np.concatenate([np.asarray(r1.results[c]["mix"]) for c in range(NCORE)], axis=1)
    del in1, r1
    SEQ, DB, DS, NK = cfg["SEQ"], cfg["DB"], cfg["DS"], cfg["NKEYS"]
    TPp = SEQ // NCORE
    TSs = DB * DS // NCORE
    nc2 = build_l2(cfg)
    perm = np.concatenate([np.concatenate([np.arange(c * 256, (c + 1) * 256), D // 2 + np.arange(c * 256, (c + 1) * 256)]) for c in range(NCORE)])
    wo = np.ascontiguousarray(f(w_out)[0][perm, :])
    wq = np.ascontiguousarray(f(peer_wq)[0])
    kT = np.zeros((128, 16, NK), np.float32)
    k1, k2 = f(peer_k1)[0], f(peer_k2)[0]
    for h in range(8):
        kT[:, 2 * h, :] = k1[h].T
        kT[:, 2 * h + 1, :] = k2[h].T
    uu = np.ascontiguousarray(f(peer_u)[0])
    vv = np.ascontiguousarray(f(peer_v)[0])
    n2 = np.ascontiguousarray(f(norm2_g)[0][None, :])
    fgv = np.ascontiguousarray(f(final_g).reshape(1, D))
    io = np.arange(256, dtype=np.float32)[None]
    in2 = []
    for c in range(NCORE):
        rows = np.concatenate([np.arange(c * TPp, (c + 1) * TPp), SEQ + np.arange(c * TSs, (c + 1) * TSs)])
        sidx = [0, 1 + (c * TSs) // DS]
        in2.append({"x": np.ascontiguousarray(x_all[rows]), "mixo": np.ascontiguousarray(mixT[rows]), "wo": wo, "wq": wq,
                    "g1o": np.ascontiguousarray(g1[sidx]), "g2o": np.ascontiguousarray(g2[sidx]),
                    "sc2o": np.ascontiguousarray(sc2[sidx]), "sh2o": np.ascontiguousarray(sh2[sidx]),
                    "n2g": n2, "fg": fgv, "kT": kT, "u": uu, "v": vv, "io256": io})
    r2 = run_bass_kernel_spmd(nc2, in2, core_ids=list(range(NCORE)))
    y_prompt = np.zeros((1, SEQ, D), np.float32)
    y_sample = np.zeros((DB * DS, D), np.float32)
    for c in range(NCORE):
        yc = np.asarray(r2.results[c]["y"])
        y_prompt[0, c * TPp:(c + 1) * TPp] = yc[:TPp]
        y_sample[c * TSs:(c + 1) * TSs] = yc[TPp:]
    return (y_prompt, y_sample.reshape(DB, DS, D))
```

```python
import numpy as np
from contextlib import ExitStack
import ml_dtypes
import concourse.bass as bass
import concourse.mybir as mybir
from concourse.bass_utils import run_bass_kernel_spmd

F32 = mybir.dt.float32
BF16 = mybir.dt.bfloat16
I32 = mybir.dt.int32
U32 = mybir.dt.uint32
AF = mybir.ActivationFunctionType
ALU = mybir.AluOpType
AX = mybir.AxisListType
EPS = 1e-6
NEG = -30000.0

CFG = dict(D=4096, SEQ=16384, DB=4, DS=2048, NKEYS=128, NC=8)


class Res:
    __slots__ = ("name", "lw", "rd", "sem", "cnt")

    def __init__(self, name):
        self.name = name
        self.lw = None
        self.rd = {}
        self.sem = None
        self.cnt = 0


class Prog:
    ENG = ("pe", "act", "dve", "pool", "sp")

    def __init__(self, nc, stack):
        self.nc = nc
        self.stack = stack
        self.sems = {}
        self.lists = {e: [] for e in self.ENG}
        self.seq = {e: 0 for e in self.ENG}
        self.seen = {e: {} for e in self.ENG}
        self.final = {}
        for e in self.ENG:
            self._sem("E_" + e)
        self.ninst = 0
        self.dsems = []
        self.dsem_i = 0

    def _sem(self, key):
        if key not in self.sems:
            self.sems[key] = self.stack.enter_context(self.nc.semaphore(key))
            self.final[key] = 0
        return self.sems[key]

    def _deps(self, e, reads, writes):
        evs = {}

        def add(ev):
            if ev is None:
                return
            k, v = ev
            if evs.get(k, 0) < v:
                evs[k] = v
        for r in reads:
            add(r.lw)
        for w in writes:
            add(w.lw)
            for k, v in w.rd.items():
                add((k, v))
        out = []
        for k, v in evs.items():
            if e == "pe" and k == "E_pe":
                continue
            if self.seen[e].get(k, 0) >= v:
                continue
            self.seen[e][k] = v
            out.append((k, v))
        return out

    def _mark(self, ev, reads, writes):
        k, v = ev
        for r in reads:
            if r.rd.get(k, 0) < v:
                r.rd[k] = v
        for w in writes:
            w.lw = ev
            w.rd = {}

    def op(self, e, fn, reads=(), writes=()):
        for k, v in self._deps(e, reads, writes):
            self.lists[e].append(("w", k, v))
        self.seq[e] += 1
        ev = ("E_" + e, self.seq[e])
        self.final[ev[0]] = ev[1]
        self.lists[e].append(("i", fn, ev[0], 1))
        self._mark(ev, reads, writes)
        self.ninst += 1

    def dma(self, e, fn, reads=(), writes=(), dst=None):
        if dst is None:
            dst = writes[0]
        if dst.sem is None:
            dst.sem = "D_" + dst.name
            self._sem(dst.sem)
        for k, v in self._deps(e, reads, writes):
            self.lists[e].append(("w", k, v))
        dst.cnt += 16
        ev = (dst.sem, dst.cnt)
        self.final[dst.sem] = dst.cnt
        self.lists[e].append(("i", fn, dst.sem, 16))
        self._mark(ev, reads, writes)
        self.ninst += 1

    def barrier(self):
        for e in self.ENG:
            for k, v in self.final.items():
                if v > 0 and self.seen[e].get(k, 0) < v and not (k == "E_" + e):
                    self.seen[e][k] = v
                    self.lists[e].append(("w", k, v))

    def flush(self):
        nc = self.nc
        lists = self.lists
        sems = self.sems

        def replay(eng, lst):
            for it in lst:
                if it[0] == "w":
                    eng.wait_ge(sems[it[1]], it[2])
                else:
                    it[1](eng).then_inc(sems[it[2]], it[3])

        with nc.Block() as block:
            @block.tensor
            def _(eng):
                replay(eng, lists["pe"])

            @block.scalar
            def _(eng):
                replay(eng, lists["act"])

            @block.vector
            def _(eng):
                replay(eng, lists["dve"])

            @block.gpsimd
            def _(eng):
                replay(eng, lists["pool"])

            @block.sync
            def _(eng):
                replay(eng, lists["sp"])
        self.lists = {e: [] for e in self.ENG}


class Ctx:
    _n = [0]

    def __init__(self, nc, st, P):
        self.nc, self.st, self.P = nc, st, P
        Ctx._n[0] += 1
        self.pre = "c%d_" % Ctx._n[0]

    def sb(self, name, shape, dt):
        t = self.st.enter_context(self.nc.sbuf_tensor(self.pre + name, shape, dt))
        return t, Res(self.pre + name)

    def ps(self, name, shape, dt):
        t = self.st.enter_context(self.nc.psum_tensor(self.pre + name, shape, dt))
        return t, Res(self.pre + name)


def make_ident(P, C, n=128):
    idf, r_idf = C.sb("identf", [128, 128], F32)
    idb, r_idb = C.sb("identb", [128, 128], BF16)
    P.op("pool", lambda e: e.memset(idf[:], 0.0), writes=[r_idf])
    P.op("pool", lambda e: e.affine_select(out=idf[:], in_=idf[:], pattern=[[-1, 128]], compare_op=ALU.not_equal,
                                           fill=1.0, base=0, channel_multiplier=1), reads=[r_idf], writes=[r_idf])
    P.op("dve", lambda e: e.tensor_copy(out=idb[:], in_=idf[:]), reads=[r_idf], writes=[r_idb])
    return idf, r_idf, idb, r_idb


def build_l0(cfg):
    D = cfg["D"]
    KD = D // 128
    NS = 1 + cfg["DB"]
    NCOL = 6 * D // cfg["NC"]
    nc = bass.Bass("TRN2", target_bir_lowering=False)
    cT = nc.dram_tensor("cT", [128, KD, NS], F32, kind="ExternalInput")
    w = nc.dram_tensor("w", [D, NCOL], F32, kind="ExternalInput")
    b = nc.dram_tensor("b", [1, NCOL], F32, kind="ExternalInput")
    y = nc.dram_tensor("y", [NS, NCOL], F32, kind="ExternalOutput")
    r_y = Res("y")
    wv = w[:, :].rearrange("(k p) n -> p k n", p=128)
    with ExitStack() as st:
        P = Prog(nc, st)
        C = Ctx(nc, st, P)
        ct, r_ct = C.sb("ct", [128, KD, NS], F32)
        sg, r_sg = C.sb("sg", [128, KD, NS], F32)
        bt, r_bt = C.sb("bt", [NS, NCOL], F32)
        ot, r_ot = C.sb("ot", [NS, NCOL], F32)
        wt = [C.sb("wt%d" % i, [128, KD, 512], F32) for i in range(2)]
        pm = [C.ps("pm%d" % i, [128, 512], F32) for i in range(2)]
        P.dma("sp", lambda e: e.dma_start(out=ct[:], in_=cT[:, :, :]), writes=[r_ct])
        P.dma("sp", lambda e: e.dma_start(out=bt[:], in_=b[:, :].partition_broadcast(NS)), writes=[r_bt])
        P.op("act", lambda e: e.activation(out=sg[:], in_=ct[:], func=AF.Silu), reads=[r_ct], writes=[r_sg])
        nb = NCOL // 512
        for j in range(nb):
            wtj, r_w = wt[j % 2]
            pmj, r_p = pm[j % 2]
            for h in range(4):
                P.dma("sp" if h % 2 == 0 else "act", lambda e, wtj=wtj, j=j, h=h: e.dma_start(
                    out=wtj[:, h * (KD // 4):(h + 1) * (KD // 4), :], in_=wv[:, h * (KD // 4):(h + 1) * (KD // 4), j * 512:(j + 1) * 512]), writes=[r_w])
            for k in range(KD):
                P.op("pe", lambda e, wtj=wtj, pmj=pmj, k=k: e.matmul(pmj[0:NS, :], lhsT=sg[:, k, :], rhs=wtj[:, k, :],
                                                                     start=(k == 0), stop=(k == KD - 1)), reads=[r_sg, r_w], writes=[r_p])
            P.op("dve", lambda e, pmj=pmj, j=j: e.tensor_tensor(out=ot[:, j * 512:(j + 1) * 512], in0=pmj[0:NS, :], in1=bt[:, j * 512:(j + 1) * 512], op=ALU.add),
                 reads=[r_p, r_bt], writes=[r_ot])
        P.dma("pool", lambda e: e.dma_start(out=y[:, :], in_=ot[:]), reads=[r_ot], writes=[r_y])
        P.barrier()
        P.flush()
    return nc


WC = 2052


def seq_list(cfg):
    s = [(0, cfg["SEQ"])]
    for b in range(cfg["DB"]):
        s.append((cfg["SEQ"] + b * cfg["DS"], cfg["DS"]))
    return s


def build_l1(cfg, phases="ABCD"):
    D = cfg["D"]
    KD = D // 128
    NS = 1 + cfg["DB"]
    NT = cfg["SEQ"] + cfg["DB"] * cfg["DS"]
    seqs = seq_list(cfg)
    nc = bass.Bass("TRN2", target_bir_lowering=False)
    x = nc.dram_tensor("x", [NT, D], F32, kind="ExternalInput")
    sc1T = nc.dram_tensor("sc1T", [128, KD, NS], F32, kind="ExternalInput")
    sh1T = nc.dram_tensor("sh1T", [128, KD, NS], F32, kind="ExternalInput")
    n1g = nc.dram_tensor("n1g", [128, KD], F32, kind="ExternalInput")
    w = nc.dram_tensor("w", [D, WC], F32, kind="ExternalInput")
    gb = nc.dram_tensor("gb", [1, 4], F32, kind="ExternalInput")
    mng = nc.dram_tensor("mng", [1, 256], F32, kind="ExternalInput")
    zall = nc.dram_tensor("zall", [30, 127], F32, kind="ExternalInput")
    nmask = nc.dram_tensor("nmask", [64, 64], F32, kind="ExternalInput")
    mix = nc.dram_tensor("mix", [NT, 512], BF16, kind="ExternalOutput")
    dbg = "E" in phases
    okind = "ExternalOutput" if dbg else "Internal"
    wb = nc.dram_tensor("wb", [D, WC], BF16, kind="Internal")
    fm = nc.dram_tensor("fm", [8, 128, NT], BF16, kind=okind)
    kv = nc.dram_tensor("kv", [NT, 512], BF16, kind=okind)
    mo = nc.dram_tensor("mo", [NT, 256], F32, kind=okind)
    nv = nc.dram_tensor("nv", [NT, 256], BF16, kind=okind)
    gt = nc.dram_tensor("gt", [NT, 4], F32, kind=okind)
    hf = nc.dram_tensor("hf", [NT, 256], F32, kind="Internal")
    r_mix, r_wb, r_fm, r_kv, r_mo, r_nv, r_gt, r_hf = [Res(n) for n in ("mix", "wb", "fm", "kv", "mo", "nv", "gt", "hf")]
    wv = w[:, :].rearrange("(k p) n -> p k n", p=128)
    wbv = wb[:, :].rearrange("(k p) n -> p k n", p=128)

    with ExitStack() as st0:
        P = Prog(nc, st0)
        if "A" in phases:
            with ExitStack() as st:
                C = Ctx(nc, st, P)
                wf = [C.sb("wf%d" % i, [128, WC], F32) for i in range(2)]
                wo = [C.sb("wo%d" % i, [128, WC], BF16) for i in range(2)]
                for k in range(KD):
                    a, r_a = wf[k % 2]
                    o, r_o = wo[k % 2]
                    P.dma("sp", lambda e, a=a, k=k: e.dma_start(out=a[:], in_=w[k * 128:(k + 1) * 128, :]), writes=[r_a])
                    P.op("act" if k % 2 else "dve", (lambda e, a=a, o=o: e.activation(out=o[:], in_=a[:], func=AF.Copy)) if k % 2 else
                         (lambda e, a=a, o=o: e.tensor_copy(out=o[:], in_=a[:])), reads=[r_a], writes=[r_o])
                    P.dma("pool", lambda e, o=o, k=k: e.dma_start(out=wb[k * 128:(k + 1) * 128, :], in_=o[:]), reads=[r_o], writes=[r_wb])
                P.barrier()
                P.flush()
        if "B" in phases:
            with ExitStack() as st:
                C = Ctx(nc, st, P)
                idf, r_idf, idb, r_idb = make_ident(P, C)
                sct, r_sct = C.sb("sct", [128, KD, NS], F32)
                sht, r_sht = C.sb("sht", [128, KD, NS], F32)
                ngt, r_ngt = C.sb("ngt", [128, KD], F32)
                gbt, r_gbt = C.sb("gbt", [128, 4], F32)
                P.dma("sp", lambda e: e.dma_start(out=sct[:], in_=sc1T[:, :, :]), writes=[r_sct])
                P.dma("sp", lambda e: e.dma_start(out=sht[:], in_=sh1T[:, :, :]), writes=[r_sht])
                P.dma("sp", lambda e: e.dma_start(out=ngt[:], in_=n1g[:, :]), writes=[r_ngt])
                P.dma("sp", lambda e: e.dma_start(out=gbt[:], in_=gb[:, :].partition_broadcast(128)), writes=[r_gbt])
                P.op("dve", lambda e: e.tensor_scalar(out=sct[:], in0=sct[:], scalar1=1.0, scalar2=None, op0=ALU.add), reads=[r_sct], writes=[r_sct])
                for s in range(NS):
                    P.op("dve", lambda e, s=s: e.tensor_tensor(out=sct[:, :, s], in0=sct[:, :, s], in1=ngt[:], op=ALU.mult), reads=[r_sct, r_ngt], writes=[r_sct])
                xt = [C.sb("xt%d" % i, [128, D], F32) for i in range(2)]
                xn, r_xn = C.sb("xn", [128, D], BF16)
                jk, r_jk = C.sb("jk", [128, D], BF16)
                ssq, r_ssq = C.sb("ssq", [128, 1], F32)
                rstd, r_rstd = C.sb("rstd", [128, 1], F32)
                hT = [C.sb("hT%d" % i, [128, KD, 512], BF16) for i in range(2)]
                wfm = [C.sb("wfm%d" % i, [128, KD, 128], BF16) for i in range(2)]
                wtm = [C.sb("wtm%d" % i, [128, KD, 512], BF16) for i in range(2)]
                wg, r_wg = C.sb("wg", [128, KD, 4], BF16)
                ofm = [C.sb("ofm%d" % i, [128, 512], BF16) for i in range(2)]
                okv = [C.sb("okv%d" % i, [128, 512], BF16) for i in range(2)]
                omo = [C.sb("omo%d" % i, [128, 512], F32) for i in range(2)]
                og = [C.sb("og%d" % i, [128, 4], F32) for i in range(2)]
                pT = [C.ps("pT%d" % i, [128, 1024], BF16) for i in range(2)]
                pM = [C.ps("pM%d" % i, [128, 512], F32) for i in range(4)]
                P.dma("sp", lambda e: e.dma_start(out=wg[:], in_=wbv[:, :, 2048:2052]), reads=[r_wb], writes=[r_wg])
                ngrp = NT // 512
                cnt = dict(t=0, p=0, fm=0, tm=0, o=0)
                for g in range(ngrp):
                    hTg, r_hT = hT[g % 2]
                    for ti in range(4):
                        tok0 = g * 512 + ti * 128
                        s = [i for i, (a, l) in enumerate(seqs) if a <= tok0 < a + l][0]
                        xtt, r_xt = xt[cnt["t"] % 2]
                        cnt["t"] += 1
                        for h in range(4):
                            P.dma("sp", lambda e, xtt=xtt, tok0=tok0, h=h: e.dma_start(out=xtt[:, h * (D // 4):(h + 1) * (D // 4)], in_=x[tok0:tok0 + 128, h * (D // 4):(h + 1) * (D // 4)]), writes=[r_xt])
                        P.op("act", lambda e, xtt=xtt: e.activation(out=jk[:], in_=xtt[:], func=AF.Square, accum_out=ssq[:]), reads=[r_xt], writes=[r_jk, r_ssq])
                        P.op("dve", lambda e: e.tensor_scalar(out=rstd[:], in0=ssq[:], scalar1=1.0 / D, scalar2=EPS, op0=ALU.mult, op1=ALU.add), reads=[r_ssq], writes=[r_rstd])
                        P.op("act", lambda e: e.activation(out=rstd[:], in_=rstd[:], func=AF.Sqrt), reads=[r_rstd], writes=[r_rstd])
                        P.op("dve", lambda e: e.reciprocal(out=rstd[:], in_=rstd[:]), reads=[r_rstd], writes=[r_rstd])
                        P.op("act", lambda e, xtt=xtt: e.activation(out=xn[:], in_=xtt[:], func=AF.Copy, scale=rstd[:]), reads=[r_xt, r_rstd], writes=[r_xn])
                        for q in range(KD // 8):
                            pTt, r_pT = pT[cnt["p"] % 2]
                            cnt["p"] += 1
                            for j in range(8):
                                k = q * 8 + j
                                P.op("pe", lambda e, pTt=pTt, j=j, k=k: e.transpose(out=pTt[:, j * 128:(j + 1) * 128], in_=xn[:, k * 128:(k + 1) * 128], identity=idb[:]),
                                     reads=[r_xn, r_idb], writes=[r_pT])
                            for j in range(8):
                                k = q * 8 + j
                                if j % 2 == 0:
                                    P.op("act", lambda e, pTt=pTt, j=j, k=k, s=s, hTg=hTg, ti=ti: e.activation(out=hTg[:, k, ti * 128:(ti + 1) * 128], in_=pTt[:, j * 128:(j + 1) * 128],
                                                                                                          func=AF.Identity, scale=sct[:, k, s:s + 1], bias=sht[:, k, s:s + 1]),
                                         reads=[r_pT, r_sct, r_sht], writes=[r_hT])
                                else:
                                    P.op("dve", lambda e, pTt=pTt, j=j, k=k, s=s, hTg=hTg, ti=ti: e.tensor_scalar(out=hTg[:, k, ti * 128:(ti + 1) * 128], in0=pTt[:, j * 128:(j + 1) * 128],
                                                                                                             scalar1=sct[:, k, s:s + 1], scalar2=sht[:, k, s:s + 1], op0=ALU.mult, op1=ALU.add),
                                         reads=[r_pT, r_sct, r_sht], writes=[r_hT])
                    if cfg.get('lim', 9) < 2:
                        continue
                    for cb in range(8):
                        wt_, r_w = wfm[cnt["fm"] % 2]
                        cnt["fm"] += 1
                        P.dma("sp", lambda e, wt_=wt_, cb=cb: e.dma_start(out=wt_[:], in_=wbv[:, :, cb * 128:(cb + 1) * 128]), reads=[r_wb], writes=[r_w])
                        pm_, r_p = pM[cnt["o"] % 4]
                        o_, r_o = ofm[cnt["o"] % 2]
                        cnt["o"] += 1
                        for k in range(KD):
                            P.op("pe", lambda e, pm_=pm_, wt_=wt_, hTg=hTg, k=k: e.matmul(pm_[:, :], lhsT=wt_[:, k, :], rhs=hTg[:, k, :], start=(k == 0), stop=(k == KD - 1)),
                                 reads=[r_w, r_hT], writes=[r_p])
                        P.op("act", lambda e, pm_=pm_, o_=o_: e.activation(out=o_[:], in_=pm_[:, :], func=AF.Copy), reads=[r_p], writes=[r_o])
                        P.dma("pool", lambda e, o_=o_, cb=cb, g=g: e.dma_start(out=fm[cb, :, g * 512:(g + 1) * 512], in_=o_[:]), reads=[r_o], writes=[r_fm])
                    if cfg.get('lim', 9) < 2.5:
                        continue
                    for blk in range(2 if cfg.get('lim', 9) != 2.5 else 1):
                        wt_, r_w = wtm[cnt["tm"] % 2]
                        cnt["tm"] += 1
                        for h in range(2):
                            P.dma("sp", lambda e, wt_=wt_, blk=blk, h=h: e.dma_start(out=wt_[:, h * (KD // 2):(h + 1) * (KD // 2), :], in_=wbv[:, h * (KD // 2):(h + 1) * (KD // 2), 1024 + blk * 512:1536 + blk * 512]), reads=[r_wb], writes=[r_w])
                        for ti in range(4):
                            tok0 = g * 512 + ti * 128
                            pm_, r_p = pM[cnt["o"] % 4]
                            o_, r_o = okv[cnt["o"] % 2]
                            o2_, r_o2 = omo[cnt["o"] % 2]
                            cnt["o"] += 1
                            for k in range(KD):
                                P.op("pe", lambda e, pm_=pm_, wt_=wt_, hTg=hTg, k=k, ti=ti: e.matmul(pm_[:, :], lhsT=hTg[:, k, ti * 128:(ti + 1) * 128], rhs=wt_[:, k, :], start=(k == 0), stop=(k == KD - 1)),
                                     reads=[r_w, r_hT], writes=[r_p])
                            if blk == 0:
                                P.op("dve", lambda e, pm_=pm_, o_=o_: e.tensor_copy(out=o_[:], in_=pm_[:, :]), reads=[r_p], writes=[r_o])
                                P.dma("pool", lambda e, o_=o_, tok0=tok0: e.dma_start(out=kv[tok0:tok0 + 128, :], in_=o_[:]), reads=[r_o], writes=[r_kv])
                            else:
                                P.op("act", lambda e, pm_=pm_, o2_=o2_: e.activation(out=o2_[:], in_=pm_[:, :], func=AF.Copy), reads=[r_p], writes=[r_o2])
                                P.op("dve", lambda e, o2_=o2_, o_=o_: e.tensor_copy(out=o_[:, 0:256], in_=o2_[:, 256:512]), reads=[r_o2], writes=[r_o])
                                P.dma("pool", lambda e, o2_=o2_, tok0=tok0: e.dma_start(out=mo[tok0:tok0 + 128, :], in_=o2_[:, 0:256]), reads=[r_o2], writes=[r_mo])
                                P.dma("pool", lambda e, o_=o_, tok0=tok0: e.dma_start(out=nv[tok0:tok0 + 128, :], in_=o_[:, 0:256]), reads=[r_o], writes=[r_nv])
                    if cfg.get('lim', 9) < 4:
                        continue
                    for ti in range(4):
                        tok0 = g * 512 + ti * 128
                        pm_, r_p = pM[cnt["o"] % 4]
                        o_, r_o = og[cnt["o"] % 2]
                        cnt["o"] += 1
                        for k in range(KD):
                            P.op("pe", lambda e, pm_=pm_, hTg=hTg, k=k, ti=ti: e.matmul(pm_[:, 0:4], lhsT=hTg[:, k, ti * 128:(ti + 1) * 128], rhs=wg[:, k, :], start=(k == 0), stop=(k == KD - 1)),
                                 reads=[r_wg, r_hT], writes=[r_p])
                        P.op("dve", lambda e, pm_=pm_, o_=o_: e.tensor_tensor(out=o_[:], in0=pm_[:, 0:4], in1=gbt[:], op=ALU.add), reads=[r_p, r_gbt], writes=[r_o])
                        P.dma("pool", lambda e, o_=o_, tok0=tok0: e.dma_start(out=gt[tok0:tok0 + 128, :], in_=o_[:]), reads=[r_o], writes=[r_gt])
                P.barrier()
                P.flush()
        if "C" in phases:
            gt2 = nc.dram_tensor("gt2", [NT, 4], F32, kind="Internal")
            r_gt2 = Res("gt2")
            with ExitStack() as st:
                C = Ctx(nc, st, P)
                idf, r_idf, idb, r_idb = make_ident(P, C)
                NB = NT // 128
                g_in, r_gin = C.sb("g_in", [128, NB, 4], F32)
                g_a, r_ga = C.sb("g_a", [128, NB, 4], F32)
                g_b, r_gb = C.sb("g_b", [128, NB, 4], F32)
                P.dma("sp", lambda e: e.dma_start(out=g_in[:], in_=gt[:, :].rearrange("(n p) c -> p n c", p=128)), reads=[r_gt], writes=[r_gin])
                P.op("act", lambda e: e.activation(out=g_a[:], in_=g_in[:], func=AF.Abs), reads=[r_gin], writes=[r_ga])
                P.op("act", lambda e: e.activation(out=g_a[:], in_=g_a[:], func=AF.Exp, scale=-1.0), reads=[r_ga], writes=[r_ga])
                P.op("act", lambda e: e.activation(out=g_a[:], in_=g_a[:], func=AF.Ln, bias=1.0), reads=[r_ga], writes=[r_ga])
                P.op("dve", lambda e: e.tensor_scalar(out=g_b[:], in0=g_in[:], scalar1=0.0, scalar2=None, op0=ALU.min), reads=[r_gin], writes=[r_gb])
                P.op("dve", lambda e: e.tensor_tensor(out=g_b[:], in0=g_b[:], in1=g_a[:], op=ALU.subtract), reads=[r_gb, r_ga], writes=[r_gb])
                for col in (0, 2):
                    P.op("dve", lambda e, col=col: e.tensor_copy(out=g_b[:, :, col:col + 1], in_=g_in[:, :, col:col + 1]), reads=[r_gin, r_gb], writes=[r_gb])
                P.dma("sp", lambda e: e.dma_start(out=gt2[:, :].rearrange("(n p) c -> p n c", p=128), in_=g_b[:]), reads=[r_gb], writes=[r_gt2])
                tri = {}
                mneg = {}
                sel = {}
                for dn in ("f", "b"):
                    t_, r_t = C.sb("tri" + dn, [64, 64], F32)
                    m_, r_m = C.sb("mneg" + dn, [64, 64], F32)
                    s_, r_s = C.sb("sel" + dn, [64, 128], F32)
                    sgn = 1 if dn == "f" else -1
                    P.op("pool", lambda e, t_=t_: e.memset(t_[:], 1.0), writes=[r_t])
                    P.op("pool", lambda e, t_=t_, sgn=sgn: e.affine_select(out=t_[:], in_=t_[:], pattern=[[sgn, 64]], compare_op=ALU.is_ge, fill=0.0, base=0, channel_multiplier=-sgn), reads=[r_t], writes=[r_t])
                    P.op("pool", lambda e, m_=m_: e.memset(m_[:], 0.0), writes=[r_m])
                    P.op("pool", lambda e, m_=m_, sgn=sgn: e.affine_select(out=m_[:], in_=m_[:], pattern=[[-sgn, 64]], compare_op=ALU.is_ge, fill=NEG, base=0, channel_multiplier=sgn), reads=[r_m], writes=[r_m])
                    last = 63 if dn == "f" else 0
                    P.op("pool", lambda e, s_=s_: e.memset(s_[:], 0.0), writes=[r_s])
                    P.op("pool", lambda e, s_=s_, last=last: e.affine_select(out=s_[:], in_=s_[:], pattern=[[0, 128]], compare_op=ALU.not_equal, fill=1.0, base=-last, channel_multiplier=1), reads=[r_s], writes=[r_s])
                    tri[dn], mneg[dn], sel[dn] = (t_, r_t), (m_, r_m), (s_, r_s)
                ones, r_ones = C.sb("ones64", [64, 64], F32)
                P.op("pool", lambda e: e.memset(ones[:], 1.0), writes=[r_ones])
                mngb, r_mngb = C.sb("mngb", [64, 256], F32)
                P.dma("sp", lambda e: e.dma_start(out=mngb[:], in_=mng[:, :].partition_broadcast(64)), writes=[r_mngb])
                hbk = nc.dram_tensor("hbk", [NT, 256], F32, kind="Internal")
                r_hbk = Res("hbk")
                KS = 256 ** -0.5
                fmv = fm[:, :, :].rearrange("h p t -> p h t")

                def mk_tiles(dn):
                    T = {}
                    T["Cs"] = C.sb(dn + "Cs", [128, 2, 257], F32)
                    T["Cb"] = C.sb(dn + "Cb", [128, 2, 257], BF16)
                    T["mcur"] = C.sb(dn + "mcur", [128, 1], F32)
                    for nm, shp, dt in (("gch", [64, 4], F32), ("qTc", [128, 2, 64], BF16), ("kTc", [128, 2, 64], BF16), ("kvt", [64, 512], BF16), ("hout", [64, 256], F32)):
                        T[nm] = [C.sb(dn + nm + str(i), shp, dt) for i in range(2)]
                    for nm in ("a", "cmax", "ws", "ecl", "wint", "em", "ad", "rc", "wsk"):
                        T[nm] = C.sb(dn + "sm_" + nm, [64, 1], F32)
                    T["sc2"] = C.sb(dn + "sc2", [64, 2], F32)
                    T["ml2"] = C.sb(dn + "ml2", [128, 2], F32)
                    T["nml2"] = C.sb(dn + "nml2", [128, 2], F32)
                    T["wold"] = C.sb(dn + "wold", [128, 1], F32)
                    T["diagA"] = C.sb(dn + "diagA", [64, 64], F32)
                    T["am"] = C.sb(dn + "am", [64, 64], F32)
                    T["PT"] = C.sb(dn + "PT", [64, 64], BF16)
                    T["vs"] = C.sb(dn + "vs", [64, 257], BF16)
                    T["H1"] = C.sb(dn + "H1", [64, 257], F32)
                    T["Hh"] = C.sb(dn + "Hh", [64, 257], F32)
                    T["psA"] = C.ps(dn + "psA", [128, 512], F32)
                    T["psI"] = C.ps(dn + "psI", [128, 512], F32)
                    T["psQ"] = C.ps(dn + "psQ", [128, 512], F32)
                    T["psU"] = C.ps(dn + "psU", [128, 512], F32)
                    return T

                def chain(dn, T):
                    icol, fcol = (0, 1) if dn == "f" else (2, 3)
                    tri_, r_tri = tri[dn]
                    mneg_, r_mneg = mneg[dn]
                    sel_, r_sel = sel[dn]
                    hdst, r_hdst = (hf, r_hf) if dn == "f" else (hbk, r_hbk)
                    Cs, r_Cs = T["Cs"]
                    Cb, r_Cb = T["Cb"]
                    mcur, r_mcur = T["mcur"]
                    a_, r_a = T["a"]
                    cm_, r_cm = T["cmax"]
                    ws_, r_ws = T["ws"]
                    ecl_, r_ecl = T["ecl"]
                    wi_, r_wi = T["wint"]
                    em_, r_em = T["em"]
                    ad_, r_ad = T["ad"]
                    rc_, r_rc = T["rc"]
                    wsk_, r_wsk = T["wsk"]
                    sc2, r_sc2 = T["sc2"]
                    ml2, r_ml2 = T["ml2"]
                    nml2, r_nml2 = T["nml2"]
                    wold, r_wold = T["wold"]
                    diagA, r_diagA = T["diagA"]
                    am, r_am = T["am"]
                    PT, r_PT = T["PT"]
                    vs, r_vs = T["vs"]
                    H1, r_H1 = T["H1"]
                    Hh, r_Hh = T["Hh"]
                    psA, r_psA = T["psA"]
                    psI, r_psI = T["psI"]
                    psQ, r_psQ = T["psQ"]
                    psU, r_psU = T["psU"]
                    it = 0
                    for (s0, sl) in seqs:
                        P.op("pool", lambda e: e.memset(Cs[:], 0.0), writes=[r_Cs])
                        P.op("pool", lambda e: e.memset(Cb[:], 0.0), writes=[r_Cb])
                        P.op("pool", lambda e: e.memset(mcur[:], 0.0), writes=[r_mcur])
                        yield
                        nch = sl // 64
                        order = range(nch) if dn == "f" else range(nch - 1, -1, -1)
                        for ch in order:
                            tok0 = s0 + ch * 64
                            g_, r_g = T["gch"][it % 2]
                            q_, r_q = T["qTc"][it % 2]
                            k_, r_k = T["kTc"][it % 2]
                            kv_, r_kvt = T["kvt"][it % 2]
                            ho_, r_ho = T["hout"][it % 2]
                            it += 1
                            P.dma("sp", lambda e, g_=g_, tok0=tok0: e.dma_start(out=g_[:], in_=gt2[tok0:tok0 + 64, :]), reads=[r_gt2], writes=[r_g])
                            P.dma("sp", lambda e, q_=q_, tok0=tok0: e.dma_start(out=q_[:], in_=fmv[:, 0:2, tok0:tok0 + 64]), reads=[r_fm], writes=[r_q])
                            P.dma("sp", lambda e, k_=k_, tok0=tok0: e.dma_start(out=k_[:], in_=fmv[:, 2:4, tok0:tok0 + 64]), reads=[r_fm], writes=[r_k])
                            P.dma("sp", lambda e, kv_=kv_, tok0=tok0: e.dma_start(out=kv_[:], in_=kv[tok0:tok0 + 64, :]), reads=[r_kv], writes=[r_kvt])
                            yield
                            ops = []
                            A = ops.append
                            A(("pe", lambda e, g_=g_: e.matmul(psA[0:64, 0:1], lhsT=tri_[:], rhs=g_[:, fcol:fcol + 1], start=True, stop=True), [r_tri, r_g], [r_psA]))
                            A(("dve", lambda e, g_=g_: e.tensor_tensor(out=a_[:], in0=g_[:, icol:icol + 1], in1=psA[0:64, 0:1], op=ALU.subtract), [r_g, r_psA], [r_a]))
                            A(("dve", lambda e: e.tensor_scalar(out=diagA[:], in0=idf[0:64, 0:64], scalar1=a_[:, 0:1], scalar2=None, op0=ALU.mult), [r_idf, r_a], [r_diagA]))
                            A(("pe", lambda e: e.matmul(psA[0:64, 64:128], lhsT=ones[:], rhs=diagA[:], start=True, stop=True), [r_ones, r_diagA], [r_psA]))
                            A(("dve", lambda e: e.tensor_tensor(out=am[:], in0=psA[0:64, 64:128], in1=mneg_[:], op=ALU.add), [r_psA, r_mneg], [r_am]))
                            A(("dve", lambda e: e.reduce_max(out=cm_[:], in_=am[:], axis=AX.X), [r_am], [r_cm]))
                            A(("dve", lambda e: e.tensor_tensor(out=sc2[:, 0:1], in0=cm_[:], in1=mcur[0:64, :], op=ALU.max), [r_cm, r_mcur], [r_sc2]))
                            A(("dve", lambda e: e.tensor_tensor(out=sc2[:, 1:2], in0=sc2[:, 0:1], in1=psA[0:64, 0:1], op=ALU.add), [r_sc2, r_psA], [r_sc2]))
                            A(("pe", lambda e: e.matmul(psA[:, 128:130], lhsT=sel_[:], rhs=sc2[:], start=True, stop=True), [r_sel, r_sc2], [r_psA]))
                            A(("dve", lambda e: e.tensor_copy(out=ml2[:], in_=psA[:, 128:130]), [r_psA], [r_ml2]))
                            A(("dve", lambda e: e.tensor_scalar(out=nml2[:], in0=ml2[:], scalar1=-1.0, scalar2=None, op0=ALU.mult), [r_ml2], [r_nml2]))
                            A(("act", lambda e: e.activation(out=ws_[:], in_=a_[:], func=AF.Exp, bias=nml2[0:64, 0:1]), [r_a, r_nml2], [r_ws]))
                            A(("act", lambda e: e.activation(out=ecl_[:], in_=sc2[:, 0:1], func=AF.Exp, scale=-1.0, bias=ml2[0:64, 0:1]), [r_sc2, r_ml2], [r_ecl]))
                            A(("act", lambda e: e.activation(out=wi_[:], in_=sc2[:, 0:1], func=AF.Exp, scale=-1.0, bias=mcur[0:64, :]), [r_sc2, r_mcur], [r_wi]))
                            A(("act", lambda e: e.activation(out=em_[:], in_=sc2[:, 1:2], func=AF.Exp, scale=-1.0), [r_sc2], [r_em]))
                            A(("act", lambda e: e.activation(out=wold[:], in_=mcur[:], func=AF.Exp, bias=nml2[:, 0:1]), [r_mcur, r_nml2], [r_wold]))
                            A(("dve", lambda e: e.tensor_scalar(out=wsk_[:], in0=ws_[:], scalar1=KS, scalar2=None, op0=ALU.mult), [r_ws], [r_wsk]))
                            A(("dve", lambda e, kv_=kv_: e.tensor_scalar(out=vs[:, 0:256], in0=kv_[:, 256:512], scalar1=wsk_[:, 0:1], scalar2=None, op0=ALU.mult), [r_kvt, r_wsk], [r_vs]))
                            A(("dve", lambda e: e.tensor_copy(out=vs[:, 256:257], in_=wsk_[:]), [r_wsk, r_vs], [r_vs]))
                            for hh in range(2):
                                A(("pe", lambda e, k_=k_, q_=q_, hh=hh: e.matmul(psA[0:64, 256:320], lhsT=k_[:, hh, :], rhs=q_[:, hh, :], start=(hh == 0), stop=(hh == 1)), [r_k, r_q], [r_psA]))
                            A(("dve", lambda e: e.tensor_tensor(out=PT[:], in0=psA[0:64, 256:320], in1=tri_[:], op=ALU.mult), [r_psA, r_tri], [r_PT]))
                            A(("pe", lambda e: e.matmul(psI[0:64, 0:257], lhsT=PT[:], rhs=vs[:], start=True, stop=True), [r_PT, r_vs], [r_psI]))
                            for hh in range(2):
                                A(("pe", lambda e, q_=q_, hh=hh: e.matmul(psQ[0:64, 0:257], lhsT=q_[:, hh, :], rhs=Cb[:, hh, :], start=(hh == 0), stop=(hh == 1)), [r_q, r_Cb], [r_psQ]))
                            A(("dve", lambda e: e.tensor_scalar(out=H1[:], in0=psQ[0:64, 0:257], scalar1=wi_[:, 0:1], scalar2=None, op0=ALU.mult), [r_psQ, r_wi], [r_H1]))
                            A(("dve", lambda e: e.scalar_tensor_tensor(out=Hh[:], in0=psI[0:64, 0:257], scalar=ecl_[:, 0:1], in1=H1[:], op0=ALU.mult, op1=ALU.add), [r_psI, r_ecl, r_H1], [r_Hh]))
                            A(("act", lambda e: e.activation(out=ad_[:], in_=Hh[:, 256:257], func=AF.Abs), [r_Hh], [r_ad]))
                            A(("dve", lambda e: e.tensor_tensor(out=ad_[:], in0=ad_[:], in1=em_[:], op=ALU.max), [r_ad, r_em], [r_ad]))
                            A(("dve", lambda e: e.reciprocal(out=rc_[:], in_=ad_[:]), [r_ad], [r_rc]))
                            A(("dve", lambda e, ho_=ho_: e.tensor_scalar(out=ho_[:], in0=Hh[:, 0:256], scalar1=rc_[:, 0:1], scalar2=None, op0=ALU.mult), [r_Hh, r_rc], [r_ho]))
                            for hh in range(2):
                                A(("pe", lambda e, kv_=kv_, hh=hh: e.matmul(psU[:, 0:257], lhsT=kv_[:, hh * 128:(hh + 1) * 128], rhs=vs[:], start=True, stop=True), [r_kvt, r_vs], [r_psU]))
                                A(("dve", lambda e, hh=hh: e.scalar_tensor_tensor(out=Cs[:, hh, :], in0=Cs[:, hh, :], scalar=wold[:, 0:1], in1=psU[:, 0:257], op0=ALU.mult, op1=ALU.add), [r_Cs, r_wold, r_psU], [r_Cs]))
                            A(("act", lambda e: e.activation(out=Cb[:], in_=Cs[:], func=AF.Copy), [r_Cs], [r_Cb]))
                            A(("dve", lambda e: e.tensor_copy(out=mcur[:], in_=ml2[:, 1:2]), [r_ml2, r_mcur], [r_mcur]))
                            for (en, fn, rd, wr) in ops:
                                P.op(en, fn, reads=rd, writes=wr)
                                yield
                            P.dma("pool", lambda e, ho_=ho_, tok0=tok0: e.dma_start(out=hdst[tok0:tok0 + 64, :], in_=ho_[:]), reads=[r_ho], writes=[r_hdst])
                            yield

                gens = [chain("f", mk_tiles("f")), chain("b", mk_tiles("b"))]
                alive = [True, True]
                while any(alive):
                    for gi in range(2):
                        if alive[gi]:
                            try:
                                next(gens[gi])
                            except StopIteration:
                                alive[gi] = False
                P.barrier()
                P.flush()
            with ExitStack() as st:
                C = Ctx(nc, st, P)
                mngb, r_mngb = C.sb("mngb2", [128, 256], F32)
                P.dma("sp", lambda e: e.dma_start(out=mngb[:], in_=mng[:, :].partition_broadcast(128)), writes=[r_mngb])
                ta = [C.sb("ca%d" % i, [128, 256], F32) for i in range(2)]
                tb = [C.sb("cb%d" % i, [128, 256], F32) for i in range(2)]
                tm = [C.sb("cm%d" % i, [128, 256], F32) for i in range(2)]
                to = [C.sb("co%d" % i, [128, 256], BF16) for i in range(2)]
                jq, r_jq = C.sb("cjq", [128, 256], F32)
                ssq_, r_ssq = C.sb("cssq", [128, 1], F32)
                rs_, r_rs = C.sb("crs", [128, 1], F32)
                for i in range(NT // 128):
                    tok0 = i * 128
                    a_, r_a = ta[i % 2]
                    b_, r_b = tb[i % 2]
                    m_, r_m = tm[i % 2]
                    o_, r_o = to[i % 2]
                    P.dma("sp", lambda e, a_=a_, tok0=tok0: e.dma_start(out=a_[:], in_=hf[tok0:tok0 + 128, :]), reads=[r_hf], writes=[r_a])
                    P.dma("sp", lambda e, b_=b_, tok0=tok0: e.dma_start(out=b_[:], in_=hbk[tok0:tok0 + 128, :]), reads=[r_hbk], writes=[r_b])
                    P.dma("sp", lambda e, m_=m_, tok0=tok0: e.dma_start(out=m_[:], in_=mo[tok0:tok0 + 128, :]), reads=[r_mo], writes=[r_m])
                    P.op("dve", lambda e, a_=a_, b_=b_: e.tensor_tensor(out=a_[:], in0=a_[:], in1=b_[:], op=ALU.add), reads=[r_a, r_b], writes=[r_a])
                    P.op("act", lambda e, a_=a_: e.activation(out=jq[:], in_=a_[:], func=AF.Square, accum_out=ssq_[:]), reads=[r_a], writes=[r_jq, r_ssq])
                    P.op("dve", lambda e: e.tensor_scalar(out=rs_[:], in0=ssq_[:], scalar1=1.0 / 256, scalar2=EPS, op0=ALU.mult, op1=ALU.add), reads=[r_ssq], writes=[r_rs])
                    P.op("act", lambda e: e.activation(out=rs_[:], in_=rs_[:], func=AF.Sqrt), reads=[r_rs], writes=[r_rs])
                    P.op("dve", lambda e: e.reciprocal(out=rs_[:], in_=rs_[:]), reads=[r_rs], writes=[r_rs])
                    P.op("act", lambda e, m_=m_: e.activation(out=m_[:], in_=m_[:], func=AF.Sigmoid), reads=[r_m], writes=[r_m])
                    P.op("dve", lambda e, a_=a_: e.scalar_tensor_tensor(out=a_[:], in0=a_[:], scalar=rs_[:, 0:1], in1=mngb[:], op0=ALU.mult, op1=ALU.mult), reads=[r_a, r_rs, r_mngb], writes=[r_a])
                    P.op("dve", lambda e, a_=a_, m_=m_, o_=o_: e.tensor_tensor(out=o_[:], in0=a_[:], in1=m_[:], op=ALU.mult), reads=[r_a, r_m], writes=[r_o])
                    P.dma("pool", lambda e, o_=o_, tok0=tok0: e.dma_start(out=mix[tok0:tok0 + 128, 0:256], in_=o_[:]), reads=[r_o], writes=[r_mix])
                P.barrier()
                P.flush()
        if "D" in phases:
            with ExitStack() as st:
                C = Ctx(nc, st, P)
                idf, r_idf, idb, r_idb = make_ident(P, C)
                Zt, r_Zt = C.sb("Zt", [64, 30, 64], F32)
                mk_, r_mk = C.sb("nmaskt", [64, 64], F32)
                P.dma("sp", lambda e: e.dma_start(out=mk_[:], in_=nmask[:, :]), writes=[r_mk])
                for qc in range(64):
                    P.dma("sp" if qc % 2 else "act", lambda e, qc=qc: e.dma_start(out=Zt[qc:qc + 1, :, :], in_=zall[:, 63 - qc:127 - qc].unsqueeze(0)), writes=[r_Zt])
                for i in range(30):
                    P.op("dve", lambda e, i=i: e.tensor_tensor(out=Zt[:, i, :], in0=Zt[:, i, :], in1=mk_[:], op=ALU.add), reads=[r_Zt, r_mk], writes=[r_Zt])
                Ztf = Zt[:].rearrange("p a b -> p (a b)")
                fmv = fm[:, :, :].rearrange("h p t -> p h t")
                NSC = 128 ** -0.5

                def na_chain(h):
                    pre = "h%d" % h
                    q2 = [C.sb(pre + "nq%d" % i, [128, 64], BF16) for i in range(2)]
                    k2 = [C.sb(pre + "nk%d" % i, [128, 512], BF16) for i in range(2)]
                    v2 = [C.sb(pre + "nvv%d" % i, [128, 4, 128], BF16) for i in range(2)]
                    sbt = [C.sb(pre + "nsb%d" % i, [64, 512], F32) for i in range(2)]
                    pb = [C.sb(pre + "npb%d" % i, [64, 512], BF16) for i in range(2)]
                    pTs = [C.sb(pre + "npT%d" % i, [128, 4, 64], BF16) for i in range(2)]
                    onb = [C.sb(pre + "nob%d" % i, [64, 128], BF16) for i in range(2)]
                    mxs = [C.sb(pre + "nmx%d" % i, [64, 1], F32) for i in range(2)]
                    rss = [C.sb(pre + "nrs%d" % i, [64, 1], F32) for i in range(2)]
                    psS = [C.ps(pre + "npsS%d" % i, [128, 512], F32) for i in range(2)]
                    psT, r_pT = C.ps(pre + "npsT", [128, 1024], BF16)
                    psO, r_pO = C.ps(pre + "npsO", [128, 512], F32)
                    it = 0
                    for (s0, sl) in seqs:
                        rows = sl // 64
                        for r in range(rows):
                            rs = min(max(r - 4, 0), rows - 8)
                            rho0 = 3 - ((r - 4) - rs)
                            tq = s0 + r * 64
                            tk = s0 + rs * 64
                            q_, r_q = q2[it % 2]
                            k_, r_k = k2[it % 2]
                            v_, r_v = v2[it % 2]
                            ob_, r_ob = onb[it % 2]
                            pS_, r_pS = psS[it % 2]
                            sb_, r_sb = sbt[it % 2]
                            p_, r_p = pb[it % 2]
                            pt_, r_pt = pTs[it % 2]
                            mx, r_mx = mxs[it % 2]
                            rsum, r_rsum = rss[it % 2]
                            it += 1
                            P.dma("sp", lambda e, q_=q_, tq=tq: e.dma_start(out=q_[:], in_=fm[4 + h, :, tq:tq + 64]), reads=[r_fm], writes=[r_q])
                            P.dma("sp", lambda e, k_=k_, tk=tk: e.dma_start(out=k_[:], in_=fm[6 + h, :, tk:tk + 512]), reads=[r_fm], writes=[r_k])
                            P.dma("sp", lambda e, v_=v_, tk=tk: e.dma_start(out=v_[:], in_=nv[tk:tk + 512, h * 128:(h + 1) * 128].rearrange("(c p) e -> p c e", p=128)), reads=[r_nv], writes=[r_v])
                            yield
                            b0 = (h * 15 + rho0) * 64
                            ops = []
                            A = ops.append
                            A(("pe", lambda e, pS_=pS_, q_=q_, k_=k_: e.matmul(pS_[0:64, :], lhsT=q_[:], rhs=k_[:], start=True, stop=True), [r_q, r_k], [r_pS]))
                            A(("dve", lambda e, pS_=pS_, sb_=sb_, b0=b0: e.scalar_tensor_tensor(out=sb_[:], in0=pS_[0:64, :], scalar=NSC, in1=Ztf[:, b0:b0 + 512], op0=ALU.mult, op1=ALU.add), [r_pS, r_Zt], [r_sb]))
                            A(("dve", lambda e, sb_=sb_, mx=mx: e.reduce_max(out=mx[:], in_=sb_[:], axis=AX.X), [r_sb], [r_mx]))
                            A(("dve", lambda e, mx=mx: e.tensor_scalar(out=mx[:], in0=mx[:], scalar1=-1.0, scalar2=None, op0=ALU.mult), [r_mx], [r_mx]))
                            A(("act", lambda e, sb_=sb_, p_=p_, mx=mx, rsum=rsum: e.activation(out=p_[:], in_=sb_[:], func=AF.Exp, bias=mx[:, 0:1], accum_out=rsum[:]), [r_sb, r_mx], [r_p, r_rsum]))
                            for c4 in range(4):
                                A(("pe", lambda e, p_=p_, c4=c4: e.transpose(out=psT[:, c4 * 64:(c4 + 1) * 64], in_=p_[:, c4 * 128:(c4 + 1) * 128], identity=idb[0:64, 0:64]), [r_p, r_idb], [r_pT]))
                            A(("act", lambda e, pt_=pt_: e.activation(out=pt_[:].rearrange("p a b -> p (a b)"), in_=psT[:, 0:256], func=AF.Copy), [r_pT], [r_pt]))
                            for c4 in range(4):
                                A(("pe", lambda e, pt_=pt_, v_=v_, c4=c4: e.matmul(psO[0:64, 0:128], lhsT=pt_[:, c4, :], rhs=v_[:, c4, :], start=(c4 == 0), stop=(c4 == 3)), [r_pt, r_v], [r_pO]))
                            A(("dve", lambda e, rsum=rsum: e.reciprocal(out=rsum[:], in_=rsum[:]), [r_rsum], [r_rsum]))
                            A(("dve", lambda e, ob_=ob_, rsum=rsum: e.tensor_scalar(out=ob_[:], in0=psO[0:64, 0:128], scalar1=rsum[:, 0:1], scalar2=None, op0=ALU.mult), [r_pO, r_rsum], [r_ob]))
                            for (en, fn, rd, wr) in ops:
                                P.op(en, fn, reads=rd, writes=wr)
                                yield
                            P.dma("pool", lambda e, ob_=ob_, tq=tq: e.dma_start(out=mix[tq:tq + 64, 256 + h * 128:256 + (h + 1) * 128], in_=ob_[:]), reads=[r_ob], writes=[r_mix])
                            yield

                gens = [na_chain(0), na_chain(1)]
                alive = [True, True]
                while any(alive):
                    for gi in range(2):
                        if alive[gi]:
                            try:
                                next(gens[gi])
                            except StopIteration:
                                alive[gi] = False
                P.barrier()
                P.flush()
    return nc


def build_l2(cfg, ntile_lim=None):
    D = cfg["D"]
    KD = D // 128
    NK = cfg["NKEYS"]
    NE = NK * NK
    NCORE = cfg["NC"]
    TPp = cfg["SEQ"] // NCORE
    TPC = TPp + cfg["DB"] * cfg["DS"] // NCORE
    ntile = TPC // 128
    nc = bass.Bass("TRN2", target_bir_lowering=False)
    x = nc.dram_tensor("x", [TPC, D], F32, kind="ExternalInput")
    mixo = nc.dram_tensor("mixo", [TPC, D], BF16, kind="ExternalInput")
    wo = nc.dram_tensor("wo", [D, D], F32, kind="ExternalInput")
    wq = nc.dram_tensor("wq", [D, 2048], F32, kind="ExternalInput")
    g1o = nc.dram_tensor("g1o", [2, D], F32, kind="ExternalInput")
    g2o = nc.dram_tensor("g2o", [2, D], F32, kind="ExternalInput")
    sc2o = nc.dram_tensor("sc2o", [2, D], F32, kind="ExternalInput")
    sh2o = nc.dram_tensor("sh2o", [2, D], F32, kind="ExternalInput")
    n2g = nc.dram_tensor("n2g", [1, D], F32, kind="ExternalInput")
    fg = nc.dram_tensor("fg", [1, D], F32, kind="ExternalInput")
    kTd = nc.dram_tensor("kT", [128, 16, NK], F32, kind="ExternalInput")
    u = nc.dram_tensor("u", [NE, D], F32, kind="ExternalInput")
    v = nc.dram_tensor("v", [NE, D], F32, kind="ExternalInput")
    io256 = nc.dram_tensor("io256", [1, 256], F32, kind="ExternalInput")
    y = nc.dram_tensor("y", [TPC, D], F32, kind="ExternalOutput")
    wob = nc.dram_tensor("wob", [D, D], BF16, kind="Internal")
    wqb = nc.dram_tensor("wqb", [D, 2048], BF16, kind="Internal")
    sc2s = nc.dram_tensor("sc2s", [2, D], F32, kind="Internal")
    h2d = nc.dram_tensor("h2d", [TPC, D], BF16, kind="Internal")
    ub = nc.dram_tensor("ub", [NE, D], BF16, kind="Internal")
    vbd = nc.dram_tensor("vbd", [NE, D], BF16, kind="Internal")
    r_y, r_wob, r_wqb, r_sc2s, r_h2d, r_ub, r_vbd = [Res(n) for n in ("y", "wob", "wqb", "sc2s", "h2d", "ub", "vbd")]
    wobv = wob[:, :].rearrange("(k p) n -> p k n", p=128)
    wqbv = wqb[:, :].rearrange("(k p) n -> p k n", p=128)
    with ExitStack() as st0:
        P = Prog(nc, st0)
        with ExitStack() as st:
            C = Ctx(nc, st, P)
            wf = [C.sb("wf%d" % i, [128, D], F32) for i in range(2)]
            wb_ = [C.sb("wb%d" % i, [128, D], BF16) for i in range(2)]
            n = 0
            for (src, dst, r_dst, ncols, nblk) in ((wo, wob, r_wob, D, KD), (wq, wqb, r_wqb, 2048, KD), (u, ub, r_ub, D, NE // 128), (v, vbd, r_vbd, D, NE // 128)):
                for k in range(nblk):
                    a, r_a = wf[n % 2]
                    o, r_o = wb_[n % 2]
                    P.dma("sp" if n % 2 else "act", lambda e, a=a, k=k, src=src, ncols=ncols: e.dma_start(out=a[:, 0:ncols], in_=src[k * 128:(k + 1) * 128, :]), writes=[r_a])
                    if n % 2:
                        P.op("act", lambda e, a=a, o=o, ncols=ncols: e.activation(out=o[:, 0:ncols], in_=a[:, 0:ncols], func=AF.Copy), reads=[r_a], writes=[r_o])
                    else:
                        P.op("dve", lambda e, a=a, o=o, ncols=ncols: e.tensor_copy(out=o[:, 0:ncols], in_=a[:, 0:ncols]), reads=[r_a], writes=[r_o])
                    P.dma("pool", lambda e, o=o, k=k, dst=dst, ncols=ncols: e.dma_start(out=dst[k * 128:(k + 1) * 128, :], in_=o[:, 0:ncols]), reads=[r_o], writes=[r_dst])
                    n += 1
            s2, r_s2 = C.sb("s2", [2, D], F32)
            n2, r_n2 = C.sb("n2", [2, D], F32)
            P.dma("sp", lambda e: e.dma_start(out=s2[:], in_=sc2o[:, :]), writes=[r_s2])
            P.dma("sp", lambda e: e.dma_start(out=n2[:], in_=n2g[:, :].partition_broadcast(2)), writes=[r_n2])
            P.op("dve", lambda e: e.scalar_tensor_tensor(out=s2[:], in0=s2[:], scalar=1.0, in1=n2[:], op0=ALU.add, op1=ALU.mult), reads=[r_s2, r_n2], writes=[r_s2])
            P.dma("pool", lambda e: e.dma_start(out=sc2s[:, :], in_=s2[:]), reads=[r_s2], writes=[r_sc2s])
            P.barrier()
            P.flush()
        with ExitStack() as st:
            C = Ctx(nc, st, P)
            idf, r_idf, idb, r_idb = make_ident(P, C)
            xt, r_xt = C.sb("xt", [128, D], F32)
            t8, r_t8 = C.sb("t8", [128, D], BF16)
            tT, r_tT = C.sb("tT", [128, KD, 128], BF16)
            wblk = [C.sb("wblk%d" % i, [128, KD, 256], BF16) for i in range(2)]
            bc = [C.sb("bc%d" % i, [128, D], F32) for i in range(1)]
            uv = [C.sb("uv%d" % i, [128, D], F32) for i in range(2)]
            NBUF = 3
            ubt = [C.sb("ubt%d" % i, [128, D], BF16) for i in range(NBUF)]
            vbt = [C.sb("vbt%d" % i, [128, D], BF16) for i in range(NBUF)]
            hb = [C.sb("hb%d" % i, [128, D], BF16) for i in range(2)]
            junk, r_junk = t8, r_t8
            tmpf = [C.sb("tmpf%d" % i, [128, 512], F32) for i in range(2)]
            qT, r_qT = uv[1][0][:, 0:2048].rearrange("p (a b) -> p a b", a=16), uv[1][1]
            scs, r_scs = uv[1][0][:, 2048:2048 + 16 * NK].rearrange("p (a b) -> p a b", a=16), uv[1][1]
            kTt, r_kTt = C.sb("kTt", [128, 16, NK], F32)
            wk, r_wk = C.sb("wk", [128, 256], F32)
            v16, r_v16 = C.sb("v16", [128, 16, 16], F32)
            i16u, r_i16u = C.sb("i16u", [128, 16, 16], U32)
            i16f, r_i16f = C.sb("i16f", [128, 16, 16], F32)
            i1s, r_i1s = C.sb("i1s", [128, 16], F32)
            cand, r_cand = C.sb("cand", [128, 16, 16], F32)
            Eh, r_Eh = C.sb("Eh", [128, 16, 16], F32)
            sc16, r_sc16 = C.sb("sc16", [128, 8, 16], F32)
            ciu, r_ciu = C.sb("ciu", [128, 16], U32)
            cif, r_cif = C.sb("cif", [128, 16], F32)
            io, r_io = C.sb("io", [128, 256], F32)
            eid, r_eid = C.sb("eid", [128, 128], F32)
            gw, r_gw = C.sb("gw", [128, 8, 16], F32)
            nmx, r_nmx = C.sb("nmx", [128, 8], F32)
            gsum, r_gsum = C.sb("gsum", [128, 8], F32)
            idxT, r_idxT = C.sb("idxT", [128, 128], I32)
            gwT, r_gwT = C.sb("gwT", [128, 128], F32)
            ACTT, r_ACTT = C.sb("ACTT", [128, 128], F32)
            Wt, r_Wt = C.sb("Wt", [128, 128], F32)
            gl, r_gl = C.sb("gl", [128, 128], F32)
            Wsel = [C.sb("Wsel%d" % i, [128, 128], BF16) for i in range(2)]
            sm_ = {n_: C.sb("sm_" + n_, [128, 1], F32) for n_ in ("x2", "u", "sg", "w")}
            Zc, r_Zc = C.sb("Zc", [128, 255], F32)
            ssq, r_ssq = C.sb("ssq", [128, 1], F32)
            rstd, r_rstd = C.sb("rstd", [128, 1], F32)
            ps = [C.ps("ps%d" % i, [128, 512], F32) for i in range(8)]
            P.op("pool", lambda e: e.memset(Zc[:], 0.0), writes=[r_Zc])
            P.op("pool", lambda e: e.memset(Zc[:, 127:128], 1.0), reads=[r_Zc], writes=[r_Zc])
            P.dma("sp", lambda e: e.dma_start(out=io[:], in_=io256[:, :].partition_broadcast(128)), writes=[r_io])
            P.dma("sp", lambda e: e.dma_start(out=kTt[:], in_=kTd[:, :, :]), writes=[r_kTt])
            cnt = dict(w=0, b=0, bc=0, uv=0, hb=0, t=0, ws=0)

            def rmsn(src_ap_fn):
                P.op("act", lambda e: e.activation(out=t8[:], in_=xt[:], func=AF.Square, accum_out=ssq[:]), reads=[r_xt], writes=[r_t8, r_ssq])
                P.op("dve", lambda e: e.tensor_scalar(out=rstd[:], in0=ssq[:], scalar1=1.0 / D, scalar2=EPS, op0=ALU.mult, op1=ALU.add), reads=[r_ssq], writes=[r_rstd])
                P.op("act", lambda e: e.activation(out=rstd[:], in_=rstd[:], func=AF.Sqrt), reads=[r_rstd], writes=[r_rstd])
                P.op("dve", lambda e: e.reciprocal(out=rstd[:], in_=rstd[:]), reads=[r_rstd], writes=[r_rstd])

            def transposes():
                for q in range(KD // 8):
                    pb_, r_pb = ps[6 + (q % 2)]
                    pv = pb_[:].bitcast(BF16)
                    for j in range(8):
                        k = q * 8 + j
                        P.op("pe", lambda e, pv=pv, j=j, k=k: e.transpose(out=pv[:, j * 128:(j + 1) * 128], in_=t8[:, k * 128:(k + 1) * 128], identity=idb[:]), reads=[r_t8, r_idb], writes=[r_pb])
                    if q % 2:
                        P.op("act", lambda e, pv=pv, q=q: e.activation(out=tT[:, q * 8:(q + 1) * 8, :].rearrange("p a b -> p (a b)"), in_=pv[:, :], func=AF.Copy), reads=[r_pb], writes=[r_tT])
                    else:
                        P.op("dve", lambda e, pv=pv, q=q: e.tensor_copy(out=tT[:, q * 8:(q + 1) * 8, :].rearrange("p a b -> p (a b)"), in_=pv[:, :]), reads=[r_pb], writes=[r_tT])

            def load_bc(src_ap):
                b_, r_b = bc[0]
                cnt["bc"] += 1
                P.dma("act", lambda e, b_=b_: e.dma_start(out=b_[:], in_=src_ap.partition_broadcast(128)), writes=[r_b])
                return b_, r_b

            def top16(src_ap, r_src, vout, r_vout, iout, r_iout, n):
                wkv = wk[:, 0:n]
                P.op("dve", lambda e: e.max(out=vout[:, 0:8], in_=src_ap), reads=[r_src], writes=[r_vout])
                P.op("dve", lambda e: e.max_index(out=iout[:, 0:8], in_max=vout[:, 0:8], in_values=src_ap), reads=[r_src, r_vout], writes=[r_iout])
                P.op("dve", lambda e: e.match_replace(out=wkv, in_to_replace=vout[:, 0:8], in_values=src_ap, imm_value=-1e30), reads=[r_src, r_vout], writes=[r_wk])
                P.op("dve", lambda e: e.max(out=vout[:, 8:16], in_=wkv), reads=[r_wk], writes=[r_vout])
                P.op("dve", lambda e: e.max_index(out=iout[:, 8:16], in_max=vout[:, 8:16], in_values=wkv), reads=[r_wk, r_vout], writes=[r_iout])

            nt_run = ntile if ntile_lim is None else ntile_lim
            for ti in range(nt_run):
                tok0 = ti * 128
                s = 0 if tok0 < TPp else 1
                for h4 in range(4):
                    P.dma("sp", lambda e, tok0=tok0, h4=h4: e.dma_start(out=xt[:, h4 * (D // 4):(h4 + 1) * (D // 4)], in_=x[tok0:tok0 + 128, h4 * (D // 4):(h4 + 1) * (D // 4)]), writes=[r_xt])
                P.dma("sp", lambda e, tok0=tok0: e.dma_start(out=t8[:], in_=mixo[tok0:tok0 + 128, :]), writes=[r_t8])
                transposes()
                b1, r_b1 = load_bc(g1o[s:s + 1, :])
                for cb in range(D // 256):
                    w_, r_w = wblk[cnt["w"] % 2]
                    cnt["w"] += 1
                    P.dma("sp", lambda e, w_=w_, cb=cb: e.dma_start(out=w_[:], in_=wobv[:, :, cb * 256:(cb + 1) * 256]), reads=[r_wob], writes=[r_w])
                    p_, r_p = ps[cnt["b"] % 6]
                    tf_, r_tf = tmpf[cnt["b"] % 2]
                    cnt["b"] += 1
                    for k in range(KD):
                        P.op("pe", lambda e, p_=p_, w_=w_, k=k: e.matmul(p_[:, 0:256], lhsT=tT[:, k, :], rhs=w_[:, k, :], start=(k == 0), stop=(k == KD - 1)), reads=[r_tT, r_w], writes=[r_p])
                    P.op("dve", lambda e, p_=p_, tf_=tf_, cb=cb, b1=b1: e.tensor_tensor(out=tf_[:, 0:256], in0=p_[:, 0:256], in1=b1[:, cb * 256:(cb + 1) * 256], op=ALU.mult), reads=[r_p, r_b1], writes=[r_tf])
                    P.op("pool", lambda e, tf_=tf_, cb=cb: e.tensor_tensor(out=xt[:, cb * 256:(cb + 1) * 256], in0=xt[:, cb * 256:(cb + 1) * 256], in1=tf_[:, 0:256], op=ALU.add), reads=[r_tf, r_xt], writes=[r_xt])
                rmsn(None)
                b2, r_b2 = load_bc(sc2s[s:s + 1, :])
                u0, r_u0 = uv[0]
                P.op("dve", lambda e, b2=b2: e.scalar_tensor_tensor(out=u0[:], in0=xt[:], scalar=rstd[:, 0:1], in1=b2[:], op0=ALU.mult, op1=ALU.mult), reads=[r_xt, r_rstd, r_b2], writes=[r_u0])
                b3, r_b3 = load_bc(sh2o[s:s + 1, :])
                P.op("dve", lambda e, b3=b3: e.tensor_tensor(out=t8[:], in0=u0[:], in1=b3[:], op=ALU.add), reads=[r_u0, r_b3], writes=[r_t8])
                P.dma("pool", lambda e, tok0=tok0: e.dma_start(out=h2d[tok0:tok0 + 128, :], in_=t8[:]), reads=[r_t8], writes=[r_h2d])
                transposes()
                for j4 in range(8):
                    w_, r_w = wblk[cnt["w"] % 2]
                    cnt["w"] += 1
                    P.dma("sp", lambda e, w_=w_, j4=j4: e.dma_start(out=w_[:], in_=wqbv[:, :, j4 * 256:(j4 + 1) * 256]), reads=[r_wqb], writes=[r_w])
                    for jj in range(2):
                        j = j4 * 2 + jj
                        p_, r_p = ps[cnt["b"] % 6]
                        cnt["b"] += 1
                        for k in range(KD):
                            P.op("pe", lambda e, p_=p_, w_=w_, k=k, jj=jj: e.matmul(p_[:, 0:128], lhsT=w_[:, k, jj * 128:(jj + 1) * 128], rhs=tT[:, k, :], start=(k == 0), stop=(k == KD - 1)), reads=[r_tT, r_w], writes=[r_p])
                        P.op("act", lambda e, p_=p_, j=j: e.activation(out=qT[:, j, :], in_=p_[:, 0:128], func=AF.Copy), reads=[r_p], writes=[r_qT])
                for j in range(16):
                    p_, r_p = ps[cnt["b"] % 6]
                    cnt["b"] += 1
                    P.op("pe", lambda e, p_=p_, j=j: e.matmul(p_[:, 0:NK], lhsT=qT[:, j, :], rhs=kTt[:, j, :], start=True, stop=True), reads=[r_qT, r_kTt], writes=[r_p])
                    P.op("dve", lambda e, p_=p_, j=j: e.tensor_copy(out=scs[:, j, :], in_=p_[:, 0:NK]), reads=[r_p], writes=[r_scs])
                for j in range(16):
                    top16(scs[:, j, :], r_scs, v16[:, j, :], r_v16, i16u[:, j, :], r_i16u, NK)
                P.op("dve", lambda e: e.tensor_copy(out=i16f[:], in_=i16u[:]), reads=[r_i16u], writes=[r_i16f])
                u0v = u0[:].rearrange("p (a b) -> p a b", a=16)
                for h in range(8):
                    j1, j2 = 2 * h, 2 * h + 1
                    P.op("dve", lambda e, j1=j1, j2=j2: e.tensor_tensor(out=cand[:], in0=v16[:, j1, :].unsqueeze(2).to_broadcast([128, 16, 16]), in1=v16[:, j2, :].unsqueeze(1).to_broadcast([128, 16, 16]), op=ALU.add), reads=[r_v16], writes=[r_cand])
                    P.op("dve", lambda e, j1=j1: e.tensor_scalar(out=i1s[:], in0=i16f[:, j1, :], scalar1=float(NK), scalar2=None, op0=ALU.mult), reads=[r_i16f], writes=[r_i1s])
                    P.op("dve", lambda e, j2=j2: e.tensor_tensor(out=Eh[:], in0=i1s[:].unsqueeze(2).to_broadcast([128, 16, 16]), in1=i16f[:, j2, :].unsqueeze(1).to_broadcast([128, 16, 16]), op=ALU.add), reads=[r_i1s, r_i16f], writes=[r_Eh])
                    top16(cand[:].rearrange("p a b -> p (a b)"), r_cand, sc16[:, h, :], r_sc16, ciu[:], r_ciu, 256)
                    P.op("dve", lambda e: e.tensor_copy(out=cif[:], in_=ciu[:]), reads=[r_ciu], writes=[r_cif])
                    P.op("dve", lambda e: e.tensor_tensor(out=u0v, in0=cif[:].unsqueeze(2).to_broadcast([128, 16, 256]), in1=io[:].unsqueeze(1).to_broadcast([128, 16, 256]), op=ALU.is_equal), reads=[r_cif, r_io], writes=[r_u0])
                    P.op("dve", lambda e: e.tensor_tensor(out=u0v, in0=u0v, in1=Eh[:].rearrange("p a b -> p (a b)").unsqueeze(1).to_broadcast([128, 16, 256]), op=ALU.mult), reads=[r_u0, r_Eh], writes=[r_u0])
                    P.op("dve", lambda e, h=h: e.reduce_sum(out=eid[:, h * 16:(h + 1) * 16], in_=u0v, axis=AX.X), reads=[r_u0], writes=[r_eid])
                P.op("dve", lambda e: e.tensor_scalar(out=nmx[:], in0=sc16[:, :, 0], scalar1=-1.0, scalar2=None, op0=ALU.mult), reads=[r_sc16], writes=[r_nmx])
                P.op("dve", lambda e: e.tensor_tensor(out=gw[:], in0=sc16[:], in1=nmx[:].unsqueeze(2).to_broadcast([128, 8, 16]), op=ALU.add), reads=[r_sc16, r_nmx], writes=[r_gw])
                P.op("act", lambda e: e.activation(out=gw[:], in_=gw[:], func=AF.Exp), reads=[r_gw], writes=[r_gw])
                P.op("dve", lambda e: e.reduce_sum(out=gsum[:], in_=gw[:], axis=AX.X), reads=[r_gw], writes=[r_gsum])
                P.op("dve", lambda e: e.reciprocal(out=gsum[:], in_=gsum[:]), reads=[r_gsum], writes=[r_gsum])
                P.op("dve", lambda e: e.tensor_tensor(out=gw[:], in0=gw[:], in1=gsum[:].unsqueeze(2).to_broadcast([128, 8, 16]), op=ALU.mult), reads=[r_gw, r_gsum], writes=[r_gw])
                p_, r_p = ps[cnt["b"] % 6]
                cnt["b"] += 1
                P.op("pe", lambda e, p_=p_: e.transpose(out=p_[:, 0:128], in_=eid[:], identity=idf[:]), reads=[r_eid, r_idf], writes=[r_p])
                P.op("pe", lambda e, p_=p_: e.transpose(out=p_[:, 128:256], in_=gw[:].rearrange("p a b -> p (a b)"), identity=idf[:]), reads=[r_gw, r_idf], writes=[r_p])
                P.op("dve", lambda e, p_=p_: e.tensor_copy(out=idxT[:], in_=p_[:, 0:128]), reads=[r_p], writes=[r_idxT])
                P.op("dve", lambda e, p_=p_: e.tensor_copy(out=gwT[:], in_=p_[:, 128:256]), reads=[r_p], writes=[r_gwT])
                x2_, r_x2 = sm_["x2"]
                uu_, r_uu = sm_["u"]
                sg_, r_sg = sm_["sg"]
                w1_, r_w1 = sm_["w"]
                for t in range(128):
                    U_, r_U = ubt[t % NBUF]
                    V_, r_V = vbt[t % NBUF]
                    H_, r_H = hb[t % 2]
                    ws_, r_ws = Wsel[t % 2]
                    P.dma("pool", lambda e, U_=U_, t=t: e.indirect_dma_start(out=U_[:], out_offset=None, in_=ub[:, :], in_offset=bass.IndirectOffsetOnAxis(ap=idxT[:, t:t + 1], axis=0)), reads=[r_idxT, r_ub], writes=[r_U])
                    P.dma("sp", lambda e, H_=H_, t=t, tok0=tok0: e.dma_start(out=H_[:], in_=h2d[tok0 + t:tok0 + t + 1, :].partition_broadcast(128)), reads=[r_h2d], writes=[r_H])
                    P.dma("pool", lambda e, V_=V_, t=t: e.indirect_dma_start(out=V_[:], out_offset=None, in_=vbd[:, :], in_offset=bass.IndirectOffsetOnAxis(ap=idxT[:, t:t + 1], axis=0)), reads=[r_idxT, r_vbd], writes=[r_V])
                    P.op("dve", lambda e, U_=U_, H_=H_, t=t: e.scalar_tensor_tensor(out=U_[:], in0=U_[:], scalar=1.0, in1=H_[:], op0=ALU.mult, op1=ALU.mult, accum_out=ACTT[:, t:t + 1]), reads=[r_U, r_H], writes=[r_U, r_ACTT])
                    P.op("dve", lambda e, t=t: e.tensor_tensor(out=x2_[:], in0=ACTT[:, t:t + 1], in1=ACTT[:, t:t + 1], op=ALU.mult), reads=[r_ACTT], writes=[r_x2])
                    P.op("dve", lambda e: e.tensor_scalar(out=x2_[:], in0=x2_[:], scalar1=0.044715, scalar2=1.0, op0=ALU.mult, op1=ALU.add), reads=[r_x2], writes=[r_x2])
                    P.op("dve", lambda e, t=t: e.tensor_tensor(out=uu_[:], in0=x2_[:], in1=ACTT[:, t:t + 1], op=ALU.mult), reads=[r_x2, r_ACTT], writes=[r_uu])
                    P.op("act", lambda e: e.activation(out=sg_[:], in_=uu_[:], func=AF.Sigmoid, scale=1.5957691216057308), reads=[r_uu], writes=[r_sg])
                    P.op("dve", lambda e, t=t: e.scalar_tensor_tensor(out=w1_[:], in0=sg_[:], scalar=ACTT[:, t:t + 1], in1=gwT[:, t:t + 1], op0=ALU.mult, op1=ALU.mult), reads=[r_sg, r_ACTT, r_gwT], writes=[r_w1])
                    P.op("act", lambda e, ws_=ws_, t=t: e.activation(out=ws_[:], in_=Zc[:, 127 - t:255 - t], func=AF.Copy, scale=w1_[:, 0:1]), reads=[r_Zc, r_w1], writes=[r_ws])
                    for cb in range(8):
                        p_, r_p = ps[cb]
                        P.op("pe", lambda e, p_=p_, ws_=ws_, V_=V_, cb=cb, t=t: e.matmul(p_[:, :], lhsT=ws_[:], rhs=V_[:, cb * 512:(cb + 1) * 512], start=(t == 0), stop=(t == 127)), reads=[r_ws, r_V], writes=[r_p])
                b4, r_b4 = load_bc(g2o[s:s + 1, :])
                for cb in range(8):
                    p_, r_p = ps[cb]
                    tf_, r_tf = tmpf[cb % 2]
                    P.op("dve", lambda e, p_=p_, tf_=tf_, cb=cb, b4=b4: e.tensor_tensor(out=tf_[:], in0=p_[:, :], in1=b4[:, cb * 512:(cb + 1) * 512], op=ALU.mult), reads=[r_p, r_b4], writes=[r_tf])
                    P.op("pool", lambda e, tf_=tf_, cb=cb: e.tensor_tensor(out=xt[:, cb * 512:(cb + 1) * 512], in0=xt[:, cb * 512:(cb + 1) * 512], in1=tf_[:], op=ALU.add), reads=[r_tf, r_xt], writes=[r_xt])
                rmsn(None)
                b5, r_b5 = load_bc(fg[0:1, :])
                u1, r_u1 = uv[1]
                P.op("dve", lambda e, b5=b5: e.scalar_tensor_tensor(out=u1[:], in0=xt[:], scalar=rstd[:, 0:1], in1=b5[:], op0=ALU.mult, op1=ALU.mult), reads=[r_xt, r_rstd, r_b5], writes=[r_u1])
                P.dma("pool", lambda e, tok0=tok0: e.dma_start(out=y[tok0:tok0 + 128, :], in_=u1[:]), reads=[r_u1], writes=[r_y])
            P.barrier()
            P.flush()
    return nc


def _T(a, KD):
    return np.ascontiguousarray(a.T.reshape(KD, 128, -1).transpose(1, 0, 2))


def _w_own(w_in, c, D):
    mw = D // 2
    g0 = 4 * mw
    n0 = g0 + 32
    sl = lambda base: w_in[:, base + c * 256: base + (c + 1) * 256]
    gates = w_in[:, [g0 + g * 8 + c for g in range(4)]]
    return np.ascontiguousarray(np.concatenate(
        [sl(0), sl(mw), sl(n0), sl(n0 + mw), sl(mw), sl(2 * mw), sl(3 * mw), sl(n0 + 2 * mw), gates], axis=1))


def _na_consts(rpb2):
    z = np.zeros((2, 15, 127), np.float32)
    z[:, :, 48:79] = rpb2
    cidx = np.arange(64)
    cs = np.clip(cidx - 8, 0, 48)
    ok = (cidx[None, :] >= cs[:, None]) & (cidx[None, :] < cs[:, None] + 16)
    m = np.full((64, 64), NEG, np.float32)
    m[ok] = 0.0
    return z.reshape(30, 127), m


def kernel(x_prompt, x_sample, c_prompt, c_sample, ada_w, ada_b, norm1_g, w_in, gate_b, mlstm_norm_g, na_rpb, w_out,
           norm2_g, peer_wq, peer_k1, peer_k2, peer_u, peer_v, final_g):
    cfg = dict(CFG)
    D, NCORE = cfg["D"], cfg["NC"]
    KD = D // 128
    f = lambda a: np.asarray(a, dtype=np.float32)
    x_all = np.ascontiguousarray(np.concatenate([f(x_prompt)[0], f(x_sample).reshape(-1, D)], axis=0))
    c_all = np.concatenate([f(c_prompt), f(c_sample)], axis=0)
    ncol = 6 * D // NCORE
    nc0 = build_l0(cfg)
    cT = _T(c_all, KD)
    in0 = [{"cT": cT, "w": np.ascontiguousarray(f(ada_w)[0][:, c * ncol:(c + 1) * ncol]),
            "b": np.ascontiguousarray(f(ada_b)[0][None, c * ncol:(c + 1) * ncol])} for c in range(NCORE)]
    r0 = run_bass_kernel_spmd(nc0, in0, core_ids=list(range(NCORE)))
    mod = np.concatenate([r0.results[c]["y"] for c in range(NCORE)], axis=1)
    sh1, sc1, g1, sh2, sc2, g2 = np.split(mod, 6, axis=1)
    nc1 = build_l1(cfg, phases="ABCD")
    n1g = np.ascontiguousarray(f(norm1_g)[0].reshape(KD, 128).T)
    sc1T, sh1T = _T(sc1, KD), _T(sh1, KD)
    in1 = []
    for c in range(NCORE):
        z, m = _na_consts(f(na_rpb)[0, 2 * c:2 * c + 2])
        in1.append({"x": x_all, "sc1T": sc1T, "sh1T": sh1T, "n1g": n1g, "w": _w_own(f(w_in)[0], c, D),
                    "gb": np.ascontiguousarray(f(gate_b)[0][[g * 8 + c for g in range(4)]][None, :]),
                    "mng": np.ascontiguousarray(f(mlstm_norm_g)[0][None, c * 256:(c + 1) * 256]), "zall": z, "nmask": m})
    r1 = run_bass_kernel_spmd(nc1, in1, core_ids=list(range(NCORE)))
    mixT = np.concatenate([np.asarray(r1.results[c]["mix"]) for c in range(NCORE)], axis=1)
    del in1, r1
    SEQ, DB, DS, NK = cfg["SEQ"], cfg["DB"], cfg["DS"], cfg["NKEYS"]
    TPp = SEQ // NCORE
    TSs = DB * DS // NCORE
    nc2 = build_l2(cfg)
    perm = np.concatenate([np.concatenate([np.arange(c * 256, (c + 1) * 256), D // 2 + np.arange(c * 256, (c + 1) * 256)]) for c in range(NCORE)])
    wo = np.ascontiguousarray(f(w_out)[0][perm, :])
    wq = np.ascontiguousarray(f(peer_wq)[0])
    kT = np.zeros((128, 16, NK), np.float32)
    k1, k2 = f(peer_k1)[0], f(peer_k2)[0]
    for h in range(8):
        kT[:, 2 * h, :] = k1[h].T
        kT[:, 2 * h + 1, :] = k2[h].T
    uu = np.ascontiguousarray(f(peer_u)[0])
    vv = np.ascontiguousarray(f(peer_v)[0])
    n2 = np.ascontiguousarray(f(norm2_g)[0][None, :])
    fgv = np.ascontiguousarray(f(final_g).reshape(1, D))
    io = np.arange(256, dtype=np.float32)[None]
    in2 = []
    for c in range(NCORE):
        rows = np.concatenate([np.arange(c * TPp, (c + 1) * TPp), SEQ + np.arange(c * TSs, (c + 1) * TSs)])
        sidx = [0, 1 + (c * TSs) // DS]
        in2.append({"x": np.ascontiguousarray(x_all[rows]), "mixo": np.ascontiguousarray(mixT[rows]), "wo": wo, "wq": wq,
                    "g1o": np.ascontiguousarray(g1[sidx]), "g2o": np.ascontiguousarray(g2[sidx]),
                    "sc2o": np.ascontiguousarray(sc2[sidx]), "sh2o": np.ascontiguousarray(sh2[sidx]),
                    "n2g": n2, "fg": fgv, "kT": kT, "u": uu, "v": vv, "io256": io})
    r2 = run_bass_kernel_spmd(nc2, in2, core_ids=list(range(NCORE)))
    y_prompt = np.zeros((1, SEQ, D), np.float32)
    y_sample = np.zeros((DB * DS, D), np.float32)
    for c in range(NCORE):
        yc = np.asarray(r2.results[c]["y"])
        y_prompt[0, c * TPp:(c + 1) * TPp] = yc[:TPp]
        y_sample[c * TSs:(c + 1) * TSs] = yc[TPp:]
    return (y_prompt, y_sample.reshape(DB, DS, D))
```
